# Optimizing a Trainium2 kernel written in Bass

```python
import jax
import jax.numpy as jnp
from jax import lax
import numpy as np

D_MODEL = 2048
BATCH = 4
SEQ = 4096
DEPTH = 1

HEAD_DIM = 128
GDN_HEADS = 8
MOBA_HEADS = 8
GDN_WIDTH = GDN_HEADS * HEAD_DIM
MOBA_WIDTH = MOBA_HEADS * HEAD_DIM
MIX_WIDTH = GDN_WIDTH + MOBA_WIDTH
CONV_WIDTH = 4
GDN_CHUNK = 64
MOBA_BLOCK = 256
MOBA_TOPK = 3
MOBA_Q_CHUNK = 32
N_GROUPS = 4
EXPERTS_PER_GROUP = 8
N_EXPERTS = N_GROUPS * EXPERTS_PER_GROUP
TOPK_IN_GROUP = 2
D_EXPERT = 512
EXPERT_ROW_BLOCK = 128
RMS_EPS = 1e-6
NEG_INF = -1e30
IN_SPLITS = (GDN_WIDTH, GDN_WIDTH, GDN_WIDTH, GDN_WIDTH, GDN_HEADS, GDN_HEADS, MOBA_WIDTH, MOBA_WIDTH, MOBA_WIDTH)
IN_PROJ_WIDTH = 4 * GDN_WIDTH + 2 * GDN_HEADS + 3 * MOBA_WIDTH

kernel_name = 'hybrid_gdn_moba_hier_moe_block'


def _rms(x):
    return x * lax.rsqrt(jnp.mean(x * x, axis=-1, keepdims=True) + RMS_EPS)


def rmsnorm(x, w):
    return (_rms(x.astype(jnp.float32)) * w.astype(jnp.float32)).astype(x.dtype)


def l2norm(x):
    return x * lax.rsqrt(jnp.sum(x * x, axis=-1, keepdims=True) + 1e-6)


def alibi_slopes(n_heads):
    return jnp.exp2(-8.0 * jnp.arange(1, n_heads + 1, dtype=jnp.float32) / n_heads)


def split_columns(p, sizes):
    offs = np.cumsum(np.array(sizes))[:-1].tolist()
    return jnp.split(p, offs, axis=-1)


def causal_depthwise_conv(x, w):
    return lax.conv_general_dilated(
        x, w[:, None, :].astype(x.dtype), window_strides=(1,),
        padding=[(CONV_WIDTH - 1, 0)], dimension_numbers=('NWC', 'WIO', 'NWC'),
        feature_group_count=x.shape[-1])


def gated_delta_rule_chunked(q, k, v, g, beta):
    B, H, T, Dk = q.shape
    Dv = v.shape[-1]
    C = GDN_CHUNK
    N = T // C
    q = q.reshape(B, H, N, C, Dk)
    k = k.reshape(B, H, N, C, Dk)
    v = v.reshape(B, H, N, C, Dv)
    g = jnp.cumsum(g.reshape(B, H, N, C), axis=-1)
    beta = beta.reshape(B, H, N, C)
    causal = jnp.tril(jnp.ones((C, C), bool))
    strict = jnp.tril(jnp.ones((C, C), bool), -1)
    decay = jnp.exp(jnp.where(causal, g[..., :, None] - g[..., None, :], NEG_INF))
    kb = k * beta[..., None]
    L = jnp.where(strict, jnp.einsum('bhnid,bhnjd->bhnij', kb, k) * decay, 0.0)
    eye = jnp.eye(C, dtype=jnp.float32)
    rhs = jnp.concatenate([v * beta[..., None], kb * jnp.exp(g)[..., None]], axis=-1)
    sol = lax.linalg.triangular_solve(eye + L, rhs, left_side=True, lower=True)
    u, w = sol[..., :Dv], sol[..., Dv:]
    attn = jnp.where(causal, jnp.einsum('bhnid,bhnjd->bhnij', q, k) * decay, 0.0)
    q_dec = q * jnp.exp(g)[..., None]
    k_dec = k * jnp.exp(g[..., -1:] - g)[..., None]
    g_tot = jnp.exp(g[..., -1])
    xs = tuple(jnp.moveaxis(t, 2, 0) for t in (u, w, q_dec, k_dec, attn, g_tot))

    def step(S, inp):
        u_i, w_i, qd_i, kd_i, a_i, gt_i = inp
        v_new = u_i - jnp.einsum('bhck,bhkv->bhcv', w_i, S)
        o_i = jnp.einsum('bhck,bhkv->bhcv', qd_i, S) + jnp.einsum('bhcj,bhjv->bhcv', a_i, v_new)
        S = S * gt_i[..., None, None] + jnp.einsum('bhck,bhcv->bhkv', kd_i, v_new)
        return S, o_i

    S0 = jnp.zeros((B, H, Dk, Dv), jnp.float32)
    _, o = lax.scan(step, S0, xs)
    return jnp.moveaxis(o, 0, 2).reshape(B, H, T, Dv)


def gdn_mixer(q, k, v, z, b, a, conv_w, A_log, dt_bias, out_norm_w):
    B, T, _ = q.shape
    qkv = jax.nn.silu(causal_depthwise_conv(jnp.concatenate([q, k, v], axis=-1), conv_w))
    q, k, v = jnp.split(qkv, 3, axis=-1)

    def heads(t):
        return t.reshape(B, T, GDN_HEADS, HEAD_DIM).transpose(0, 2, 1, 3).astype(jnp.float32)

    qh = l2norm(heads(q)) * (HEAD_DIM ** -0.5)
    kh = l2norm(heads(k))
    vh = heads(v)
    beta = jax.nn.sigmoid(b.astype(jnp.float32)).transpose(0, 2, 1)
    g = -(jnp.exp(A_log.astype(jnp.float32))
          * jax.nn.softplus(a.astype(jnp.float32) + dt_bias.astype(jnp.float32)))
    g = g.transpose(0, 2, 1)
    o = gated_delta_rule_chunked(qh, kh, vh, g, beta).transpose(0, 2, 1, 3)
    zf = z.reshape(B, T, GDN_HEADS, HEAD_DIM).astype(jnp.float32)
    o = rmsnorm(o, out_norm_w) * jax.nn.silu(zf)
    return o.reshape(B, T, GDN_WIDTH).astype(q.dtype)


def moba_mixer(q, k, v, out_norm_w):
    B, T, _ = q.shape
    H, D = MOBA_HEADS, HEAD_DIM

    def heads(t):
        return t.reshape(B, T, H, D).transpose(0, 2, 1, 3).astype(jnp.float32)

    qh, kh, vh = heads(q), heads(k), heads(v)
    n_blocks = -(-T // MOBA_BLOCK)
    t_pad = n_blocks * MOBA_BLOCK
    pad = [(0, 0), (0, 0), (0, t_pad - T), (0, 0)]
    k_blk = jnp.pad(kh, pad).reshape(B, H, n_blocks, MOBA_BLOCK, D)
    v_blk = jnp.pad(vh, pad).reshape(B, H, n_blocks, MOBA_BLOCK, D)
    k_mean = jnp.mean(k_blk, axis=3)
    n_sel = min(MOBA_TOPK, n_blocks)
    q_block = jnp.arange(T) // MOBA_BLOCK
    gate = jnp.einsum('bhtd,bhnd->bhtn', qh, k_mean)
    gate = jnp.where(jnp.arange(n_blocks)[None, :] < q_block[:, None], gate, NEG_INF)
    _, sel_idx = lax.top_k(gate, n_sel)
    slopes = alibi_slopes(H)
    scale = HEAD_DIM ** -0.5
    QC = MOBA_Q_CHUNK
    n_qc = T // QC
    q_c = qh.reshape(B, H, n_qc, QC, D).transpose(2, 0, 1, 3, 4)
    idx_c = sel_idx.reshape(B, H, n_qc, QC, n_sel).transpose(2, 0, 1, 3, 4)
    b_ix = jnp.arange(B)[:, None, None, None]
    h_ix = jnp.arange(H)[None, :, None, None]
    offs = jnp.arange(MOBA_BLOCK)

    def chunk(args):
        qi, idx, c = args
        start = c * QC
        blk = start // MOBA_BLOCK
        t_pos = start + jnp.arange(QC)
        k_own = lax.dynamic_index_in_dim(k_blk, blk, axis=2, keepdims=False)
        v_own = lax.dynamic_index_in_dim(v_blk, blk, axis=2, keepdims=False)
        dist_own = (t_pos[:, None] - (blk * MOBA_BLOCK + offs)[None, :]).astype(jnp.float32)
        s_own = (jnp.einsum('bhqd,bhkd->bhqk', qi, k_own) * scale
                 - slopes[None, :, None, None] * dist_own)
        s_own = jnp.where(dist_own >= 0, s_own, NEG_INF)
        k_sel = k_blk[b_ix, h_ix, idx]
        v_sel = v_blk[b_ix, h_ix, idx]
        dist_sel = (t_pos[None, None, :, None, None]
                    - (idx[..., None] * MOBA_BLOCK + offs)).astype(jnp.float32)
        s_sel = (jnp.einsum('bhqd,bhqskd->bhqsk', qi, k_sel) * scale
                 - slopes[None, :, None, None, None] * dist_sel)
        valid = jnp.arange(n_sel) < blk
        s_sel = jnp.where(valid[:, None], s_sel, NEG_INF).reshape(B, H, QC, n_sel * MOBA_BLOCK)
        p = jax.nn.softmax(jnp.concatenate([s_own, s_sel], axis=-1), axis=-1)
        p_own = p[..., :MOBA_BLOCK]
        p_sel = p[..., MOBA_BLOCK:].reshape(B, H, QC, n_sel, MOBA_BLOCK)
        return (jnp.einsum('bhqk,bhkd->bhqd', p_own, v_own)
                + jnp.einsum('bhqsk,bhqskd->bhqd', p_sel, v_sel))

    o = lax.map(chunk, (q_c, idx_c, jnp.arange(n_qc)))
    o = o.transpose(1, 0, 3, 2, 4).reshape(B, T, H, D)
    o = rmsnorm(o, out_norm_w)
    return o.reshape(B, T, MOBA_WIDTH).astype(q.dtype)


def grouped_expert_ffn(xf, expert_id, w_gate, w_up, w_down):
    n_tok, k = expert_id.shape
    d = xf.shape[-1]
    RB = EXPERT_ROW_BLOCK
    n_assign = n_tok * k
    e_flat = expert_id.reshape(-1)
    tok = jnp.arange(n_assign) // k
    order = jnp.argsort(e_flat)
    e_sorted = e_flat[order]
    counts = jnp.bincount(e_flat, length=N_EXPERTS)
    padded = (counts + RB - 1) // RB * RB
    pad_end = jnp.cumsum(padded)
    pad_start = pad_end - padded
    seg_start = jnp.cumsum(counts) - counts
    dest = pad_start[e_sorted] + jnp.arange(n_assign) - seg_start[e_sorted]
    n_rb = -(-n_assign // RB) + N_EXPERTS
    src_tok = jnp.zeros((n_rb * RB,), jnp.int32).at[dest].set(tok[order])
    blk_expert = jnp.minimum(jnp.searchsorted(pad_end, jnp.arange(n_rb) * RB, side='right'), N_EXPERTS - 1)
    xb = xf[src_tok].reshape(n_rb, RB, d)

    def run(args):
        xi, e = args
        h = jax.nn.silu(xi @ w_gate[e]) * (xi @ w_up[e])
        return h @ w_down[e]

    yb = lax.map(run, (xb, blk_expert)).reshape(n_rb * RB, d)
    y = jnp.zeros((n_assign, d), yb.dtype).at[order].set(yb[dest])
    return y.reshape(n_tok, k, d)


def hierarchical_moe(x, w_rg, b_rg, w_re, b_re, w_gate, w_up, w_down):
    B, T, D = x.shape
    xf = x.reshape(-1, D)
    n_tok = xf.shape[0]
    p_g = jax.nn.softmax((xf @ w_rg).astype(jnp.float32) + b_rg.astype(jnp.float32), axis=-1)
    p_top_g, g_idx = lax.top_k(p_g, 1)
    logits_e = ((xf @ w_re).astype(jnp.float32) + b_re.astype(jnp.float32)).reshape(
        n_tok, N_GROUPS, EXPERTS_PER_GROUP)
    logits_e = jnp.take_along_axis(logits_e, g_idx[:, :, None], axis=1)[:, 0]
    p_e = jax.nn.softmax(logits_e, axis=-1)
    p_top_e, e_idx = lax.top_k(p_e, TOPK_IN_GROUP)
    gates = p_top_g * p_top_e / jnp.sum(p_top_e, axis=-1, keepdims=True)
    expert_id = g_idx * EXPERTS_PER_GROUP + e_idx
    y = grouped_expert_ffn(xf, expert_id, w_gate, w_up, w_down)
    out = jnp.einsum('nk,nkd->nd', gates, y.astype(jnp.float32))
    return out.reshape(B, T, D).astype(x.dtype)


def setup_inputs(seed: int = 0) -> dict:
    key = jax.random.key(seed)
    ks = jax.random.split(key, 20)
    f32 = jnp.float32

    def nrm(k, shape, scale):
        return jax.random.normal(k, shape, f32) * scale

    x = nrm(ks[0], (BATCH, SEQ, D_MODEL), 1.0)
    norm_mix_w = 1.0 + nrm(ks[1], (DEPTH, D_MODEL), 0.02)
    w_in = nrm(ks[2], (DEPTH, D_MODEL, IN_PROJ_WIDTH), D_MODEL ** -0.5)
    gdn_conv_w = nrm(ks[3], (DEPTH, CONV_WIDTH, 3 * GDN_WIDTH), CONV_WIDTH ** -0.5)
    gdn_A_log = jnp.log(jax.random.uniform(ks[4], (DEPTH, GDN_HEADS), f32, 1.0, 16.0))
    dt = jnp.exp(jax.random.uniform(ks[5], (DEPTH, GDN_HEADS), f32, float(np.log(1e-3)), float(np.log(1e-1))))
    gdn_dt_bias = dt + jnp.log(-jnp.expm1(-dt))
    gdn_out_norm_w = 1.0 + nrm(ks[6], (DEPTH, HEAD_DIM), 0.02)
    moba_out_norm_w = 1.0 + nrm(ks[7], (DEPTH, HEAD_DIM), 0.02)
    w_out = nrm(ks[8], (DEPTH, MIX_WIDTH, D_MODEL), MIX_WIDTH ** -0.5)
    norm_ffn_w = 1.0 + nrm(ks[9], (DEPTH, D_MODEL), 0.02)
    w_router_group = nrm(ks[10], (DEPTH, D_MODEL, N_GROUPS), D_MODEL ** -0.5)
    b_router_group = nrm(ks[11], (DEPTH, N_GROUPS), 0.01)
    w_router_expert = nrm(ks[12], (DEPTH, D_MODEL, N_EXPERTS), D_MODEL ** -0.5)
    b_router_expert = nrm(ks[13], (DEPTH, N_EXPERTS), 0.01)
    w_expert_gate = nrm(ks[14], (DEPTH, N_EXPERTS, D_MODEL, D_EXPERT), D_MODEL ** -0.5)
    w_expert_up = nrm(ks[15], (DEPTH, N_EXPERTS, D_MODEL, D_EXPERT), D_MODEL ** -0.5)
    w_expert_down = nrm(ks[16], (DEPTH, N_EXPERTS, D_EXPERT, D_MODEL), D_EXPERT ** -0.5)
    norm_final_w = 1.0 + nrm(ks[17], (D_MODEL,), 0.02)
    return {'x': x, 'norm_mix_w': norm_mix_w, 'w_in': w_in, 'gdn_conv_w': gdn_conv_w,
            'gdn_A_log': gdn_A_log, 'gdn_dt_bias': gdn_dt_bias, 'gdn_out_norm_w': gdn_out_norm_w,
            'moba_out_norm_w': moba_out_norm_w, 'w_out': w_out, 'norm_ffn_w': norm_ffn_w,
            'w_router_group': w_router_group, 'b_router_group': b_router_group,
            'w_router_expert': w_router_expert, 'b_router_expert': b_router_expert,
            'w_expert_gate': w_expert_gate, 'w_expert_up': w_expert_up,
            'w_expert_down': w_expert_down, 'norm_final_w': norm_final_w}


def reference(x, norm_mix_w, w_in, gdn_conv_w, gdn_A_log, gdn_dt_bias, gdn_out_norm_w,
              moba_out_norm_w, w_out, norm_ffn_w, w_router_group, b_router_group,
              w_router_expert, b_router_expert, w_expert_gate, w_expert_up, w_expert_down,
              norm_final_w):
    for l in range(DEPTH):
        h = rmsnorm(x, norm_mix_w[l])
        proj = h @ w_in[l]
        gq, gk, gv, gz, gb, ga, mq, mk, mv = split_columns(proj, IN_SPLITS)
        o_gdn = gdn_mixer(gq, gk, gv, gz, gb, ga, gdn_conv_w[l], gdn_A_log[l],
                          gdn_dt_bias[l], gdn_out_norm_w[l])
        o_moba = moba_mixer(mq, mk, mv, moba_out_norm_w[l])
        mix = jnp.concatenate([o_gdn, o_moba], axis=-1)
        x = x + (mix @ w_out[l]).astype(x.dtype)
        h = rmsnorm(x, norm_ffn_w[l])
        x = x + hierarchical_moe(h, w_router_group[l], b_router_group[l], w_router_expert[l],
                                 b_router_expert[l], w_expert_gate[l], w_expert_up[l],
                                 w_expert_down[l])
    return rmsnorm(x, norm_final_w)
```

```python
import contextlib
import numpy as np
import ml_dtypes
import concourse.bass as bass
import concourse.mybir as mybir
from concourse.bass_utils import run_bass_kernel_spmd

F32 = mybir.dt.float32
BF16 = mybir.dt.bfloat16
I32 = mybir.dt.int32
AF = mybir.ActivationFunctionType
ALU = mybir.AluOpType
AX = mybir.AxisListType

D = 2048
T = 4096
NH = 4
HD = 128
NFM = 20
TM0 = NFM * 128
WCOLS = TM0 + 512 + 512 + 8
EPS = 1e-6


class Sched:
    ENGS = ("pe", "act", "dve", "pool", "sp")

    _G = {}

    def __init__(self, nc, stack):
        self.nc = nc
        self.stack = stack
        self.ops = {e: [] for e in self.ENGS}
        g = Sched._G.get(id(nc))
        if g is None:
            g = {"csem": {e: nc.alloc_semaphore("c_%s" % e) for e in self.ENGS}, "cnt": {e: 0 for e in self.ENGS},
                 "seen": {e: {} for e in self.ENGS}, "pool": []}
            Sched._G.clear()
            Sched._G[id(nc)] = g
        self.g = g
        self.csem = g["csem"]
        self.cnt = g["cnt"]
        self.seen = g["seen"]
        self.last_w = {}
        self.readers = {}
        self.dsem = {}
        self.all_dma_events = {}
        self._cap = None

    def capture(self):
        self._cap = []

    def end_capture(self):
        c, self._cap = self._cap, None
        return c

    def replay(self, items):
        for kind, eng, fn, reads, writes, key in items:
            if kind == "op":
                self.op(eng, fn, reads, writes)
            else:
                self.dma(eng, fn, reads, writes, key)

    def _waits(self, eng, reads, writes):
        evs = []
        for b in reads:
            if b in self.last_w:
                evs.append(self.last_w[b])
        for b in writes:
            if b in self.last_w:
                evs.append(self.last_w[b])
            evs.extend(self.readers.get(b, ()))
        need = {}
        for sem, val in evs:
            if eng == "pe" and sem is self.csem["pe"]:
                continue
            if self.seen[eng].get(sem, 0) < val:
                need[sem] = max(need.get(sem, 0), val)
        for sem, val in need.items():
            self.seen[eng][sem] = val
        return list(need.items())

    def _record(self, ev, reads, writes):
        for b in reads:
            self.readers.setdefault(b, []).append(ev)
        for b in writes:
            self.last_w[b] = ev
            self.readers[b] = []

    def op(self, eng, fn, reads=(), writes=()):
        if self._cap is not None:
            self._cap.append(("op", eng, fn, tuple(reads), tuple(writes), None))
            return
        waits = self._waits(eng, reads, writes)
        self.cnt[eng] += 1
        ev = (self.csem[eng], self.cnt[eng])
        sem = self.csem[eng]

        def emit(e):
            for s, v in waits:
                e.wait_ge(s, v)
            fn(e).then_inc(sem, 1)
        self.ops[eng].append(emit)
        self._record(ev, reads, writes)

    def dma(self, eng, fn, reads=(), writes=(), key=None):
        if self._cap is not None:
            self._cap.append(("dma", eng, fn, tuple(reads), tuple(writes), key))
            return
        waits = self._waits(eng, reads, writes)
        if key not in self.dsem:
            i = len(self.dsem)
            if i >= len(self.g["pool"]):
                self.g["pool"].append([self.nc.alloc_semaphore("d%d" % i), 0])
            self.dsem[key] = self.g["pool"][i]
        ent = self.dsem[key]
        ent[1] += 16
        sem = ent[0]
        ev = (sem, ent[1])
        self.all_dma_events[sem] = ev

        def emit(e):
            for s, v in waits:
                e.wait_ge(s, v)
            fn(e).then_inc(sem, 16)
        self.ops[eng].append(emit)
        self._record(ev, reads, writes)

    def finish(self):
        finals = list(self.all_dma_events.values()) + [(self.csem[e], self.cnt[e]) for e in self.ENGS if self.cnt[e]]
        for eng in self.ENGS:
            waits = [(s, v) for s, v in finals if self.seen[eng].get(s, 0) < v]

            def emit(e, waits=waits):
                for s, v in waits:
                    e.wait_ge(s, v)
            self.ops[eng].append(emit)
        with self.nc.Block() as block:
            @block.tensor
            def _(e):
                for f in self.ops["pe"]:
                    f(e)

            @block.scalar
            def _(e):
                for f in self.ops["act"]:
                    f(e)

            @block.vector
            def _(e):
                for f in self.ops["dve"]:
                    f(e)

            @block.gpsimd
            def _(e):
                for f in self.ops["pool"]:
                    f(e)

            @block.sync
            def _(e):
                for f in self.ops["sp"]:
                    f(e)


def merge_prop(A, B):
    out = []
    i = j = 0
    while i < len(A) or j < len(B):
        if j >= len(B) or (i < len(A) and i * len(B) <= j * len(A)):
            out.append(A[i])
            i += 1
        else:
            out.append(B[j])
            j += 1
    return out


def phase_a1(nc, io, dbg):
    x = io["x_b"]
    with contextlib.ExitStack() as st:
        S = Sched(nc, st)
        sb = lambda name, shape, dt: st.enter_context(nc.sbuf_tensor("a1_" + name, shape, dt))
        W = sb("W", [128, 16, WCOLS], BF16)
        wbc = sb("wbc", [128, D], F32)
        ident = sb("ident", [128, 128], BF16)
        xt = [sb("xt%d" % i, [128, D], F32) for i in range(2)]
        hn = [sb("hn%d" % i, [128, D], BF16) for i in range(2)]
        hT = [sb("hT%d" % i, [128, 16, 512], BF16) for i in range(2)]
        junk = sb("junk", [128, D], BF16)
        stat = [sb("stat%d" % i, [128, 4], F32) for i in range(2)]
        stg = [sb("stg%d" % i, [128, 512], F32) for i in range(4)]
        stgb = [sb("stgb%d" % i, [128, 512], BF16) for i in range(4)]
        stgs = [sb("stgs%d" % i, [128, 8], F32) for i in range(2)]
        PT = [st.enter_context(nc.psum_tensor("a1_PT%d" % i, [128, 2048], BF16)) for i in range(1)]
        PM = [st.enter_context(nc.psum_tensor("a1_PM%d" % i, [128, 512], F32)) for i in range(4)]

        S.dma("sp", lambda e: e.dma_start(out=wbc[:], in_=io["nw1"][0:1, :].partition_broadcast(128)),
              writes=["wbc"], key="c_wbc")
        S.dma("sp", lambda e: e.dma_start(out=ident[:], in_=io["ident"]), writes=["ident"], key="c_ident")
        epsb = sb("epsb", [128, 1], F32)
        S.op("dve", lambda e: e.memset(epsb[:], EPS), writes=["epsb"])
        for k in range(16):
            S.dma("pool", lambda e, k=k: e.dma_start(out=W[:, k, :], in_=io["w_in"][k * 128:(k + 1) * 128, :]),
                  writes=["W"], key="W")

        evac_i = [0]

        def evac(out_ap, in_ap, reads, writes):
            writes = list(writes) + list(reads)
            reads = []
            evac_i[0] += 1
            if evac_i[0] % 2:
                S.op("act", lambda e: e.copy(out=out_ap, in_=in_ap), reads=reads, writes=writes)
            else:
                S.op("dve", lambda e: e.tensor_copy(out=out_ap, in_=in_ap), reads=reads, writes=writes)

        mi = 0
        S1, S2 = [], []
        for st_i in range(T // 512):
            hTb = hT[st_i % 2]
            hTk = ("hT", st_i % 2)
            S.capture()
            for tt in range(4):
                ti = st_i * 4 + tt
                xb_, hb_, sb_ = xt[ti % 2], hn[ti % 2], stat[ti % 2]
                kx, kh, ks = ("xt", ti % 2), ("hn", ti % 2), ("stat", ti % 2)
                S.dma("sp", lambda e, ti=ti, xb_=xb_: e.dma_start(out=xb_[:], in_=x[ti * 128:(ti + 1) * 128, :]),
                      writes=[kx], key=kx)
                S.op("act", lambda e, xb_=xb_, sb_=sb_: e.activation(out=junk[:], in_=xb_[:], func=AF.Square,
                                                                    accum_out=sb_[:, 0:1]),
                     reads=[kx], writes=["junk", ks])
                S.op("act", lambda e, sb_=sb_: e.activation(out=sb_[:, 1:2], in_=sb_[:, 0:1], func=AF.Sqrt,
                                                           bias=epsb[:, 0:1], scale=1.0 / D),
                     reads=[ks, "epsb"], writes=[ks])
                S.op("dve", lambda e, sb_=sb_: e.reciprocal(out=sb_[:, 2:3], in_=sb_[:, 1:2]),
                     reads=[ks], writes=[ks])
                S.op("dve", lambda e, xb_=xb_, hb_=hb_, sb_=sb_: e.scalar_tensor_tensor(
                    out=hb_[:], in0=xb_[:], scalar=sb_[:, 2:3], in1=wbc[:], op0=ALU.mult, op1=ALU.mult),
                     reads=[kx, ks, "wbc"], writes=[kh])
                for k in range(16):
                    S.op("pe", lambda e, k=k, hb_=hb_: e.transpose(out=PT[0][:, k * 128:(k + 1) * 128],
                                                                   in_=hb_[:, k * 128:(k + 1) * 128],
                                                                   identity=ident[:]),
                         reads=[kh, "ident"], writes=["PT"])
                evac(hTb[:, :, tt * 128:(tt + 1) * 128], PT[0][:].rearrange("p (k t) -> p k t", k=16),
                     reads=["PT"], writes=[hTk])
            S1.append(S.end_capture())
            S.capture()
            for c in range(NFM):
                pm = PM[mi % 4]
                kp = ("PM", mi % 4)
                mi += 1
                for k in range(16):
                    S.op("pe", lambda e, k=k, c=c, pm=pm, hTb=hTb: e.matmul(
                        pm[:], lhsT=W[:, k, c * 128:(c + 1) * 128], rhs=hTb[:, k, :], start=(k == 0), stop=(k == 15)),
                         reads=[hTk, "W"], writes=[kp])
                grp, h = divmod(c, 4)
                dst = [io["gqT"], io["gkT"], io["gvT"], io["mqT"], io["mkT"]][grp]
                if grp < 3:
                    sg = stg[c % 4]
                    ksg = ("stg", c % 4)
                else:
                    sg = stgb[c % 4]
                    ksg = ("stgb", c % 4)
                evac(sg[:], pm[:], reads=[kp], writes=[ksg])
                S.dma("act", lambda e, dst=dst, h=h, sg=sg, st_i=st_i: e.dma_start(
                    out=dst[h, :, st_i * 512:(st_i + 1) * 512], in_=sg[:]), reads=[ksg], key=ksg)
            for tt in range(4):
                ti = st_i * 4 + tt
                for g in range(3):
                    c0 = TM0 + g * 512
                    nco = 512 if g < 2 else 8
                    pm = PM[mi % 4]
                    kp = ("PM", mi % 4)
                    mi += 1
                    for k in range(16):
                        S.op("pe", lambda e, k=k, pm=pm, hTb=hTb, tt=tt, c0=c0, nco=nco: e.matmul(
                            pm[:, 0:nco], lhsT=hTb[:, k, tt * 128:(tt + 1) * 128], rhs=W[:, k, c0:c0 + nco],
                            start=(k == 0), stop=(k == 15)),
                             reads=[hTk, "W"], writes=[kp])
                    if g == 0:
                        sg, ksg, dst = stg[tt % 4], ("stg", tt % 4), io["gz"]
                    elif g == 1:
                        sg, ksg, dst = stgb[tt % 4], ("stgb", tt % 4), io["mv"]
                    else:
                        sg, ksg, dst = stgs[tt % 2], ("stgs", tt % 2), io["gba"]
                    evac(sg[:, 0:nco], pm[:, 0:nco], reads=[kp], writes=[ksg])
                    S.dma("act", lambda e, dst=dst, sg=sg, ti=ti, nco=nco: e.dma_start(
                        out=dst[ti * 128:(ti + 1) * 128, :], in_=sg[:, 0:nco]), reads=[ksg], key=ksg)
            S2.append(S.end_capture())
        order = list(S1[0])
        for st_i in range(len(S2)):
            order.extend(S2[st_i])
            if st_i + 1 < len(S1):
                order.extend(S1[st_i + 1])
        S.replay(order)
        S.finish()


def phase_moba(nc, io, ctx=None, zero_xg=False):
    scale = float(HD) ** -0.5
    with contextlib.ExitStack() as st:
        if ctx is not None:
            st = ctx["st"]
        S = ctx["S"] if ctx is not None else Sched(nc, st)
        sb = lambda name, shape, dt: st.enter_context(nc.sbuf_tensor("mb_" + name, shape, dt))
        ps = lambda name, shape, dt: st.enter_context(nc.psum_tensor("mb_" + name, shape, dt))
        QT = [sb("QT%d" % i, [128, T], BF16) for i in range(2)]
        KT = [sb("KT%d" % i, [128, T], BF16) for i in range(2)]
        V = [sb("V%d" % i, [128, 32, 132], BF16) for i in range(2)]
        pastneg = sb("pastneg", [128, 512], F32)
        past01 = sb("past01", [128, 512], F32)
        abias = sb("abias", [128, 128], F32)
        cmask = sb("cmask", [128, 2, 256], BF16)
        nwb = sb("nwb", [128, 128], F32)
        epsb = sb("epsb", [128, 1], F32)
        ksumL = [sb("ksum%d" % i, [128, 16], F32) for i in range(2)]
        khiL = [sb("khi%d" % i, [128, 16], BF16) for i in range(2)]
        khfL = [sb("khf%d" % i, [128, 16], F32) for i in range(2)]
        kloL = [sb("klo%d" % i, [128, 16], BF16) for i in range(2)]
        gmL = [sb("gm%d" % i, [128, 512], F32) for i in range(2)]
        selL = [sb("sel%d" % i, [128, 512], F32) for i in range(2)]
        top8L = [sb("top8%d" % i, [128, 32, 8], F32) for i in range(2)]
        PTs = [sb("PTs%d" % i, [128, 256], BF16) for i in range(4)]
        acc = [sb("acc%d" % i, [128, 132], F32) for i in range(4)]
        osb = [sb("osb%d" % i, [128, 128], F32) for i in range(2)]
        ojk = sb("ojk", [128, 128], F32)
        ost = [sb("ost%d" % i, [128, 8], F32) for i in range(2)]
        obf = [sb("obf%d" % i, [128, 128], BF16) for i in range(2)]
        if ctx is None:
            PG = ps("PG", [128, 512], F32)
            PS = [ps("PS%d" % i, [128, 512], F32) for i in range(4)]
            PO = [ps("PO%d" % i, [128, 512], F32) for i in range(3)]
            KPG, KPS, KPO = "PG", [("PS", i) for i in range(4)], [("PO", i) for i in range(3)]
        else:
            bk = ctx["moba_banks"]
            PG, KPG = bk[0]
            PS, KPS = [bk[0][0], bk[1][0]], [bk[0][1], bk[1][1]]
            PO, KPO = [bk[2][0]], [bk[2][1]]
        NPS, NPO = len(PS), len(PO)

        S.dma("sp", lambda e: e.dma_start(out=pastneg[:], in_=io["pastneg"]), writes=["pastneg"], key="c_pastneg")
        S.dma("sp", lambda e: e.dma_start(out=past01[:], in_=io["past01"]), writes=["past01"], key="c_past01")
        S.dma("sp", lambda e: e.dma_start(out=abias[:], in_=io["abias"]), writes=["abias"], key="c_abias")
        S.dma("sp", lambda e: e.dma_start(out=cmask[:], in_=io["cmask"]), writes=["cmask"], key="c_cmask")
        S.dma("sp", lambda e: e.dma_start(out=nwb[:], in_=io["nwm"][0:1, :].partition_broadcast(128)),
              writes=["nwb"], key="c_nwbm")
        S.op("dve", lambda e: e.memset(epsb[:], EPS), writes=["epsb"])
        if zero_xg:
            zrow = sb("zrow", [128, D], BF16)
            S.op("pool", lambda e: e.memset(zrow[:], 0.0), writes=["zrow"])
            for zi in range((NE * CAP + 128) // 128):
                S.dma("act", lambda e, zi=zi: e.dma_start(out=io["xg"][zi * 128:(zi + 1) * 128, :], in_=zrow[:]), reads=["zrow"], key="c_xgz")
        si = 0
        oi = 0
        outer_cap = S._cap
        S._cap = None
        P = []
        Useg = []

        def head_body(h):
            nonlocal si, oi
            qt, kt_, v = QT[h % 2], KT[h % 2], V[h % 2]
            ksum, khi, khf, klo, gm, sel, top8 = (x[h % 2] for x in (ksumL, khiL, khfL, kloL, gmL, selL, top8L))
            kksum, kkhi, kkhf, kklo, kgm, ksel, ktop = (("pro", nm_, h % 2) for nm_ in ("ksum", "khi", "khf", "klo", "gm", "sel", "top8"))
            S.capture()
            kq, kk, kv = ("QT", h % 2), ("KT", h % 2), ("V", h % 2)
            S.dma("sp", lambda e, h=h, qt=qt: e.dma_start(out=qt[:], in_=io["mqT"][h]), writes=[kq], key=kq)
            S.dma("sp", lambda e, h=h, kt_=kt_: e.dma_start(out=kt_[:], in_=io["mkT"][h]), writes=[kk], key=kk)
            S.dma("sp", lambda e, h=h, v=v: e.dma_start(
                out=v[:, :, 0:128], in_=io["mv"][:, h * 128:(h + 1) * 128].rearrange("(n p) d -> p n d", p=128)),
                writes=[kv], key=kv)
            S.op("pool", lambda e, v=v: e.memset(v[:, :, 128:129], 1.0), writes=[kv])
            S.op("dve", lambda e, kt_=kt_: e.tensor_reduce(out=ksum[:], in_=kt_[:].rearrange("p (n k) -> p n k", k=256),
                                                          axis=AX.X, op=ALU.add), reads=[kk], writes=[kksum])
            S.op("dve", lambda e: e.tensor_copy(out=khi[:], in_=ksum[:]), reads=[kksum], writes=[kkhi])
            S.op("dve", lambda e: e.tensor_copy(out=khf[:], in_=khi[:]), reads=[kkhi], writes=[kkhf])
            S.op("dve", lambda e: e.tensor_tensor(out=klo[:], in0=ksum[:], in1=khf[:], op=ALU.subtract),
                 reads=[kksum, kkhf], writes=[kklo])
            for t in range(32):
                S.op("pe", lambda e, t=t, qt=qt: e.matmul(PG[:, t * 16:(t + 1) * 16], lhsT=qt[:, t * 128:(t + 1) * 128],
                                                          rhs=khi[:], start=True, stop=False),
                     reads=[kq, kkhi], writes=[KPG])
                S.op("pe", lambda e, t=t, qt=qt: e.matmul(PG[:, t * 16:(t + 1) * 16], lhsT=qt[:, t * 128:(t + 1) * 128],
                                                          rhs=klo[:], start=False, stop=True),
                     reads=[kq, kklo], writes=[KPG])
            S.op("dve", lambda e: e.tensor_tensor(out=gm[:], in0=PG[:], in1=pastneg[:], op=ALU.add),
                 reads=["pastneg"], writes=[kgm, KPG])
            for t in range(32):
                S.op("dve", lambda e, t=t: e.max(out=top8[:, t, :], in_=gm[:, t * 16:(t + 1) * 16]),
                     reads=[kgm], writes=[ktop])
            for t in range(32):
                S.op("dve", lambda e, t=t: e.tensor_scalar(out=sel[:, t * 16:(t + 1) * 16], in0=gm[:, t * 16:(t + 1) * 16],
                                                           scalar1=top8[:, t, 2:3], scalar2=None, op0=ALU.is_ge),
                     reads=[kgm, ktop], writes=[ksel])
            S.op("dve", lambda e: e.tensor_tensor(out=sel[:], in0=sel[:], in1=past01[:], op=ALU.mult),
                 reads=[ksel, "past01"], writes=[ksel])
            P.append(S.end_capture())
            for n in range(16):
                accs = [acc[(2 * n + q) % 4] for q in range(2)]
                kacc = [("acc", (2 * n + q) % 4) for q in range(2)]
                for j in range(n, -1, -1):
                    S.capture()
                    pts = []
                    for kt in range(2):
                        pS = PS[si % NPS]
                        half = 0
                        kps = KPS[si % NPS]
                        pt = PTs[si % 4]
                        kpt = ("PTs", si % 4)
                        si += 1
                        pts.append((pt, kpt))
                        k0 = j * 256 + kt * 128
                        S.op("pe", lambda e, pS=pS, half=half, k0=k0, n=n, kt_=kt_, qt=qt: e.matmul(
                            pS[:, half * 256:(half + 1) * 256], lhsT=kt_[:, k0:k0 + 128], rhs=qt[:, n * 256:(n + 1) * 256],
                            start=True, stop=True), reads=[kk, kq], writes=[kps])
                        bi = h * 32 + (n - j) * 2 + kt
                        S.op("act", lambda e, pS=pS, half=half, pt=pt, bi=bi: e.activation(
                            out=pt[:], in_=pS[:, half * 256:(half + 1) * 256], func=AF.Exp,
                            bias=abias[:, bi:bi + 1], scale=scale), reads=["abias"], writes=[kpt, kps])
                        if j == n:
                            S.op("pool", lambda e, pt=pt, kt=kt: e.tensor_tensor(out=pt[:], in0=pt[:], in1=cmask[:, kt, :],
                                                                                 op=ALU.mult),
                                 reads=[kpt, "cmask"], writes=[kpt])
                    s1 = S.end_capture()
                    S.capture()
                    pos = []
                    for q in range(2):
                        po = PO[oi % NPO]
                        kpo = KPO[oi % NPO]
                        oi += 1
                        pos.append((po, kpo))
                        for kt in range(2):
                            pt, kpt = pts[kt]
                            S.op("pe", lambda e, po=po, q=q, pt=pt, v=v, j=j, kt=kt: e.matmul(
                                po[:, 0:129], lhsT=pt[:, q * 128:(q + 1) * 128], rhs=v[:, j * 2 + kt, 0:129],
                                start=(kt == 0), stop=(kt == 1)), reads=[kpt, kv], writes=[kpo])
                    for q in range(2):
                        tq = 2 * n + q
                        po, kpo = pos[q]
                        if j == n:
                            S.op("act", lambda e, po=po, q=q, a=accs[q]: e.copy(out=a[:, 0:129], in_=po[:, 0:129]),
                                 writes=[kacc[q], kpo])
                        else:
                            S.op("dve", lambda e, po=po, q=q, a=accs[q], tq=tq, j=j: e.scalar_tensor_tensor(
                                out=a[:, 0:129], in0=po[:, 0:129], scalar=sel[:, tq * 16 + j:tq * 16 + j + 1],
                                in1=a[:, 0:129], op0=ALU.mult, op1=ALU.add), reads=[ksel], writes=[kacc[q], kpo])
                    Useg.append([h, s1, S.end_capture()])
                S.capture()
                for q in range(2):
                    tq = 2 * n + q
                    a = accs[q]
                    o_, os_, ob_ = osb[tq % 2], ost[tq % 2], obf[tq % 2]
                    ko, kos, kob = ("osb", tq % 2), ("ost", tq % 2), ("obf", tq % 2)
                    S.op("dve", lambda e, a=a, os_=os_: e.reciprocal(out=os_[:, 0:1], in_=a[:, 128:129]),
                         reads=[kacc[q]], writes=[kos])
                    S.op("dve", lambda e, a=a, os_=os_, o_=o_: e.tensor_scalar(out=o_[:], in0=a[:, 0:128], scalar1=os_[:, 0:1],
                                                                              scalar2=None, op0=ALU.mult),
                         reads=[kacc[q], kos], writes=[ko])
                    S.op("act", lambda e, o_=o_, os_=os_: e.activation(out=ojk[:], in_=o_[:], func=AF.Square,
                                                                      accum_out=os_[:, 1:2]),
                         reads=[ko], writes=["ojk", kos])
                    S.op("act", lambda e, os_=os_: e.activation(out=os_[:, 2:3], in_=os_[:, 1:2], func=AF.Sqrt,
                                                               bias=epsb[:, 0:1], scale=1.0 / HD),
                         reads=[kos, "epsb"], writes=[kos])
                    S.op("dve", lambda e, os_=os_: e.reciprocal(out=os_[:, 3:4], in_=os_[:, 2:3]), reads=[kos], writes=[kos])
                    S.op("dve", lambda e, o_=o_, os_=os_, ob_=ob_: e.scalar_tensor_tensor(
                        out=ob_[:], in0=o_[:], scalar=os_[:, 3:4], in1=nwb[:], op0=ALU.mult, op1=ALU.mult),
                         reads=[ko, kos, "nwb"], writes=[kob])
                    S.dma("sp", lambda e, ob_=ob_, tq=tq, h=h: e.dma_start(
                        out=io["mix"][tq * 128:(tq + 1) * 128, 512 + h * 128:512 + (h + 1) * 128], in_=ob_[:]),
                        reads=[kob], key=kob)
                Useg[-1][2].extend(S.end_capture())
        for h_ in range(NH):
            head_body(h_)
        order = list(P[0])
        nunits = {hh: sum(1 for u in Useg if u[0] == hh) for hh in range(NH)}
        ppos = {hh: 0 for hh in range(NH)}
        if Useg:
            order.extend(Useg[0][1])
        for idx, (hh, s1_, s2_) in enumerate(Useg):
            if idx + 1 < len(Useg):
                nh = Useg[idx + 1][0]
                if nh != hh:
                    order.extend(P[nh][ppos[nh]:])
                    ppos[nh] = len(P[nh])
                order.extend(Useg[idx + 1][1])
            order.extend(s2_)
            if hh + 1 < NH and ppos[hh + 1] < len(P[hh + 1]):
                step = -(-len(P[hh + 1]) // max(1, nunits[hh] - 8))
                order.extend(P[hh + 1][ppos[hh + 1]:ppos[hh + 1] + step])
                ppos[hh + 1] += step
        if outer_cap is not None:
            S._cap = outer_cap
            outer_cap.extend(order)
        else:
            S.replay(order)
        if ctx is None:
            S.finish()


def phase_gdn(nc, io):
    C = 64
    NCH = T // C
    with contextlib.ExitStack() as st:
        S = Sched(nc, st)
        sb = lambda name, shape, dt: st.enter_context(nc.sbuf_tensor("gd_" + name, shape, dt))
        PB = [st.enter_context(nc.psum_tensor("gd_P%d" % i, [128, 512], F32)) for i in range(8)]
        pbi = [0]

        def bank():
            i = pbi[0] % 8
            pbi[0] += 1
            return PB[i], ("PB", i)

        raw = sb("raw", [128, T + 3], F32)
        cv = [sb("cv%d" % i, [128, T], F32) for i in range(3)]
        tmpf = sb("tmpf", [128, T], F32)
        cw = sb("cw", [128, 12, 4], F32)
        identf = sb("identf", [128, 128], F32)
        ones = sb("ones", [128, 128], F32)
        maskSL = sb("maskSL", [64, 64], F32)
        maskUI = sb("maskUI", [64, 64], F32)
        Utri = sb("Utri", [64, 64], F32)
        gcon = sb("gcon", [64, 8], F32)
        nwb = sb("nwb", [64, 128], F32)
        c1 = sb("c1", [128, 1], F32)
        cq = sb("cq", [128, 1], F32)
        ck = sb("ck", [128, 1], F32)
        ce = sb("ce", [128, 1], F32)
        gba = sb("gba", [64, NCH, 8], F32)
        zt = sb("zt", [64, NCH, 128], F32)
        obuf = sb("obuf", [64, NCH, 128], BF16)
        col = {nm: sb("col_" + nm, [64, NCH], F32) for nm in ("beta", "nbeta", "gl", "g", "eg", "egl", "beg", "tmp", "tmp2")}
        gt = sb("gt", [128, NCH], F32)
        Sst = sb("Sst", [128, 128], F32)
        dg = sb("dg", [64, 64], F32)
        t64 = [sb("t64_%d" % i, [64, 64], F32) for i in range(4)]
        E1 = sb("E1", [64, 64], F32)
        E2 = sb("E2", [64, 64], F32)
        Am = [sb("Am%d" % i, [64, 64], F32) for i in range(2)]
        Bm = [sb("Bm%d" % i, [64, 64], F32) for i in range(2)]
        X = sb("X", [64, 64], F32)
        kbe = sb("kbe", [64, 128], F32)
        kdec = sb("kdec", [64, 128], F32)
        vb = sb("vb", [64, 128], F32)
        wTn = sb("wTn", [128, 64], F32)
        attnT = sb("attnT", [64, 64], F32)
        vnew = sb("vnew", [64, 128], F32)
        oq = sb("oq", [64, 128], F32)
        osb = sb("osb", [64, 128], F32)
        ojk = sb("ojk", [64, 128], F32)
        ost = sb("ost", [64, 4], F32)

        def cdma(dst, src, key):
            S.dma("sp", lambda e: e.dma_start(out=dst, in_=src), writes=[key], key="const")
        cdma(cw[:], io["conv_w"], "cw")
        cdma(identf[:], io["identf"], "identf")
        cdma(maskSL[:], io["maskSL"], "maskSL")
        cdma(maskUI[:], io["maskUI"], "maskUI")
        cdma(Utri[:], io["maskUI"], "Utri")
        cdma(gcon[:], io["gcon"], "gcon")
        cdma(nwb[:], io["nwg"][0:1, :].partition_broadcast(64), "nwb")
        cdma(gba[:], io["gba"].rearrange("(n p) c -> p n c", p=C), "gba")
        S.op("dve", lambda e: e.memset(ones[:], 1.0), writes=["ones"])
        S.op("dve", lambda e: e.memset(c1[:], 1.0), writes=["c1"])
        S.op("dve", lambda e: e.memset(cq[:], 128.0 * 1e-6), writes=["cq"])
        S.op("dve", lambda e: e.memset(ck[:], 1e-6), writes=["ck"])
        S.op("dve", lambda e: e.memset(ce[:], EPS), writes=["ce"])
        S.op("dve", lambda e: e.memset(raw[:, 0:3], 0.0), writes=["rawpad"])
        S.op("act", lambda e: e.activation(out=gcon[:, 0:4], in_=gcon[:, 0:4], func=AF.Exp), reads=["gcon"], writes=["gcon"])

        for h in range(NH):
            for ti, (src, nm) in enumerate(((io["gqT"], "q"), (io["gkT"], "k"), (io["gvT"], "v"))):
                S.dma("sp", lambda e, src=src, h=h: e.dma_start(out=raw[:, 3:], in_=src[h]), reads=["rawpad"], writes=["raw"], key="raw")
                cvt = cv[ti]
                kc = ("cv", ti)
                wi = ti * 4 + h
                S.op("dve", lambda e, cvt=cvt, wi=wi: e.tensor_scalar(out=cvt[:], in0=raw[:, 3:3 + T], scalar1=cw[:, wi, 3:4],
                                                                     scalar2=None, op0=ALU.mult), reads=["raw", "cw"], writes=[kc])
                for j in (2, 1, 0):
                    S.op("dve", lambda e, cvt=cvt, wi=wi, j=j: e.scalar_tensor_tensor(
                        out=cvt[:], in0=raw[:, j:j + T], scalar=cw[:, wi, j:j + 1], in1=cvt[:], op0=ALU.mult, op1=ALU.add),
                         reads=["raw", "cw", kc], writes=[kc])
                S.op("act", lambda e, cvt=cvt: e.activation(out=cvt[:], in_=cvt[:], func=AF.Silu), reads=[kc], writes=[kc])
                if ti < 2:
                    S.op("act", lambda e, cvt=cvt: e.activation(out=tmpf[:], in_=cvt[:], func=AF.Square), reads=[kc], writes=["tmpf"])
                    for g8 in range(T // 512):
                        pb, kb = bank()
                        S.op("pe", lambda e, pb=pb, g8=g8: e.matmul(pb[:, :], lhsT=ones[:, :], rhs=tmpf[:, g8 * 512:(g8 + 1) * 512],
                                                                    start=True, stop=True), reads=["ones", "tmpf"], writes=[kb])
                        if ti == 0:
                            S.op("act", lambda e, pb=pb, g8=g8: e.activation(out=raw[:, 3 + g8 * 512:3 + (g8 + 1) * 512], in_=pb[:, :],
                                                                            func=AF.Sqrt, bias=cq[:, 0:1], scale=128.0),
                                 reads=["cq"], writes=[kb, "raw"])
                        else:
                            S.op("act", lambda e, pb=pb, g8=g8: e.activation(out=raw[:, 3 + g8 * 512:3 + (g8 + 1) * 512], in_=pb[:, :],
                                                                            func=AF.Sqrt, bias=ck[:, 0:1], scale=1.0),
                                 reads=["ck"], writes=[kb, "raw"])
                    S.op("dve", lambda e: e.reciprocal(out=tmpf[:], in_=raw[:, 3:3 + T]), reads=["raw"], writes=["tmpf"])
                    S.op("dve", lambda e, cvt=cvt: e.tensor_tensor(out=cvt[:], in0=cvt[:], in1=tmpf[:], op=ALU.mult),
                         reads=[kc, "tmpf"], writes=[kc])
            qn, kn, vn = cv
            cb, cnb, cgl, cg, ceg, cegl, cbeg, ctmp, ctmp2 = (col[k_] for k_ in ("beta", "nbeta", "gl", "g", "eg", "egl", "beg", "tmp", "tmp2"))
            S.op("act", lambda e, h=h: e.activation(out=cb[:], in_=gba[:, :, h], func=AF.Sigmoid), reads=["gba"], writes=["c_beta"])
            S.op("dve", lambda e: e.tensor_scalar(out=cnb[:], in0=cb[:], scalar1=-1.0, scalar2=None, op0=ALU.mult),
                 reads=["c_beta"], writes=["c_nbeta"])
            S.op("act", lambda e, h=h: e.activation(out=ctmp[:], in_=gba[:, :, 4 + h], func=AF.Exp, bias=gcon[:, 4 + h:5 + h], scale=1.0),
                 reads=["gba", "gcon"], writes=["c_tmp"])
            S.op("act", lambda e: e.activation(out=ctmp[:], in_=ctmp[:], func=AF.Ln, bias=c1[0:64, 0:1], scale=1.0),
                 reads=["c_tmp", "c1"], writes=["c_tmp"])
            S.op("dve", lambda e, h=h: e.tensor_scalar(out=cgl[:], in0=ctmp[:], scalar1=gcon[:, h:h + 1], scalar2=-1.0,
                                                      op0=ALU.mult, op1=ALU.mult), reads=["c_tmp", "gcon"], writes=["c_gl"])
            pb, kb = bank()
            S.op("pe", lambda e, pb=pb: e.matmul(pb[0:64, 0:NCH], lhsT=Utri[:, :], rhs=cgl[:, :], start=True, stop=True),
                 reads=["Utri", "c_gl"], writes=[kb])
            S.op("dve", lambda e, pb=pb: e.tensor_copy(out=cg[:], in_=pb[0:64, 0:NCH]), writes=[kb, "c_g"])
            pb, kb = bank()
            S.op("pe", lambda e, pb=pb: e.matmul(pb[:, 0:NCH], lhsT=ones[0:64, :], rhs=cgl[:, :], start=True, stop=True),
                 reads=["ones", "c_gl"], writes=[kb])
            S.op("act", lambda e, pb=pb: e.activation(out=gt[:], in_=pb[:, 0:NCH], func=AF.Exp), writes=[kb, "gt"])
            S.op("dve", lambda e, pb=pb: e.tensor_tensor(out=ctmp2[:], in0=pb[0:64, 0:NCH], in1=cg[:], op=ALU.subtract),
                 reads=["c_g"], writes=[kb, "c_tmp2"])
            S.op("act", lambda e: e.activation(out=cegl[:], in_=ctmp2[:], func=AF.Exp), reads=["c_tmp2"], writes=["c_egl"])
            S.op("act", lambda e: e.activation(out=ceg[:], in_=cg[:], func=AF.Exp), reads=["c_g"], writes=["c_eg"])
            S.op("dve", lambda e: e.tensor_tensor(out=cbeg[:], in0=cb[:], in1=ceg[:], op=ALU.mult),
                 reads=["c_beta", "c_eg"], writes=["c_beg"])
            S.dma("sp", lambda e, h=h: e.dma_start(out=zt[:], in_=io["gz"][:, h * 128:(h + 1) * 128].rearrange("(n p) d -> p n d", p=C)),
                  writes=["zt"], key="zt")
            S.op("act", lambda e: e.activation(out=zt[:], in_=zt[:], func=AF.Silu), reads=["zt"], writes=["zt"])
            for n8 in range(8):
                S.op("pool", lambda e, n8=n8: e.tensor_tensor(out=zt[:, n8 * 8:(n8 + 1) * 8, :], in0=zt[:, n8 * 8:(n8 + 1) * 8, :],
                                                             in1=nwb[:].unsqueeze(1).to_broadcast([64, 8, 128]), op=ALU.mult),
                     reads=["zt", "nwb"], writes=["zt"])
            S.op("dve", lambda e: e.memset(Sst[:], 0.0), writes=["S"])

            for n in range(NCH):
                c0 = n * C
                kT_c, qT_c, vT_c = kn[:, c0:c0 + C], qn[:, c0:c0 + C], vn[:, c0:c0 + C]
                pk, kpk = bank()
                S.op("pe", lambda e, pk=pk, kT_c=kT_c: e.transpose(out=pk[0:64, 0:128], in_=kT_c, identity=identf[:]),
                     reads=[("cv", 1), "identf"], writes=[kpk])
                S.op("dve", lambda e, pk=pk, n=n: e.tensor_scalar(out=kbe[:], in0=pk[0:64, 0:128], scalar1=cbeg[:, n:n + 1], scalar2=None,
                                                                 op0=ALU.mult), reads=["c_beg"], writes=[kpk, "kbe"])
                S.op("act", lambda e, pk=pk, n=n: e.activation(out=kdec[:], in_=pk[0:64, 0:128], func=AF.Copy, scale=cegl[:, n:n + 1]),
                     reads=["c_egl"], writes=[kpk, "kdec"])
                pv, kpv = bank()
                S.op("pe", lambda e, pv=pv, vT_c=vT_c: e.transpose(out=pv[0:64, 0:128], in_=vT_c, identity=identf[:]),
                     reads=[("cv", 2), "identf"], writes=[kpv])
                S.op("act", lambda e, pv=pv, n=n: e.activation(out=vb[:], in_=pv[0:64, 0:128], func=AF.Copy, scale=cb[:, n:n + 1]),
                     reads=["c_beta"], writes=[kpv, "vb"])
                S.op("dve", lambda e, n=n: e.tensor_scalar(out=dg[:], in0=identf[0:64, 0:64], scalar1=cg[:, n:n + 1], scalar2=None,
                                                          op0=ALU.mult), reads=["identf", "c_g"], writes=["dg"])
                pg, kpg = bank()
                S.op("pe", lambda e, pg=pg: e.matmul(pg[0:64, 0:64], lhsT=ones[0:64, 0:64], rhs=dg[:, :], start=True, stop=True),
                     reads=["ones", "dg"], writes=[kpg])
                S.op("dve", lambda e, pg=pg, n=n: e.tensor_scalar(out=t64[0][:], in0=pg[0:64, 0:64], scalar1=cg[:, n:n + 1], scalar2=0.0,
                                                                 op0=ALU.subtract, op1=ALU.max), reads=["c_g"], writes=[kpg, "t0"])
                S.op("dve", lambda e, pg=pg, n=n: e.tensor_scalar(out=t64[1][:], in0=pg[0:64, 0:64], scalar1=cg[:, n:n + 1], scalar2=0.0,
                                                                 op0=ALU.subtract, op1=ALU.min), reads=["c_g"], writes=[kpg, "t1"])
                S.op("act", lambda e: e.activation(out=E1[:], in_=t64[0][:], func=AF.Exp, scale=-1.0), reads=["t0"], writes=["E1"])
                S.op("act", lambda e: e.activation(out=E2[:], in_=t64[1][:], func=AF.Exp), reads=["t1"], writes=["E2"])
                S.op("pool", lambda e: e.tensor_tensor(out=E2[:], in0=E2[:], in1=maskUI[:], op=ALU.mult),
                     reads=["maskUI"], writes=["E2"])
                pkk, kpkk = bank()
                S.op("pe", lambda e, pkk=pkk, kT_c=kT_c: e.matmul(pkk[0:64, 0:64], lhsT=kT_c, rhs=kT_c, start=True, stop=True),
                     reads=[("cv", 1)], writes=[kpkk])
                S.op("dve", lambda e, pkk=pkk: e.tensor_tensor(out=t64[2][:], in0=pkk[0:64, 0:64], in1=E1[:], op=ALU.mult),
                     reads=["E1"], writes=[kpkk, "t2"])
                A_, B_ = Am[0], Bm[0]
                S.op("dve", lambda e, n=n, A_=A_: e.scalar_tensor_tensor(out=A_[:], in0=t64[2][:], scalar=cnb[:, n:n + 1], in1=maskSL[:],
                                                                        op0=ALU.mult, op1=ALU.mult),
                     reads=["t2", "c_nbeta", "maskSL"], writes=[("A", 0)])
                pt_, kpt_ = bank()
                S.op("pe", lambda e, pt_=pt_, A_=A_: e.transpose(out=pt_[0:64, 0:64], in_=A_[:, :], identity=identf[0:64, 0:64]),
                     reads=[("A", 0), "identf"], writes=[kpt_])
                S.op("act", lambda e, pt_=pt_, B_=B_: e.copy(out=B_[:], in_=pt_[0:64, 0:64]), writes=[kpt_, ("B", 0)])
                S.op("dve", lambda e, pt_=pt_: e.tensor_tensor(out=X[:], in0=pt_[0:64, 0:64], in1=identf[0:64, 0:64], op=ALU.add),
                     reads=["identf"], writes=[kpt_, "X"])
                cur = 0
                for lv in range(5):
                    nxt = 1 - cur
                    pa, kpa = bank()
                    S.op("pe", lambda e, pa=pa, cur=cur: e.matmul(pa[0:64, 0:64], lhsT=Bm[cur][:, :], rhs=Am[cur][:, :], start=True, stop=True),
                         reads=[("A", cur), ("B", cur)], writes=[kpa])
                    if lv < 4:
                        pbb, kpbb = bank()
                        S.op("pe", lambda e, pbb=pbb, cur=cur: e.matmul(pbb[0:64, 0:64], lhsT=Am[cur][:, :], rhs=Bm[cur][:, :],
                                                                        start=True, stop=True),
                             reads=[("A", cur), ("B", cur)], writes=[kpbb])
                    S.op("act", lambda e, pa=pa, nxt=nxt: e.copy(out=Am[nxt][:], in_=pa[0:64, 0:64]), writes=[kpa, ("A", nxt)])
                    if lv < 4:
                        S.op("dve", lambda e, pbb=pbb, nxt=nxt: e.tensor_copy(out=Bm[nxt][:], in_=pbb[0:64, 0:64]), writes=[kpbb, ("B", nxt)])
                    px, kpx = bank()
                    S.op("pe", lambda e, px=px, nxt=nxt: e.matmul(px[0:64, 0:64], lhsT=Am[nxt][:, :], rhs=X[:, :], start=True, stop=True),
                         reads=[("A", nxt), "X"], writes=[kpx])
                    S.op("dve", lambda e, px=px: e.tensor_tensor(out=X[:], in0=px[0:64, 0:64], in1=X[:], op=ALU.add),
                         writes=[kpx, "X"])
                    cur = nxt
                pw, kpw = bank()
                S.op("pe", lambda e, pw=pw: e.matmul(pw[:, 0:64], lhsT=kbe[:, :], rhs=X[:, :], start=True, stop=True),
                     reads=["kbe", "X"], writes=[kpw])
                S.op("act", lambda e, pw=pw: e.mul(out=wTn[:], in_=pw[:, 0:64], mul=-1.0), writes=[kpw, "wTn"])
                pq, kpq = bank()
                S.op("pe", lambda e, pq=pq, kT_c=kT_c, qT_c=qT_c: e.matmul(pq[0:64, 0:64], lhsT=kT_c, rhs=qT_c, start=True, stop=True),
                     reads=[("cv", 0), ("cv", 1)], writes=[kpq])
                S.op("dve", lambda e, pq=pq: e.tensor_tensor(out=attnT[:], in0=pq[0:64, 0:64], in1=E2[:], op=ALU.mult),
                     reads=["E2"], writes=[kpq, "attnT"])
                pvn, kpvn = bank()
                S.op("pe", lambda e, pvn=pvn: e.matmul(pvn[0:64, 0:128], lhsT=X[:, :], rhs=vb[:, :], start=True, stop=False),
                     reads=["X", "vb"], writes=[kpvn])
                S.op("pe", lambda e, pvn=pvn: e.matmul(pvn[0:64, 0:128], lhsT=wTn[:, :], rhs=Sst[:, :], start=False, stop=True),
                     reads=["wTn", "S"], writes=[kpvn])
                S.op("act", lambda e, pvn=pvn: e.copy(out=vnew[:], in_=pvn[0:64, 0:128]), writes=[kpvn, "vnew"])
                po1, kpo1 = bank()
                S.op("pe", lambda e, po1=po1, qT_c=qT_c: e.matmul(po1[0:64, 0:128], lhsT=qT_c, rhs=Sst[:, :], start=True, stop=True),
                     reads=[("cv", 0), "S"], writes=[kpo1])
                S.op("act", lambda e, po1=po1, n=n: e.activation(out=oq[:], in_=po1[0:64, 0:128], func=AF.Copy, scale=ceg[:, n:n + 1]),
                     reads=["c_eg"], writes=[kpo1, "oq"])
                po2, kpo2 = bank()
                S.op("pe", lambda e, po2=po2: e.matmul(po2[0:64, 0:128], lhsT=attnT[:, :], rhs=vnew[:, :], start=True, stop=True),
                     reads=["attnT", "vnew"], writes=[kpo2])
                S.op("dve", lambda e, po2=po2: e.tensor_tensor(out=osb[:], in0=po2[0:64, 0:128], in1=oq[:], op=ALU.add),
                     reads=["oq"], writes=[kpo2, "osb"])
                pS_, kpS = bank()
                S.op("pe", lambda e, pS_=pS_: e.matmul(pS_[:, 0:128], lhsT=kdec[:, :], rhs=vnew[:, :], start=True, stop=True),
                     reads=["kdec", "vnew"], writes=[kpS])
                S.op("dve", lambda e, pS_=pS_, n=n: e.scalar_tensor_tensor(out=Sst[:], in0=Sst[:], scalar=gt[:, n:n + 1], in1=pS_[:, 0:128],
                                                                          op0=ALU.mult, op1=ALU.add),
                     reads=["gt"], writes=[kpS, "S"])
                S.op("act", lambda e: e.activation(out=ojk[:], in_=osb[:], func=AF.Square, accum_out=ost[:, 0:1]),
                     reads=["osb"], writes=["ojk", "ost"])
                S.op("act", lambda e: e.activation(out=ost[:, 1:2], in_=ost[:, 0:1], func=AF.Sqrt, bias=ce[0:64, 0:1], scale=1.0 / HD),
                     reads=["ost", "ce"], writes=["ost"])
                S.op("dve", lambda e: e.reciprocal(out=ost[:, 2:3], in_=ost[:, 1:2]), reads=["ost"], writes=["ost"])
                S.op("dve", lambda e, n=n: e.scalar_tensor_tensor(out=obuf[:, n, :], in0=osb[:], scalar=ost[:, 2:3], in1=zt[:, n, :],
                                                                 op0=ALU.mult, op1=ALU.mult),
                     reads=["osb", "ost", "zt"], writes=["obuf"])
            S.dma("sp", lambda e, h=h: e.dma_start(out=io["mix"][:, h * 128:(h + 1) * 128].rearrange("(n p) d -> p n d", p=C), in_=obuf[:]),
                  reads=["obuf"], key="obuf")
        S.finish()


def phase_gdn2(nc, io, ctx=None):
    C = 64
    NCH = T // C
    G = 8
    NG = NCH // G
    CB = 512
    with contextlib.ExitStack() as st:
        if ctx is not None:
            st = ctx["st"]
        S = ctx["S"] if ctx is not None else Sched(nc, st)
        sb = lambda name, shape, dt: st.enter_context(nc.sbuf_tensor("g2_" + name, shape, dt))
        if ctx is None:
            PBK = [st.enter_context(nc.psum_tensor("g2_P%d" % i, [128, 512], F32)) for i in range(8)]
            LB = [(PBK[i], ("PB", i)) for i in range(4)]
            SB_ = [(PBK[i], ("PB", i)) for i in range(4, 7)]
            PREPB = [(PBK[7], ("PB", 7))]
        else:
            LB, SB_ = ctx["gdn_local_banks"], ctx["gdn_scan_banks"]
            PREPB = LB
        li = [0]
        si = [0]

        def lbank():
            i = li[0] % len(LB)
            li[0] += 1
            return LB[i]

        pi_ = [0]

        def pbank():
            i = pi_[0] % len(PREPB)
            pi_[0] += 1
            return PREPB[i]

        def sbank():
            i = si[0] % len(SB_)
            si[0] += 1
            return SB_[i]

        cw = sb("cw", [128, 12, 4], F32)
        identf = sb("identf", [128, 128], F32)
        ones = sb("ones", [128, 128], F32)
        maskSL = sb("maskSL", [64, 64], F32)
        maskUI = sb("maskUI", [64, 64], F32)
        gcon = sb("gcon", [64, 8], F32)
        nwb = sb("nwb", [64, 128], F32)
        c1 = sb("c1", [128, 1], F32)
        cq = sb("cq", [128, 1], F32)
        ck = sb("ck", [128, 1], F32)
        ce = sb("ce", [128, 1], F32)
        gba = sb("gba", [64, NCH, 8], F32)
        rawb = [sb("rawb%d" % i, [128, CB + 3], F32) for i in range(2)]
        tmpb = [sb("tmpb%d" % i, [128, CB], F32) for i in range(2)]
        cv = [[sb("cv%d_%d" % (hd, i), [128, T], BF16) for i in range(3)] for hd in range(NH)]
        dstb = [sb("dstb%d" % i, [128, CB], F32) for i in range(2)]
        identb = sb("identb", [128, 128], BF16)
        Sbf = [sb("Sbf%d" % hd, [128, 128], BF16) for hd in range(NH)]
        cnames = ("beta", "nbeta", "gl", "g", "eg", "egl", "beg", "tmp", "tmp2")
        col = [{nm: sb("col%d_%s" % (hd, nm), [64, NCH], F32) for nm in cnames} for hd in range(NH)]
        gt = [sb("gt%d" % hd, [128, NCH], F32) for hd in range(NH)]
        Sst = [sb("Sst%d" % hd, [128, 128], F32) for hd in range(NH)]
        X = [[sb("X%d_%d" % (hd, b), [64, G, 64], BF16) for b in range(2)] for hd in range(2)]
        vb = [[sb("vb%d_%d" % (hd, b), [64, G, 128], BF16) for b in range(2)] for hd in range(2)]
        kdec = [[sb("kdec%d_%d" % (hd, b), [64, G, 128], BF16) for b in range(2)] for hd in range(2)]
        wTn = [[sb("wTn%d_%d" % (hd, b), [128, G, 64], BF16) for b in range(2)] for hd in range(2)]
        attnT = [[sb("attnT%d_%d" % (hd, b), [64, G, 64], BF16) for b in range(2)] for hd in range(2)]
        zt = [[sb("zt%d_%d" % (hd, b), [64, G, 128], BF16) for b in range(2)] for hd in range(2)]
        kbe = [sb("kbe%d" % hd, [64, G, 128], BF16) for hd in range(2)]
        dg = [sb("dg%d" % hd, [64, G, 64], F32) for hd in range(2)]
        dd = [sb("dd%d" % hd, [64, G, 64], F32) for hd in range(2)]
        E1 = [sb("E1_%d" % hd, [64, G, 64], F32) for hd in range(2)]
        E2 = [sb("E2_%d" % hd, [64, G, 64], F32) for hd in range(2)]
        Am = [[sb("Am%d_%d" % (hd, b), [64, G, 64], BF16) for b in range(2)] for hd in range(2)]
        Bm = [[sb("Bm%d_%d" % (hd, b), [64, G, 64], BF16) for b in range(2)] for hd in range(2)]
        osb = [sb("osb%d" % hd, [64, G, 128], F32) for hd in range(2)]
        ojk = [sb("ojk%d" % hd, [64, G, 128], BF16) for hd in range(2)]
        obuf = [sb("obuf%d" % hd, [64, G, 128], BF16) for hd in range(2)]
        ost = [sb("ost%d" % hd, [64, 4 * G], F32) for hd in range(2)]
        vnew = [sb("vnew%d" % hd, [64, 128], BF16) for hd in range(2)]
        oq = [sb("oq%d" % hd, [64, 128], F32) for hd in range(2)]

        def cdma(dst, src, key):
            S.dma("sp", lambda e: e.dma_start(out=dst, in_=src), writes=[key], key="c_" + key)
        cdma(cw[:], io["conv_w"], "cw")
        cdma(identf[:], io["identf"], "identf")
        cdma(identb[:], io["ident"], "identb")
        cdma(maskSL[:], io["maskSL"], "maskSL")
        cdma(maskUI[:], io["maskUI"], "maskUI")
        cdma(gcon[:], io["gcon"], "gcon")
        cdma(nwb[:], io["nwg"][0:1, :].partition_broadcast(64), "nwb")
        cdma(gba[:], io["gba"].rearrange("(n p) c -> p n c", p=C), "gba")
        S.op("dve", lambda e: e.memset(ones[:], 1.0), writes=["ones"])
        S.op("dve", lambda e: e.memset(c1[:], 1.0), writes=["c1"])
        S.op("dve", lambda e: e.memset(cq[:], 128.0 * 1e-6), writes=["cq"])
        S.op("dve", lambda e: e.memset(ck[:], 1e-6), writes=["ck"])
        S.op("dve", lambda e: e.memset(ce[:], EPS), writes=["ce"])
        S.op("act", lambda e: e.activation(out=gcon[:, 0:4], in_=gcon[:, 0:4], func=AF.Exp), reads=["gcon"], writes=["gcon"])
        ident64 = identf[0:64, 0:64]
        ident64b = identb[0:64, 0:64]
        F32R = mybir.dt.float32r

        class _PE:
            def __init__(self, e):
                self.e = e

            def matmul(self, out, lhsT, rhs, start, stop):
                return getattr(self.e, 'matmul')(out, lhsT=lhsT, rhs=rhs, start=start, stop=stop)
        bi = [0]

        def prep(h, hd):
            for ti, src in enumerate((io["gqT"], io["gkT"], io["gvT"])):
                cvt = cv[h][ti]
                kc = ("cv", h, ti)
                wi = ti * 4 + h
                for blk in range(T // CB):
                    b = bi[0] % 2
                    bi[0] += 1
                    rb, tb = rawb[b], tmpb[b]
                    krb, ktb = ("rawb", b), ("tmpb", b)
                    c0 = blk * CB
                    if blk == 0:
                        S.op("pool", lambda e, rb=rb: e.memset(rb[:, 0:3], 0.0), writes=[krb])
                        S.dma("sp", lambda e, rb=rb, src=src, h=h: e.dma_start(out=rb[:, 3:], in_=src[h][:, 0:CB]), writes=[krb], key=krb)
                    else:
                        S.dma("sp", lambda e, rb=rb, src=src, h=h, c0=c0: e.dma_start(out=rb[:, :], in_=src[h][:, c0 - 3:c0 + CB]), writes=[krb], key=krb)
                    dst = dstb[b][:, :]
                    kd_ = ("dstb", b)
                    fin = cvt[:, c0:c0 + CB]
                    S.op("dve", lambda e, rb=rb, dst=dst, wi=wi: e.tensor_scalar(out=dst, in0=rb[:, 3:3 + CB], scalar1=cw[:, wi, 3:4],
                                                                                scalar2=None, op0=ALU.mult), reads=[krb, "cw"], writes=[kd_])
                    for j in (2, 1, 0):
                        S.op("dve", lambda e, rb=rb, dst=dst, wi=wi, j=j: e.scalar_tensor_tensor(
                            out=dst, in0=rb[:, j:j + CB], scalar=cw[:, wi, j:j + 1], in1=dst, op0=ALU.mult, op1=ALU.add),
                             reads=[krb, "cw"], writes=[kd_])
                    if ti == 2:
                        S.op("act", lambda e, dst=dst, fin=fin: e.activation(out=fin, in_=dst, func=AF.Silu), reads=[kd_], writes=[kc])
                    else:
                        S.op("act", lambda e, dst=dst: e.activation(out=dst, in_=dst, func=AF.Silu), writes=[kd_])
                    if ti < 2:
                        S.op("act", lambda e, dst=dst, tb=tb: e.activation(out=tb[:], in_=dst, func=AF.Square), reads=[kd_], writes=[ktb])
                        for g8 in range(CB // 512):
                            pb, kb = pbank()
                            S.op("pe", lambda e, pb=pb, g8=g8, tb=tb: _PE(e).matmul(pb[:, :], lhsT=ones[:, :], rhs=tb[:, g8 * 512:(g8 + 1) * 512],
                                                                              start=True, stop=True), reads=["ones", ktb], writes=[kb])
                            S.op("act", lambda e, pb=pb, g8=g8, rb=rb, ti=ti: e.activation(
                                out=rb[:, g8 * 512:(g8 + 1) * 512], in_=pb[:, :], func=AF.Ln,
                                bias=(cq if ti == 0 else ck)[:, 0:1], scale=(128.0 if ti == 0 else 1.0)),
                                 reads=["cq", "ck"], writes=[kb, krb])
                        S.op("act", lambda e, tb=tb, rb=rb: e.activation(out=tb[:], in_=rb[:, 0:CB], func=AF.Exp, scale=-0.5), reads=[krb], writes=[ktb])
                        S.op("pool", lambda e, dst=dst, tb=tb, fin=fin: e.tensor_tensor(out=fin, in0=dst, in1=tb[:], op=ALU.mult), reads=[ktb, kd_], writes=[kc])
            cl = col[h]
            cb_, cnb, cgl, cg, ceg, cegl, cbeg, ctmp, ctmp2 = (cl[k_] for k_ in cnames)
            kcol = ("col", h)
            S.op("act", lambda e: e.activation(out=cb_[:], in_=gba[:, :, h], func=AF.Sigmoid), reads=["gba"], writes=[kcol])
            S.op("dve", lambda e: e.tensor_scalar(out=cnb[:], in0=cb_[:], scalar1=-1.0, scalar2=None, op0=ALU.mult), writes=[kcol])
            S.op("act", lambda e: e.activation(out=ctmp[:], in_=gba[:, :, 4 + h], func=AF.Exp, bias=gcon[:, 4 + h:5 + h], scale=1.0),
                 reads=["gba", "gcon"], writes=[kcol])
            S.op("act", lambda e: e.activation(out=ctmp[:], in_=ctmp[:], func=AF.Ln, bias=c1[0:64, 0:1], scale=1.0), reads=["c1"], writes=[kcol])
            S.op("dve", lambda e: e.tensor_scalar(out=cgl[:], in0=ctmp[:], scalar1=gcon[:, h:h + 1], scalar2=-1.0, op0=ALU.mult, op1=ALU.mult),
                 reads=["gcon"], writes=[kcol])
            pb, kb = pbank()
            S.op("pe", lambda e, pb=pb: _PE(e).matmul(pb[0:64, 0:NCH], lhsT=maskUI[:, :], rhs=cgl[:, :], start=True, stop=True),
                 reads=["maskUI", kcol], writes=[kb])
            S.op("dve", lambda e, pb=pb: e.tensor_copy(out=cg[:], in_=pb[0:64, 0:NCH]), writes=[kb, kcol])
            pb2, kb2 = pbank()
            S.op("pe", lambda e, pb2=pb2: _PE(e).matmul(pb2[:, 0:NCH], lhsT=ones[0:64, :], rhs=cgl[:, :], start=True, stop=True),
                 reads=["ones", kcol], writes=[kb2])
            S.op("act", lambda e, pb2=pb2: e.activation(out=gt[h][:], in_=pb2[:, 0:NCH], func=AF.Exp), writes=[kb2, ("gt", h)])
            S.op("dve", lambda e, pb2=pb2: e.tensor_tensor(out=ctmp2[:], in0=pb2[0:64, 0:NCH], in1=cg[:], op=ALU.subtract), writes=[kb2, kcol])
            S.op("act", lambda e: e.activation(out=cegl[:], in_=ctmp2[:], func=AF.Exp), writes=[kcol])
            S.op("act", lambda e: e.activation(out=ceg[:], in_=cg[:], func=AF.Exp), writes=[kcol])
            S.op("dve", lambda e: e.tensor_tensor(out=cbeg[:], in0=cb_[:], in1=ceg[:], op=ALU.mult), writes=[kcol])
            S.op("dve", lambda e: e.memset(Sst[h][:], 0.0), writes=[("S", h)])
            S.op("pool", lambda e: e.memset(Sbf[h][:], 0.0), writes=[("Sb", h)])

        def bc2(ap2, n):
            return ap2.unsqueeze(2).to_broadcast([64, G, n])

        def bc1(ap2, n=64):
            return ap2.unsqueeze(1).to_broadcast([64, G, n])

        def v3(bank_ap, n):
            return bank_ap.rearrange("p (g i) -> p g i", g=G)

        def local_steps(h, hd, g):
            b = g % 2
            n0 = g * G
            qn, kn, vn = cv[h]
            cl = col[h]
            kcol = ("col", h)
            kX, kvb, kkd, kw, kat, kz = (("X", hd, b), ("vb", hd, b), ("kdec", hd, b), ("wTn", hd, b), ("attnT", hd, b), ("zt", hd, b))
            steps = []

            def s_kv():
                pk, kpk = lbank()
                for gi in range(G):
                    c0 = (n0 + gi) * C
                    S.op("pe", lambda e, pk=pk, gi=gi, c0=c0: e.transpose(out=pk[:].bitcast(BF16)[0:64, gi * 128:(gi + 1) * 128], in_=kn[:, c0:c0 + C], identity=identb[:]),
                         reads=[("cv", h, 1), "identb"], writes=[kpk])
                S.op("dve", lambda e, pk=pk: e.tensor_tensor(out=kbe[hd][:], in0=v3(pk[:].bitcast(BF16)[0:64, 0:G * 128], 128), in1=bc2(cl["beg"][:, n0:n0 + G], 128), op=ALU.mult),
                     reads=[kcol], writes=[kpk, ("kbe", hd)])
                S.op("dve", lambda e, pk=pk: e.tensor_tensor(out=kdec[hd][b][:], in0=v3(pk[:].bitcast(BF16)[0:64, 0:G * 128], 128), in1=bc2(cl["egl"][:, n0:n0 + G], 128), op=ALU.mult),
                     reads=[kcol], writes=[kpk, kkd])
                pv, kpv = lbank()
                for gi in range(G):
                    c0 = (n0 + gi) * C
                    S.op("pe", lambda e, pv=pv, gi=gi, c0=c0: e.transpose(out=pv[:].bitcast(BF16)[0:64, gi * 128:(gi + 1) * 128], in_=vn[:, c0:c0 + C], identity=identb[:]),
                         reads=[("cv", h, 2), "identb"], writes=[kpv])
                S.op("dve", lambda e, pv=pv: e.tensor_tensor(out=vb[hd][b][:], in0=v3(pv[:].bitcast(BF16)[0:64, 0:G * 128], 128), in1=bc2(cl["beta"][:, n0:n0 + G], 128), op=ALU.mult),
                     reads=[kcol], writes=[kpv, kvb])
            steps.append(s_kv)

            def s_decay():
                S.op("dve", lambda e: e.tensor_tensor(out=dg[hd][:], in0=bc1(ident64), in1=bc2(cl["g"][:, n0:n0 + G], 64), op=ALU.mult),
                     reads=["identf", kcol], writes=[("dg", hd)])
                pg, kpg = lbank()
                S.op("pe", lambda e, pg=pg: _PE(e).matmul(pg[0:64, 0:G * 64], lhsT=ones[0:64, 0:64], rhs=dg[hd][:].rearrange("p g i -> p (g i)"),
                                                     start=True, stop=True), reads=["ones", ("dg", hd)], writes=[kpg])
                S.op("dve", lambda e, pg=pg: e.tensor_tensor(out=dd[hd][:], in0=v3(pg[0:64, 0:G * 64], 64), in1=bc2(cl["g"][:, n0:n0 + G], 64), op=ALU.subtract),
                     reads=[kcol], writes=[kpg, ("dd", hd)])
                S.op("dve", lambda e: e.tensor_scalar(out=E1[hd][:], in0=dd[hd][:], scalar1=0.0, scalar2=None, op0=ALU.max),
                     reads=[("dd", hd)], writes=[("E1", hd)])
                S.op("act", lambda e: e.activation(out=E1[hd][:], in_=E1[hd][:], func=AF.Exp, scale=-1.0), writes=[("E1", hd)])
                S.op("dve", lambda e: e.tensor_tensor(out=E1[hd][:], in0=E1[hd][:], in1=bc1(maskSL[:]), op=ALU.mult), reads=["maskSL"], writes=[("E1", hd)])
                S.op("dve", lambda e: e.tensor_tensor(out=E1[hd][:], in0=E1[hd][:], in1=bc2(cl["nbeta"][:, n0:n0 + G], 64), op=ALU.mult),
                     reads=[kcol], writes=[("E1", hd)])
                S.op("dve", lambda e: e.tensor_scalar(out=E2[hd][:], in0=dd[hd][:], scalar1=0.0, scalar2=None, op0=ALU.min),
                     reads=[("dd", hd)], writes=[("E2", hd)])
                S.op("act", lambda e: e.activation(out=E2[hd][:], in_=E2[hd][:], func=AF.Exp), writes=[("E2", hd)])
                S.op("pool", lambda e: e.tensor_tensor(out=E2[hd][:], in0=E2[hd][:], in1=bc1(maskUI[:]), op=ALU.mult), reads=["maskUI"], writes=[("E2", hd)])
            steps.append(s_decay)

            def s_A():
                pkk, kpkk = lbank()
                for gi in range(G):
                    c0 = (n0 + gi) * C
                    S.op("pe", lambda e, pkk=pkk, gi=gi, c0=c0: _PE(e).matmul(pkk[0:64, gi * 64:(gi + 1) * 64], lhsT=kn[:, c0:c0 + C], rhs=kn[:, c0:c0 + C],
                                                                         start=True, stop=True), reads=[("cv", h, 1)], writes=[kpkk])
                S.op("dve", lambda e, pkk=pkk: e.tensor_tensor(out=Am[hd][0][:], in0=v3(pkk[0:64, 0:G * 64], 64), in1=E1[hd][:], op=ALU.mult),
                     reads=[("E1", hd)], writes=[kpkk, ("A", hd, 0)])
                pt_, kpt_ = lbank()
                for gi in range(G):
                    S.op("pe", lambda e, pt_=pt_, gi=gi: e.transpose(out=pt_[:].bitcast(BF16)[0:64, gi * 64:(gi + 1) * 64], in_=Am[hd][0][:, gi, :], identity=ident64b),
                         reads=[("A", hd, 0), "identb"], writes=[kpt_])
                S.op("act", lambda e, pt_=pt_: e.copy(out=Bm[hd][0][:], in_=v3(pt_[:].bitcast(BF16)[0:64, 0:G * 64], 64)), writes=[kpt_, ("B", hd, 0)])
                S.op("dve", lambda e, pt_=pt_: e.tensor_tensor(out=X[hd][b][:], in0=v3(pt_[:].bitcast(BF16)[0:64, 0:G * 64], 64), in1=bc1(ident64), op=ALU.add),
                     reads=["identf"], writes=[kpt_, kX])
            steps.append(s_A)

            def mk_level(lv):
                def s_lv():
                    cur = lv % 2
                    nxt = 1 - cur
                    pa, kpa = lbank()
                    for gi in range(G):
                        S.op("pe", lambda e, pa=pa, gi=gi: _PE(e).matmul(pa[0:64, gi * 64:(gi + 1) * 64], lhsT=Bm[hd][cur][:, gi, :], rhs=Am[hd][cur][:, gi, :],
                                                                    start=True, stop=True), reads=[("A", hd, cur), ("B", hd, cur)], writes=[kpa])
                    if lv < 4:
                        pbb, kpbb = lbank()
                        for gi in range(G):
                            S.op("pe", lambda e, pbb=pbb, gi=gi: _PE(e).matmul(pbb[0:64, gi * 64:(gi + 1) * 64], lhsT=Am[hd][cur][:, gi, :], rhs=Bm[hd][cur][:, gi, :],
                                                                          start=True, stop=True), reads=[("A", hd, cur), ("B", hd, cur)], writes=[kpbb])
                    S.op("act", lambda e, pa=pa: e.copy(out=Am[hd][nxt][:], in_=v3(pa[0:64, 0:G * 64], 64)), writes=[kpa, ("A", hd, nxt)])
                    if lv < 4:
                        S.op("dve", lambda e, pbb=pbb: e.tensor_copy(out=Bm[hd][nxt][:], in_=v3(pbb[0:64, 0:G * 64], 64)), writes=[kpbb, ("B", hd, nxt)])
                    px, kpx = lbank()
                    for gi in range(G):
                        S.op("pe", lambda e, px=px, gi=gi: _PE(e).matmul(px[0:64, gi * 64:(gi + 1) * 64], lhsT=Am[hd][nxt][:, gi, :], rhs=X[hd][b][:, gi, :],
                                                                    start=True, stop=True), reads=[("A", hd, nxt), kX], writes=[kpx])
                    S.op("dve", lambda e, px=px: e.tensor_tensor(out=X[hd][b][:], in0=v3(px[0:64, 0:G * 64], 64), in1=X[hd][b][:], op=ALU.add),
                         writes=[kpx, kX])
                return s_lv
            for lv in range(5):
                steps.append(mk_level(lv))

            def s_w():
                pw, kpw = lbank()
                for gi in range(G):
                    S.op("pe", lambda e, pw=pw, gi=gi: _PE(e).matmul(pw[:, gi * 64:(gi + 1) * 64], lhsT=kbe[hd][:, gi, :], rhs=X[hd][b][:, gi, :],
                                                                start=True, stop=True), reads=[("kbe", hd), kX], writes=[kpw])
                S.op("act", lambda e, pw=pw: e.mul(out=wTn[hd][b][:], in_=pw[:, 0:G * 64].rearrange("p (g i) -> p g i", g=G), mul=-1.0),
                     writes=[kpw, kw])
                pq, kpq = lbank()
                for gi in range(G):
                    c0 = (n0 + gi) * C
                    S.op("pe", lambda e, pq=pq, gi=gi, c0=c0: _PE(e).matmul(pq[0:64, gi * 64:(gi + 1) * 64], lhsT=kn[:, c0:c0 + C], rhs=qn[:, c0:c0 + C],
                                                                       start=True, stop=True), reads=[("cv", h, 0), ("cv", h, 1)], writes=[kpq])
                S.op("dve", lambda e, pq=pq: e.tensor_tensor(out=attnT[hd][b][:], in0=v3(pq[0:64, 0:G * 64], 64), in1=E2[hd][:], op=ALU.mult),
                     reads=[("E2", hd)], writes=[kpq, kat])
                S.dma("pool", lambda e: e.dma_start(out=zt[hd][b][:], in_=io["gz"][n0 * C:(n0 + G) * C, h * 128:(h + 1) * 128].rearrange("(g p) d -> p g d", p=C)),
                      writes=[kz], key=kz)
                S.op("act", lambda e: e.activation(out=zt[hd][b][:], in_=zt[hd][b][:], func=AF.Silu), writes=[kz])
                S.op("pool", lambda e: e.tensor_tensor(out=zt[hd][b][:], in0=zt[hd][b][:], in1=bc1(nwb[:], 128), op=ALU.mult), reads=["nwb"], writes=[kz])
            steps.append(s_w)
            return steps

        def scan_steps(h, hd, g):
            b = g % 2
            n0 = g * G
            qn = cv[h][0]
            cl = col[h]
            kcol = ("col", h)
            kX, kvb, kkd, kw, kat, kz = (("X", hd, b), ("vb", hd, b), ("kdec", hd, b), ("wTn", hd, b), ("attnT", hd, b), ("zt", hd, b))
            steps = []
            for gi in range(G):
                n = n0 + gi
                c0 = n * C

                def s_v(gi=gi, n=n, c0=c0):
                    pvn, kpvn = sbank()
                    S.op("pe", lambda e, pvn=pvn: _PE(e).matmul(pvn[0:64, 0:128], lhsT=X[hd][b][:, gi, :], rhs=vb[hd][b][:, gi, :], start=True, stop=False),
                         reads=[kX, kvb], writes=[kpvn])
                    S.op("pe", lambda e, pvn=pvn: _PE(e).matmul(pvn[0:64, 0:128], lhsT=wTn[hd][b][:, gi, :], rhs=Sbf[h][:, :], start=False, stop=True),
                         reads=[kw, ("Sb", h)], writes=[kpvn])
                    S.op("act", lambda e, pvn=pvn: e.copy(out=vnew[hd][:], in_=pvn[0:64, 0:128]), writes=[kpvn, ("vnew", hd)])
                    po1, kpo1 = sbank()
                    S.op("pe", lambda e, po1=po1: _PE(e).matmul(po1[0:64, 0:128], lhsT=qn[:, c0:c0 + C], rhs=Sbf[h][:, :], start=True, stop=True),
                         reads=[("cv", h, 0), ("Sb", h)], writes=[kpo1])
                    S.op("act", lambda e, po1=po1: e.activation(out=oq[hd][:], in_=po1[0:64, 0:128], func=AF.Copy, scale=cl["eg"][:, n:n + 1]),
                         reads=[kcol], writes=[kpo1, ("oq", hd)])
                steps.append(s_v)

                def s_o(gi=gi, n=n):
                    po2, kpo2 = sbank()
                    S.op("pe", lambda e, po2=po2: _PE(e).matmul(po2[0:64, 0:128], lhsT=attnT[hd][b][:, gi, :], rhs=vnew[hd][:, :], start=True, stop=True),
                         reads=[kat, ("vnew", hd)], writes=[kpo2])
                    S.op("dve", lambda e, po2=po2: e.tensor_tensor(out=osb[hd][:, gi, :], in0=po2[0:64, 0:128], in1=oq[hd][:], op=ALU.add),
                         reads=[("oq", hd)], writes=[kpo2, ("osb", hd)])
                    pS_, kpS = sbank()
                    S.op("pe", lambda e, pS_=pS_: _PE(e).matmul(pS_[:, 0:128], lhsT=kdec[hd][b][:, gi, :], rhs=vnew[hd][:, :], start=True, stop=True),
                         reads=[kkd, ("vnew", hd)], writes=[kpS])
                    S.op("dve", lambda e, pS_=pS_: e.scalar_tensor_tensor(out=Sst[h][:], in0=Sst[h][:], scalar=gt[h][:, n:n + 1], in1=pS_[:, 0:128],
                                                                         op0=ALU.mult, op1=ALU.add),
                         reads=[("gt", h)], writes=[kpS, ("S", h)])
                    S.op("act", lambda e: e.copy(out=Sbf[h][:], in_=Sst[h][:]), reads=[("S", h)], writes=[("Sb", h)])
                steps.append(s_o)

            def s_norm():
                o_ = ost[hd]
                ko = ("ost", hd)
                S.op("act", lambda e: e.activation(out=ojk[hd][:], in_=osb[hd][:], func=AF.Square), reads=[("osb", hd)], writes=[("ojk", hd)])
                S.op("dve", lambda e: e.tensor_reduce(out=o_[:, 0:G], in_=ojk[hd][:], axis=AX.X, op=ALU.add), reads=[("ojk", hd)], writes=[ko])
                S.op("act", lambda e: e.activation(out=o_[:, G:2 * G], in_=o_[:, 0:G], func=AF.Sqrt, bias=ce[0:64, 0:1], scale=1.0 / HD),
                     reads=["ce"], writes=[ko])
                S.op("dve", lambda e: e.reciprocal(out=o_[:, 2 * G:3 * G], in_=o_[:, G:2 * G]), writes=[ko])
                S.op("dve", lambda e: e.tensor_tensor(out=osb[hd][:], in0=osb[hd][:], in1=bc2(o_[:, 2 * G:3 * G], 128), op=ALU.mult),
                     reads=[ko], writes=[("osb", hd)])
                S.op("pool", lambda e: e.tensor_tensor(out=obuf[hd][:], in0=osb[hd][:], in1=zt[hd][b][:], op=ALU.mult),
                     reads=[kz], writes=[("osb", hd), ("obuf", hd)])
                S.dma("sp", lambda e: e.dma_start(out=io["mix"][n0 * C:(n0 + G) * C, h * 128:(h + 1) * 128].rearrange("(g p) d -> p g d", p=C), in_=obuf[hd][:]),
                      reads=[("obuf", hd)], key=("obuf", hd))
            steps.append(s_norm)
            return steps

        def lock(step_lists):
            n = max(len(x) for x in step_lists)
            for i in range(n):
                for x in step_lists:
                    if i < len(x):
                        x[i]()

        outer_cap = S._cap
        S._cap = None
        PP, LP = [], []
        for pair in range(NH // 2):
            hs = (2 * pair, 2 * pair + 1)
            S.capture()
            for hd, h in enumerate(hs):
                prep(h, hd)
            PP.append(S.end_capture())
            S.capture()
            lock([local_steps(h, hd, 0) for hd, h in enumerate(hs)])
            for g in range(NG):
                loc = [local_steps(h, hd, g + 1) for hd, h in enumerate(hs)] if g + 1 < NG else []
                scn = [scan_steps(h, hd, g) for hd, h in enumerate(hs)]
                lock(loc + scn)
            LP.append(S.end_capture())
        order = list(PP[0])
        for pair in range(NH // 2):
            order.extend(merge_prop(LP[pair], PP[pair + 1]) if pair + 1 < len(PP) else LP[pair])
        if outer_cap is not None:
            S._cap = outer_cap
            outer_cap.extend(order)
        else:
            S.replay(order)
        if ctx is None:
            S.finish()


def phase_mixers(nc, io):
    with contextlib.ExitStack() as st:
        S = Sched(nc, st)
        banks = [(st.enter_context(nc.psum_tensor("mx_P%d" % i, [128, 512], F32)), ("PB", i)) for i in range(8)]
        ctx = {"S": S, "st": st, "gdn_local_banks": banks[0:3], "gdn_scan_banks": banks[3:5], "moba_banks": banks[5:8]}
        S.capture()
        phase_moba(nc, io, ctx)
        M = S.end_capture()
        S.capture()
        phase_gdn2(nc, io, ctx)
        Gd = S.end_capture()
        i = j = 0
        merged = []
        while i < len(Gd) or j < len(M):
            if j >= len(M) or (i < len(Gd) and i * len(M) <= j * len(Gd)):
                merged.append(Gd[i])
                i += 1
            else:
                merged.append(M[j])
                j += 1
        S.replay(merged)
        S.finish()


TB = 2048
NE = 32
CAP = 256
DUMMY = NE * CAP


def phase_b1(nc, io):
    with contextlib.ExitStack() as st:
        S = Sched(nc, st)
        sb = lambda name, shape, dt: st.enter_context(nc.sbuf_tensor("b1_" + name, shape, dt))
        ps = lambda name, shape, dt: st.enter_context(nc.psum_tensor("b1_" + name, shape, dt))
        Wo, wsem = io["Wo_pre"]
        S.last_w["Wo"] = (wsem, 16 * 16)
        Wr = sb("Wr", [128, 16, 36], BF16)
        ident = sb("ident", [128, 128], BF16)
        nwb = sb("nwb", [128, D], F32)
        brb = sb("brb", [128, 36], F32)
        epsb = sb("epsb", [128, 1], F32)
        mt = [sb("mt%d" % i, [128, D], BF16) for i in range(4)]
        mT = [sb("mT%d" % i, [128, 16, 128], BF16) for i in range(4)]
        xt = [sb("xt%d" % i, [128, D], F32) for i in range(4)]
        h2 = [sb("h2_%d" % i, [128, D], BF16) for i in range(4)]
        h2T = [sb("h2T%d" % i, [128, 16, 128], BF16) for i in range(4)]
        junk = sb("junk", [128, D], BF16)
        stt = [sb("stt%d" % i, [128, 16], F32) for i in range(4)]
        NT_ = TB // 128
        LG = sb("LG", [128, NT_, 36], F32)
        mg = sb("mg", [128, NT_], F32)
        zz = sb("zz", [128, NT_], F32)
        ptg = sb("ptg", [128, NT_], F32)
        rr = sb("rr", [128, NT_], F32)
        den = sb("den", [128, NT_], F32)
        eg4 = sb("eg4", [128, NT_, 4], F32)
        og4 = sb("og4", [128, NT_, 4], F32)
        LE = sb("LE", [128, NT_, 32], F32)
        T8 = sb("T8", [128, NT_, 8], F32)
        M1 = sb("M1", [128, NT_, 32], F32)
        M2 = sb("M2", [128, NT_, 32], F32)
        M01 = sb("M01", [128, NT_, 32], F32)
        SL = sb("SL", [128, NT_, 32], F32)
        VL = sb("VL", [128, NT_, 32], F32)
        GS = sb("GS", [128, NT_, 2], F32)
        DS = sb("DS", [128, NT_, 2], F32)
        DI = sb("DI", [128, NT_, 2], I32)
        cnt = sb("cnt", [128, 32], F32)
        ebase = sb("ebase", [128, 32], F32)
        UTs = sb("UTs", [128, 128], F32)
        onesf = sb("onesf", [128, 128], F32)
        PT = ps("PT", [128, 2048], BF16)
        PM = [ps("PM%d" % i, [128, 512], F32) for i in range(2)]
        PT2 = ps("PT2", [128, 2048], BF16)
        PR = ps("PR", [128, 512], F32)
        PP = ps("PP", [128, 512], F32)
        S.op("dve", lambda e: e.memset(onesf[:], 1.0), writes=["onesf"])
        S.dma("sp", lambda e: e.dma_start(out=ebase[:], in_=io["ebase"]), writes=["ebase"], key="c_eb")
        S.dma("sp", lambda e: e.dma_start(out=UTs[:], in_=io["uts"]), writes=["UTs"], key="c_uts")

        S.dma("sp", lambda e: e.dma_start(out=ident[:], in_=io["ident"]), writes=["ident"], key="c_ident")
        S.dma("sp", lambda e: e.dma_start(out=nwb[:], in_=io["nw2"][0:1, :].partition_broadcast(128)), writes=["nwb"], key="c_nwb")
        S.dma("sp", lambda e: e.dma_start(out=brb[:], in_=io["br"][0:1, :].partition_broadcast(128)), writes=["brb"], key="c_brb")
        ridx = sb("ridx", [128, 32], I32)
        S.dma("sp", lambda e: e.dma_start(out=ridx[:], in_=io["rowidx"]), writes=["ridx"], key="c_ridx")
        S.op("dve", lambda e: e.memset(epsb[:], EPS), writes=["epsb"])
        S.dma("pool", lambda e: e.dma_start(out=Wr[:], in_=io["wr"].rearrange("(k p) c -> p k c", p=128)), writes=["Wr"], key="c_wr")
        ei = [0]

        def evac(out_ap, in_ap, bankkey, writes):
            ei[0] += 1
            if ei[0] % 2:
                S.op("act", lambda e: e.copy(out=out_ap, in_=in_ap), writes=[bankkey] + writes)
            else:
                S.op("dve", lambda e: e.tensor_copy(out=out_ap, in_=in_ap), writes=[bankkey] + writes)

        mi = 0
        SA, SBq = [], []
        for t in range(TB // 128):
            p = t % 4
            r0 = t * 128
            S.capture()
            for r in range(2):
                S.dma("pool", lambda e, p=p, t=t, r=r: e.indirect_dma_start(
                    out=mt[p][:, r * 1024:(r + 1) * 1024], out_offset=None, in_=io["mixg"][:, :],
                    in_offset=bass.IndirectOffsetOnAxis(ap=ridx[:, t * 2 + r:t * 2 + r + 1], axis=0)),
                    reads=["ridx"], writes=[("mt", p)], key=("mt", p, r))
            S.dma("sp", lambda e, p=p, r0=r0: e.dma_start(out=xt[p][:], in_=io["xc"][r0:r0 + 128, :]), writes=[("xt", p)], key=("xt", p))
            for k in range(16):
                S.op("pe", lambda e, k=k, p=p: e.transpose(out=PT[:, k * 128:(k + 1) * 128], in_=mt[p][:, k * 128:(k + 1) * 128],
                                                           identity=ident[:]), reads=[("mt", p), "ident"], writes=["PT"])
            evac(mT[p][:], PT[:].rearrange("p (k t) -> p k t", k=16), "PT", [("mT", p)])
            for cg in range(4):
                pm, kp = PM[mi % 2], ("PM", mi % 2)
                mi += 1
                for k in range(16):
                    S.op("pe", lambda e, k=k, p=p, pm=pm, cg=cg: e.matmul(pm[:], lhsT=mT[p][:, k, :], rhs=Wo[:, k, cg * 512:(cg + 1) * 512],
                                                                          start=(k == 0), stop=(k == 15)),
                         reads=[("mT", p), "Wo"], writes=[kp])
                S.op("dve", lambda e, pm=pm, p=p, cg=cg: e.tensor_tensor(out=xt[p][:, cg * 512:(cg + 1) * 512], in0=pm[:],
                                                                         in1=xt[p][:, cg * 512:(cg + 1) * 512], op=ALU.add),
                     writes=[kp, ("xt", p)])
            S.dma("act", lambda e, p=p, r0=r0: e.dma_start(out=io["xmid"][r0:r0 + 128, :], in_=xt[p][:]), reads=[("xt", p)], key=("xts", p))
            sp_ = stt[p]
            ks = ("stt", p)
            S.op("act", lambda e, p=p, sp_=sp_: e.activation(out=junk[:], in_=xt[p][:], func=AF.Square, accum_out=sp_[:, 0:1]),
                 reads=[("xt", p)], writes=["junk", ks])
            S.op("act", lambda e, sp_=sp_: e.activation(out=sp_[:, 1:2], in_=sp_[:, 0:1], func=AF.Sqrt, bias=epsb[:, 0:1], scale=1.0 / D),
                 reads=["epsb"], writes=[ks])
            S.op("dve", lambda e, sp_=sp_: e.reciprocal(out=sp_[:, 2:3], in_=sp_[:, 1:2]), writes=[ks])
            S.op("dve", lambda e, p=p, sp_=sp_, t=t: e.scalar_tensor_tensor(out=h2[p][:], in0=xt[p][:], scalar=sp_[:, 2:3], in1=nwb[:],
                                                                      op0=ALU.mult, op1=ALU.mult),
                 reads=[("xt", p), ks, "nwb"], writes=[("h2", p)])
            S.dma("act", lambda e, p=p, r0=r0: e.dma_start(out=io["h2d"][r0:r0 + 128, :], in_=h2[p][:]), reads=[("h2", p)], writes=[("h2d", t)], key=("h2s", p))
            sa_ = S.end_capture()
            S.capture()
            for k in range(16):
                S.op("pe", lambda e, k=k, p=p, t=t: e.transpose(out=PT2[:, k * 128:(k + 1) * 128], in_=h2[p][:, k * 128:(k + 1) * 128],
                                                           identity=ident[:]), reads=[("h2", p), "ident"], writes=["PT2"])
            evac(h2T[p][:], PT2[:].rearrange("p (k t) -> p k t", k=16), "PT2", [("h2T", p)])
            for k in range(16):
                S.op("pe", lambda e, k=k, p=p: e.matmul(PR[:, 0:36], lhsT=h2T[p][:, k, :], rhs=Wr[:, k, :], start=(k == 0), stop=(k == 15)),
                     reads=[("h2T", p), "Wr"], writes=["PR"])
            S.op("dve", lambda e, t=t: e.tensor_tensor(out=LG[:, t, :], in0=PR[:, 0:36], in1=brb[:], op=ALU.add),
                 reads=["brb"], writes=["PR", ("LG", t)])
            SA.append(sa_)
            SBq.append(S.end_capture())
        order = list(SA[0])
        for t in range(len(SA)):
            order.extend(merge_prop(SA[t + 1], SBq[t]) if t + 1 < len(SA) else SBq[t])
        S.replay(order)
        NT = TB // 128
        R = "route"

        def b3(ap2, n):
            return ap2.unsqueeze(2).to_broadcast([128, NT, n])
        lgG = LG[:, :, 0:4]
        LGK = [("LG", t_) for t_ in range(NT)]
        S.op("dve", lambda e: e.tensor_reduce(out=mg[:], in_=lgG, axis=AX.X, op=ALU.max), reads=LGK, writes=[R])
        S.op("dve", lambda e: e.tensor_tensor(out=eg4[:], in0=lgG, in1=b3(mg[:], 4), op=ALU.subtract), reads=LGK, writes=[R])
        S.op("act", lambda e: e.activation(out=eg4[:], in_=eg4[:], func=AF.Exp), writes=[R])
        S.op("dve", lambda e: e.tensor_reduce(out=zz[:], in_=eg4[:], axis=AX.X, op=ALU.add), writes=[R])
        S.op("dve", lambda e: e.reciprocal(out=ptg[:], in_=zz[:]), writes=[R])
        S.op("dve", lambda e: e.tensor_tensor(out=og4[:], in0=lgG, in1=b3(mg[:], 4), op=ALU.is_equal), reads=LGK, writes=[R])
        S.op("dve", lambda e: e.tensor_scalar(out=og4[:], in0=og4[:], scalar1=-1.0, scalar2=1e30, op0=ALU.add, op1=ALU.mult), writes=[R])
        S.op("dve", lambda e: e.tensor_tensor(
            out=LE[:].rearrange("p t (g x) -> p t g x", g=4), in0=LG[:, :, 4:36].rearrange("p t (g x) -> p t g x", g=4),
            in1=og4[:].unsqueeze(3).to_broadcast([128, NT, 4, 8]), op=ALU.add), reads=LGK, writes=[R])
        for t in range(NT):
            S.op("dve", lambda e, t=t: e.max(out=T8[:, t, :], in_=LE[:, t, :]), writes=[R])
        S.op("dve", lambda e: e.tensor_tensor(out=rr[:], in0=T8[:, :, 1], in1=T8[:, :, 0], op=ALU.subtract), writes=[R])
        S.op("act", lambda e: e.activation(out=rr[:], in_=rr[:], func=AF.Exp), writes=[R])
        S.op("dve", lambda e: e.tensor_scalar(out=den[:], in0=rr[:], scalar1=1.0, scalar2=None, op0=ALU.add), writes=[R])
        S.op("dve", lambda e: e.reciprocal(out=den[:], in_=den[:]), writes=[R])
        S.op("dve", lambda e: e.tensor_tensor(out=GS[:, :, 0], in0=den[:], in1=ptg[:], op=ALU.mult), writes=[R])
        S.op("dve", lambda e: e.tensor_tensor(out=GS[:, :, 1], in0=GS[:, :, 0], in1=rr[:], op=ALU.mult), writes=[R])
        S.op("dve", lambda e: e.tensor_tensor(out=M1[:], in0=LE[:], in1=b3(T8[:, :, 0], 32), op=ALU.is_equal), writes=[R])
        S.op("dve", lambda e: e.tensor_tensor(out=M2[:], in0=LE[:], in1=b3(T8[:, :, 1], 32), op=ALU.is_equal), writes=[R])
        S.op("dve", lambda e: e.tensor_tensor(out=M01[:], in0=M1[:], in1=M2[:], op=ALU.add), writes=[R])
        for t in range(NT):
            S.op("pe", lambda e, t=t: e.matmul(PP[:, t * 32:(t + 1) * 32], lhsT=UTs[:, :], rhs=M01[:, t, :], start=True, stop=(t == 0)),
                 reads=[R, "UTs"], writes=["PP"])
            for t2 in range(t):
                S.op("pe", lambda e, t=t, t2=t2: e.matmul(PP[:, t * 32:(t + 1) * 32], lhsT=onesf[:, :], rhs=M01[:, t2, :], start=False, stop=(t2 == t - 1)),
                     reads=[R, "onesf"], writes=["PP"])
        S.op("dve", lambda e: e.tensor_copy(out=SL[:], in_=PP[:, 0:NT * 32].rearrange("p (t x) -> p t x", t=NT)), writes=["PP", R])
        S.op("dve", lambda e: e.tensor_scalar(out=VL[:], in0=SL[:], scalar1=float(CAP), scalar2=None, op0=ALU.is_lt), writes=[R])
        S.op("dve", lambda e: e.tensor_tensor(out=SL[:], in0=SL[:], in1=ebase[:].unsqueeze(1).to_broadcast([128, NT, 32]), op=ALU.add),
             reads=["ebase"], writes=[R])
        S.op("dve", lambda e: e.tensor_tensor(out=SL[:], in0=SL[:], in1=VL[:], op=ALU.mult), writes=[R])
        S.op("dve", lambda e: e.tensor_scalar(out=SL[:], in0=SL[:], scalar1=float(DUMMY), scalar2=None, op0=ALU.add), writes=[R])
        for kk, MK in ((0, M1), (1, M2)):
            S.op("dve", lambda e, MK=MK: e.tensor_tensor(out=MK[:], in0=MK[:], in1=SL[:], op=ALU.mult), writes=[R])
            S.op("dve", lambda e, MK=MK, kk=kk: e.tensor_reduce(out=DS[:, :, kk], in_=MK[:], axis=AX.X, op=ALU.add), writes=[R])
        S.op("dve", lambda e: e.tensor_copy(out=DI[:], in_=DS[:]), writes=[R])
        S.dma("sp", lambda e: e.dma_start(out=io["dsti"].rearrange("(n p) c -> p n c", p=128), in_=DI[:]), reads=[R], key="dis")
        S.dma("sp", lambda e: e.dma_start(out=io["gsel"].rearrange("(n p) c -> p n c", p=128), in_=GS[:]), reads=[R], key="dfs")
        for t in range(NT):
            p = t % 4
            S.dma("sp", lambda e, p=p, t=t: e.dma_start(out=h2[p][:], in_=io["h2d"][t * 128:(t + 1) * 128, :]),
                  reads=[("h2d", t)], writes=[("h2", p)], key=("h2l", p))
            for kk in range(2):
                S.dma("pool", lambda e, t=t, kk=kk, p=p: e.indirect_dma_start(
                    out=io["xg"][:, :], out_offset=bass.IndirectOffsetOnAxis(ap=DI[:, t, kk:kk + 1], axis=0),
                    in_=h2[p][:, :], in_offset=None), reads=[R, ("h2", p)], key=("scat", (t * 2 + kk) % 8))
        S.finish()


def phase_b2(nc, io):
    with contextlib.ExitStack() as st:
        S = Sched(nc, st)
        sb = lambda name, shape, dt: st.enter_context(nc.sbuf_tensor("b2_" + name, shape, dt))
        ps = lambda name, shape, dt: st.enter_context(nc.psum_tensor("b2_" + name, shape, dt))
        W1 = [sb("W1_%d" % i, [128, 16, 512], BF16) for i in range(2)]
        W2 = [sb("W2_%d" % i, [128, 16, 512], BF16) for i in range(2)]
        W3 = [sb("W3_%d" % i, [128, 4, D], BF16) for i in range(2)]
        ident = sb("ident", [128, 128], BF16)
        xgt = [sb("xgt%d" % i, [128, 2, D], BF16) for i in range(2)]
        xT = [sb("xT%d" % i, [128, 16, 256], BF16) for i in range(2)]
        hT = [sb("hT%d" % i, [128, 4, 256], BF16) for i in range(2)]
        sg = [sb("sg%d" % i, [128, 256], F32) for i in range(2)]
        ysb = [sb("ysb%d" % i, [128, D], F32) for i in range(2)]
        PT = ps("PT", [128, 2048], BF16)
        PGU = [ps("PGU%d" % i, [128, 512], F32) for i in range(4)]
        PD = [ps("PD%d" % i, [128, 512], F32) for i in range(2)]
        S.dma("sp", lambda e: e.dma_start(out=ident[:], in_=io["ident"]), writes=["ident"], key="c_ident")
        gi = 0
        di = 0
        ei = 0
        yi = 0
        for ex in range(NE):
            w = ex % 2
            S.dma("pool", lambda e, w=w, ex=ex: e.dma_start(out=W1[w][:], in_=io["wg"][ex].rearrange("(k p) f -> p k f", p=128)),
                  writes=[("W1", w)], key=("W1", w))
            S.dma("pool", lambda e, w=w, ex=ex: e.dma_start(out=W2[w][:], in_=io["wu"][ex].rearrange("(k p) f -> p k f", p=128)),
                  writes=[("W2", w)], key=("W2", w))
            S.dma("pool", lambda e, w=w, ex=ex: e.dma_start(out=W3[w][:], in_=io["wd"][ex].rearrange("(k p) f -> p k f", p=128)),
                  writes=[("W3", w)], key=("W3", w))
            S.dma("sp", lambda e, w=w, ex=ex: e.dma_start(out=xgt[w][:], in_=io["xg"][ex * CAP:(ex + 1) * CAP, :].rearrange("(n p) d -> p n d", p=128)),
                  writes=[("xgt", w)], key=("xgt", w))
            for n in range(2):
                for k in range(16):
                    S.op("pe", lambda e, k=k, n=n, w=w: e.transpose(out=PT[:, k * 128:(k + 1) * 128], in_=xgt[w][:, n, k * 128:(k + 1) * 128],
                                                                    identity=ident[:]), reads=[("xgt", w), "ident"], writes=["PT"])
                ei += 1
                if ei % 2:
                    S.op("act", lambda e, n=n, w=w: e.copy(out=xT[w][:, :, n * 128:(n + 1) * 128], in_=PT[:].rearrange("p (k t) -> p k t", k=16)),
                         writes=["PT", ("xT", w)])
                else:
                    S.op("dve", lambda e, n=n, w=w: e.tensor_copy(out=xT[w][:, :, n * 128:(n + 1) * 128], in_=PT[:].rearrange("p (k t) -> p k t", k=16)),
                         writes=["PT", ("xT", w)])
            for f in range(4):
                pg, kpg = PGU[gi % 4], ("PGU", gi % 4)
                gi += 1
                pu, kpu = PGU[gi % 4], ("PGU", gi % 4)
                gi += 1
                for k in range(16):
                    S.op("pe", lambda e, k=k, f=f, pg=pg, w=w: e.matmul(pg[:, 0:256], lhsT=W1[w][:, k, f * 128:(f + 1) * 128], rhs=xT[w][:, k, :],
                                                                        start=(k == 0), stop=(k == 15)),
                         reads=[("W1", w), ("xT", w)], writes=[kpg])
                for k in range(16):
                    S.op("pe", lambda e, k=k, f=f, pu=pu, w=w: e.matmul(pu[:, 0:256], lhsT=W2[w][:, k, f * 128:(f + 1) * 128], rhs=xT[w][:, k, :],
                                                                        start=(k == 0), stop=(k == 15)),
                         reads=[("W2", w), ("xT", w)], writes=[kpu])
                sgb, ksg = sg[f % 2], ("sg", f % 2)
                S.op("act", lambda e, pg=pg, sgb=sgb: e.activation(out=sgb[:], in_=pg[:, 0:256], func=AF.Silu), writes=[kpg, ksg])
                S.op("dve", lambda e, pu=pu, sgb=sgb, w=w, f=f: e.tensor_tensor(out=hT[w][:, f, :], in0=pu[:, 0:256], in1=sgb[:], op=ALU.mult),
                     reads=[ksg], writes=[kpu, ("hT", w)])
            for n in range(2):
                yb, kyb = ysb[yi % 2], ("ysb", yi % 2)
                yi += 1
                for cg in range(4):
                    pd, kpd = PD[di % 2], ("PD", di % 2)
                    di += 1
                    for f in range(4):
                        S.op("pe", lambda e, f=f, n=n, cg=cg, pd=pd, w=w: e.matmul(
                            pd[:], lhsT=hT[w][:, f, n * 128:(n + 1) * 128], rhs=W3[w][:, f, cg * 512:(cg + 1) * 512],
                            start=(f == 0), stop=(f == 3)), reads=[("hT", w), ("W3", w)], writes=[kpd])
                    if cg % 2:
                        S.op("act", lambda e, pd=pd, yb=yb, cg=cg: e.copy(out=yb[:, cg * 512:(cg + 1) * 512], in_=pd[:]), writes=[kpd, kyb])
                    else:
                        S.op("dve", lambda e, pd=pd, yb=yb, cg=cg: e.tensor_copy(out=yb[:, cg * 512:(cg + 1) * 512], in_=pd[:]), writes=[kpd, kyb])
                S.dma("act", lambda e, yb=yb, ex=ex, n=n: e.dma_start(out=io["yg"][ex * CAP + n * 128:ex * CAP + (n + 1) * 128, :], in_=yb[:]),
                      reads=[kyb], key=kyb)
        S.finish()


def phase_b3(nc, io):
    with contextlib.ExitStack() as st:
        S = Sched(nc, st)
        sb = lambda name, shape, dt: st.enter_context(nc.sbuf_tensor("b3_" + name, shape, dt))
        y1 = [sb("y1_%d" % i, [128, D], F32) for i in range(2)]
        y2 = [sb("y2_%d" % i, [128, D], F32) for i in range(2)]
        xm = [sb("xm%d" % i, [128, D], F32) for i in range(2)]
        yt = [sb("yt%d" % i, [128, D], F32) for i in range(2)]
        junk = sb("junk", [128, D], BF16)
        nwb = sb("nwb", [128, D], F32)
        epsb = sb("epsb", [128, 1], F32)
        gs = sb("gs", [128, 16, 2], F32)
        di_ = sb("di", [128, 16, 2], I32)
        stt = [sb("stt%d" % i, [128, 4], F32) for i in range(2)]
        S.dma("sp", lambda e: e.dma_start(out=nwb[:], in_=io["nwf"][0:1, :].partition_broadcast(128)), writes=["nwb"], key="c_nwb")
        S.dma("sp", lambda e: e.dma_start(out=gs[:], in_=io["gsel"].rearrange("(n p) c -> p n c", p=128)), writes=["gs"], key="c_gs")
        S.dma("sp", lambda e: e.dma_start(out=di_[:], in_=io["dsti"].rearrange("(n p) c -> p n c", p=128)), writes=["di"], key="c_di")
        S.op("dve", lambda e: e.memset(epsb[:], EPS), writes=["epsb"])
        for t in range(TB // 128):
            p = t % 2
            r0 = t * 128
            S.dma("sp", lambda e, p=p, r0=r0: e.dma_start(out=xm[p][:], in_=io["xmid"][r0:r0 + 128, :]), writes=[("xm", p)], key=("xm", p))
            for kk, yy in ((0, y1), (1, y2)):
                S.dma("pool", lambda e, p=p, t=t, kk=kk, yy=yy: e.indirect_dma_start(
                    out=yy[p][:, :], out_offset=None, in_=io["yg"][:, :],
                    in_offset=bass.IndirectOffsetOnAxis(ap=di_[:, t, kk:kk + 1], axis=0)),
                    reads=["di"], writes=[("y", kk, p)], key=("y", kk, p))
            S.op("dve", lambda e, p=p, t=t: e.scalar_tensor_tensor(out=xm[p][:], in0=y1[p][:], scalar=gs[:, t, 0:1], in1=xm[p][:],
                                                                  op0=ALU.mult, op1=ALU.add),
                 reads=[("y", 0, p), "gs"], writes=[("xm", p)])
            S.op("dve", lambda e, p=p, t=t: e.scalar_tensor_tensor(out=xm[p][:], in0=y2[p][:], scalar=gs[:, t, 1:2], in1=xm[p][:],
                                                                   op0=ALU.mult, op1=ALU.add),
                 reads=[("y", 1, p), "gs"], writes=[("xm", p)])
            sp_, ks = stt[p], ("stt", p)
            S.op("act", lambda e, p=p, sp_=sp_: e.activation(out=junk[:], in_=xm[p][:], func=AF.Square, accum_out=sp_[:, 0:1]),
                 reads=[("xm", p)], writes=["junk", ks])
            S.op("act", lambda e, sp_=sp_: e.activation(out=sp_[:, 1:2], in_=sp_[:, 0:1], func=AF.Sqrt, bias=epsb[:, 0:1], scale=1.0 / D),
                 reads=["epsb"], writes=[ks])
            S.op("dve", lambda e, sp_=sp_: e.reciprocal(out=sp_[:, 2:3], in_=sp_[:, 1:2]), writes=[ks])
            S.op("dve", lambda e, p=p, sp_=sp_: e.scalar_tensor_tensor(out=yt[p][:], in0=xm[p][:], scalar=sp_[:, 2:3], in1=nwb[:],
                                                                      op0=ALU.mult, op1=ALU.mult),
                 reads=[("xm", p), ks, "nwb"], writes=[("yt", p)])
            S.dma("act", lambda e, p=p, r0=r0: e.dma_start(out=io["y"][r0:r0 + 128, :], in_=yt[p][:]), reads=[("yt", p)], key=("yt", p))
        S.finish()


def phase_exchange(nc, io, Wo_t, wsem):
    sem = nc.alloc_semaphore("cc_sem")
    with nc.Block() as block:
        @block.gpsimd
        def _(e):
            for k in range(16):
                e.dma_start(out=Wo_t[:, k, :], in_=io["w_out"][k * 128:(k + 1) * 128, :]).then_inc(wsem, 16)
            for q in range(4):
                e.collective_compute("AllGather", ALU.bypass, replica_groups=[[0, 1], [2, 3], [4, 5], [6, 7]],
                                     ins=[io["mix_t"][q * 1024:(q + 1) * 1024, :].opt()],
                                     outs=[io["mixg_t"][q * 2048:(q + 1) * 2048, :].opt()]).then_inc(sem)
            e.wait_ge(sem, 4)


def build_program(upto="all"):
    nc = bass.Bass("TRN2", target_bir_lowering=False)
    io = {}

    def inp(name, shape, dt):
        io[name] = nc.dram_tensor(name, list(shape), dt, kind="ExternalInput").ap()

    def scr(name, shape, dt, out=False):
        io[name] = nc.dram_tensor(name, list(shape), dt, kind="ExternalOutput" if out else "Internal").ap()

    inp("x_b", [T, D], F32)
    inp("nw1", [1, D], F32)
    inp("w_in", [D, WCOLS], F32)
    inp("ident", [128, 128], BF16)
    dbg = upto != "all"
    scr("gqT", [NH, 128, T], F32, dbg)
    scr("gkT", [NH, 128, T], F32, dbg)
    scr("gvT", [NH, 128, T], F32, dbg)
    scr("mqT", [NH, 128, T], BF16, dbg)
    scr("mkT", [NH, 128, T], BF16, dbg)
    scr("gz", [T, 512], F32, dbg)
    scr("mv", [T, 512], BF16, dbg)
    scr("gba", [T, 8], F32, dbg)
    inp("pastneg", [128, 512], F32)
    inp("past01", [128, 512], F32)
    inp("abias", [128, 128], F32)
    inp("cmask", [128, 2, 256], BF16)
    inp("nwm", [1, 128], F32)
    inp("nwg", [1, 128], F32)
    inp("conv_w", [128, 12, 4], F32)
    inp("identf", [128, 128], F32)
    inp("maskSL", [64, 64], F32)
    inp("maskUI", [64, 64], F32)
    inp("gcon", [64, 8], F32)
    if dbg:
        scr("mix", [T, 1024], BF16, True)
    else:
        io["mix_t"] = nc.dram_tensor("mix", [T, 1024], BF16)
        io["mixg_t"] = nc.dram_tensor("mixg", [2 * T, 1024], BF16)
        io["mix"] = io["mix_t"].ap()
        io["mixg"] = io["mixg_t"].ap()
    phase_a1(nc, io, dbg)
    if upto == "a1":
        return nc
    if upto == "moba":
        phase_moba(nc, io)
        return nc
    if upto == "gdn":
        phase_gdn2(nc, io)
        return nc
    if upto == "mixers":
        phase_mixers(nc, io)
        return nc
    io["xg"] = nc.dram_tensor("xg", [NE * CAP + 128, D], BF16, kind="Internal").ap()
    phase_moba(nc, io, zero_xg=True)
    phase_gdn2(nc, io)
    if upto == "gdn":
        return nc
    inp("w_out", [D, D], F32)
    inp("xc", [TB, D], F32)
    inp("rowidx", [128, 32], I32)
    inp("nw2", [1, D], F32)
    inp("wr", [D, 36], F32)
    inp("br", [1, 36], F32)
    inp("wg", [NE, D, 512], F32)
    inp("wu", [NE, D, 512], F32)
    inp("wd", [NE, 512, D], F32)
    inp("nwf", [1, D], F32)
    io["xmid"] = nc.dram_tensor("xmid", [TB, D], F32, kind="Internal").ap()
    inp("ebase", [128, 32], F32)
    inp("uts", [128, 128], F32)
    io["h2d"] = nc.dram_tensor("h2d", [TB, D], BF16, kind="Internal").ap()
    io["yg"] = nc.dram_tensor("yg", [NE * CAP + 128, D], F32, kind="Internal").ap()
    io["gsel"] = nc.dram_tensor("gsel", [TB, 2], F32, kind="Internal").ap()
    io["dsti"] = nc.dram_tensor("dsti", [TB, 2], I32, kind="Internal").ap()
    io["y"] = nc.dram_tensor("y", [TB, D], F32, kind="ExternalOutput").ap()
    with nc.sbuf_tensor("b1_Wo", [128, 16, D], BF16) as Wo_t:
        wsem = nc.alloc_semaphore("wo_sem")
        phase_exchange(nc, io, Wo_t, wsem)
        io["Wo_pre"] = (Wo_t, wsem)
        phase_b1(nc, io)
    phase_b2(nc, io)
    phase_b3(nc, io)
    return nc


def core_inputs(inputs, c):
    b, hh = divmod(c, 2)
    w_in = inputs["w_in"][0]
    hs = slice(hh * 512, hh * 512 + 512)
    G = 1024
    cols = [w_in[:, 0 * G:1 * G][:, hs], w_in[:, 1 * G:2 * G][:, hs], w_in[:, 2 * G:3 * G][:, hs],
            w_in[:, 4 * G + 16 + 0 * G:4 * G + 16 + 1 * G][:, hs], w_in[:, 4 * G + 16 + 1 * G:4 * G + 16 + 2 * G][:, hs],
            w_in[:, 3 * G:4 * G][:, hs], w_in[:, 4 * G + 16 + 2 * G:4 * G + 16 + 3 * G][:, hs],
            w_in[:, 4 * G + hh * 4:4 * G + hh * 4 + 4], w_in[:, 4 * G + 8 + hh * 4:4 * G + 8 + hh * 4 + 4]]
    m = {
        "x_b": np.ascontiguousarray(inputs["x"][b]),
        "nw1": np.ascontiguousarray(inputs["norm_mix_w"].reshape(1, D)),
        "w_in": np.ascontiguousarray(np.concatenate(cols, axis=1)),
        "ident": np.eye(128, dtype=ml_dtypes.bfloat16),
        "nwm": np.ascontiguousarray(inputs["moba_out_norm_w"].reshape(1, 128)),
        "nwg": np.ascontiguousarray(inputs["gdn_out_norm_w"].reshape(1, 128)),
        "conv_w": np.ascontiguousarray(inputs["gdn_conv_w"][0].reshape(4, 3, 8, 128)[:, :, hh * 4:hh * 4 + 4, :].transpose(3, 1, 2, 0).reshape(128, 12, 4)),
        "gcon": np.ascontiguousarray(np.broadcast_to(np.concatenate([inputs["gdn_A_log"][0, hh * 4:hh * 4 + 4], inputs["gdn_dt_bias"][0, hh * 4:hh * 4 + 4]])[None, :], (64, 8))).astype(np.float32),
    }
    m.update(consts(hh))
    return m


_CONSTS = {}


def consts(hh):
    if hh in _CONSTS:
        return _CONSTS[hh]
    p = np.arange(128)
    tile = np.arange(32)
    j = np.arange(16)
    past = (j[None, :] < (tile[:, None] // 2))
    past01 = np.broadcast_to(past.astype(np.float32).reshape(1, 512), (128, 512)).copy()
    pastneg = ((past01 - 1.0) * 1e30).astype(np.float32)
    slopes = 2.0 ** (-8.0 * np.arange(1, 9) / 8.0)
    ab = np.zeros((128, 4, 16, 2), np.float32)
    for h in range(4):
        for dl in range(16):
            for kt in range(2):
                ab[:, h, dl, kt] = slopes[hh * 4 + h] * (-dl * 256 + kt * 128 + p - 128)
    cm = np.zeros((128, 2, 256), np.float32)
    q = np.arange(256)
    for kt in range(2):
        cm[:, kt, :] = ((kt * 128 + p)[:, None] <= q[None, :])
    i64 = np.arange(64)
    c = {"identf": np.eye(128, dtype=np.float32),
         "maskSL": (i64[:, None] > i64[None, :]).astype(np.float32),
         "maskUI": (i64[:, None] <= i64[None, :]).astype(np.float32),
         "pastneg": pastneg, "past01": past01, "abias": ab.reshape(128, 128),
         "cmask": cm.astype(ml_dtypes.bfloat16)}
    _CONSTS[hh] = c
    return c


def kernel(**inputs):
    inputs = {k: np.asarray(v) for k, v in inputs.items()}
    nc = build_program()
    w_out = inputs["w_out"][0]
    shared = {
        "w_out": np.ascontiguousarray(np.concatenate([w_out[0:512], w_out[1024:1536], w_out[512:1024], w_out[1536:2048]], axis=0)),
        "nw2": np.ascontiguousarray(inputs["norm_ffn_w"].reshape(1, D)),
        "wr": np.ascontiguousarray(np.concatenate([inputs["w_router_group"][0], inputs["w_router_expert"][0]], axis=1)),
        "br": np.ascontiguousarray(np.concatenate([inputs["b_router_group"][0], inputs["b_router_expert"][0]]).reshape(1, 36)),
        "wg": np.ascontiguousarray(inputs["w_expert_gate"][0]),
        "wu": np.ascontiguousarray(inputs["w_expert_up"][0]),
        "wd": np.ascontiguousarray(inputs["w_expert_down"][0]),
        "nwf": np.ascontiguousarray(inputs["norm_final_w"].reshape(1, D)),
        "ebase": np.ascontiguousarray(np.broadcast_to((np.arange(NE) * CAP - DUMMY).astype(np.float32)[None, :], (128, NE))),
        "uts": (np.arange(128)[:, None] < np.arange(128)[None, :]).astype(np.float32),
    }
    in_maps = []
    for c in range(8):
        b, hh = divmod(c, 2)
        m = core_inputs(inputs, c)
        m.update(shared)
        m["xc"] = np.ascontiguousarray(inputs["x"][b, hh * TB:(hh + 1) * TB])
        p = np.arange(128)[:, None]
        t = np.arange(16)[None, :]
        ri = np.zeros((128, 16, 2), np.int32)
        for r in range(2):
            ri[:, :, r] = (2 * hh + t // 8) * 2048 + r * 1024 + (t % 8) * 128 + p
        m["rowidx"] = ri.reshape(128, 32)
        in_maps.append(m)
    res = run_bass_kernel_spmd(nc, in_maps, core_ids=list(range(8)))
    y = np.stack([np.asarray(res.results[c]["y"]) for c in range(8)], axis=0)
    return y.reshape(4, T, D).astype(np.float32)
```

```python
import contextlib
import numpy as np
import ml_dtypes
import concourse.bass as bass
import concourse.mybir as mybir
from concourse.bass_utils import run_bass_kernel_spmd

F32 = mybir.dt.float32
BF16 = mybir.dt.bfloat16
I32 = mybir.dt.int32
AF = mybir.ActivationFunctionType
ALU = mybir.AluOpType
AX = mybir.AxisListType

D = 2048
T = 4096
NH = 4
HD = 128
NFM = 20
TM0 = NFM * 128
WCOLS = TM0 + 512 + 512 + 8
EPS = 1e-6


class Sched:
    ENGS = ("pe", "act", "dve", "pool", "sp")

    _G = {}

    def __init__(self, nc, stack):
        self.nc = nc
        self.stack = stack
        self.ops = {e: [] for e in self.ENGS}
        g = Sched._G.get(id(nc))
        if g is None:
            g = {"csem": {e: nc.alloc_semaphore("c_%s" % e) for e in self.ENGS}, "cnt": {e: 0 for e in self.ENGS},
                 "seen": {e: {} for e in self.ENGS}, "pool": []}
            Sched._G.clear()
            Sched._G[id(nc)] = g
        self.g = g
        self.csem = g["csem"]
        self.cnt = g["cnt"]
        self.seen = g["seen"]
        self.last_w = {}
        self.readers = {}
        self.dsem = {}
        self.all_dma_events = {}
        self._cap = None

    def capture(self):
        self._cap = []

    def end_capture(self):
        c, self._cap = self._cap, None
        return c

    def replay(self, items):
        for kind, eng, fn, reads, writes, key in items:
            if kind == "op":
                self.op(eng, fn, reads, writes)
            else:
                self.dma(eng, fn, reads, writes, key)

    def _waits(self, eng, reads, writes):
        evs = []
        for b in reads:
            if b in self.last_w:
                evs.append(self.last_w[b])
        for b in writes:
            if b in self.last_w:
                evs.append(self.last_w[b])
            evs.extend(self.readers.get(b, ()))
        need = {}
        for sem, val in evs:
            if eng == "pe" and sem is self.csem["pe"]:
                continue
            if self.seen[eng].get(sem, 0) < val:
                need[sem] = max(need.get(sem, 0), val)
        for sem, val in need.items():
            self.seen[eng][sem] = val
        return list(need.items())

    def _record(self, ev, reads, writes):
        for b in reads:
            self.readers.setdefault(b, []).append(ev)
        for b in writes:
            self.last_w[b] = ev
            self.readers[b] = []

    def op(self, eng, fn, reads=(), writes=()):
        if self._cap is not None:
            self._cap.append(("op", eng, fn, tuple(reads), tuple(writes), None))
            return
        waits = self._waits(eng, reads, writes)
        self.cnt[eng] += 1
        ev = (self.csem[eng], self.cnt[eng])
        sem = self.csem[eng]

        def emit(e):
            for s, v in waits:
                e.wait_ge(s, v)
            fn(e).then_inc(sem, 1)
        self.ops[eng].append(emit)
        self._record(ev, reads, writes)

    def dma(self, eng, fn, reads=(), writes=(), key=None):
        if self._cap is not None:
            self._cap.append(("dma", eng, fn, tuple(reads), tuple(writes), key))
            return
        waits = self._waits(eng, reads, writes)
        if key not in self.dsem:
            kind = "sw" if eng == "pool" else "hw"
            pool = self.g.setdefault("pool_" + kind, [])
            i = sum(1 for k_ in self.dsem.values() if k_[2] == kind)
            if i >= len(pool):
                pool.append([self.nc.alloc_semaphore("d%s%d" % (kind, i)), 0, kind])
            self.dsem[key] = pool[i]
        ent = self.dsem[key]
        ent[1] += 16
        sem = ent[0]
        ev = (sem, ent[1])
        self.all_dma_events[sem] = ev

        def emit(e):
            for s, v in waits:
                e.wait_ge(s, v)
            fn(e).then_inc(sem, 16)
        self.ops[eng].append(emit)
        self._record(ev, reads, writes)

    def finish(self):
        finals = list(self.all_dma_events.values()) + [(self.csem[e], self.cnt[e]) for e in self.ENGS if self.cnt[e]]
        for eng in self.ENGS:
            waits = [(s, v) for s, v in finals if self.seen[eng].get(s, 0) < v]

            def emit(e, waits=waits):
                for s, v in waits:
                    e.wait_ge(s, v)
            self.ops[eng].append(emit)
        with self.nc.Block() as block:
            @block.tensor
            def _(e):
                for f in self.ops["pe"]:
                    f(e)

            @block.scalar
            def _(e):
                for f in self.ops["act"]:
                    f(e)

            @block.vector
            def _(e):
                for f in self.ops["dve"]:
                    f(e)

            @block.gpsimd
            def _(e):
                for f in self.ops["pool"]:
                    f(e)

            @block.sync
            def _(e):
                for f in self.ops["sp"]:
                    f(e)


def merge_prop(A, B):
    out = []
    i = j = 0
    while i < len(A) or j < len(B):
        if j >= len(B) or (i < len(A) and i * len(B) <= j * len(A)):
            out.append(A[i])
            i += 1
        else:
            out.append(B[j])
            j += 1
    return out


def phase_a1(nc, io, dbg):
    x = io["x_b"]
    with contextlib.ExitStack() as st:
        S = Sched(nc, st)
        sb = lambda name, shape, dt: st.enter_context(nc.sbuf_tensor("a1_" + name, shape, dt))
        W = sb("W", [128, 16, WCOLS], BF16)
        wbc = sb("wbc", [128, D], F32)
        ident = sb("ident", [128, 128], BF16)
        xt = [sb("xt%d" % i, [128, D], F32) for i in range(2)]
        hn = [sb("hn%d" % i, [128, D], BF16) for i in range(2)]
        hT = [sb("hT%d" % i, [128, 16, 512], BF16) for i in range(2)]
        junk = sb("junk", [128, D], BF16)
        stat = [sb("stat%d" % i, [128, 4], F32) for i in range(2)]
        stg = [sb("stg%d" % i, [128, 512], F32) for i in range(4)]
        stgb = [sb("stgb%d" % i, [128, 512], BF16) for i in range(4)]
        stgs = [sb("stgs%d" % i, [128, 8], F32) for i in range(2)]
        PT = [st.enter_context(nc.psum_tensor("a1_PT%d" % i, [128, 2048], BF16)) for i in range(1)]
        PM = [st.enter_context(nc.psum_tensor("a1_PM%d" % i, [128, 512], F32)) for i in range(4)]

        S.dma("sp", lambda e: e.dma_start(out=wbc[:], in_=io["nw1"][0:1, :].partition_broadcast(128)),
              writes=["wbc"], key="c_wbc")
        S.dma("sp", lambda e: e.dma_start(out=ident[:], in_=io["ident"]), writes=["ident"], key="c_ident")
        epsb = sb("epsb", [128, 1], F32)
        S.op("dve", lambda e: e.memset(epsb[:], EPS), writes=["epsb"])
        for k in range(16):
            S.dma("pool", lambda e, k=k: e.dma_start(out=W[:, k, :], in_=io["w_in"][k * 128:(k + 1) * 128, :]),
                  writes=["W"], key="W")

        evac_i = [0]

        def evac(out_ap, in_ap, reads, writes):
            writes = list(writes) + list(reads)
            reads = []
            evac_i[0] += 1
            if evac_i[0] % 2:
                S.op("act", lambda e: e.copy(out=out_ap, in_=in_ap), reads=reads, writes=writes)
            else:
                S.op("dve", lambda e: e.tensor_copy(out=out_ap, in_=in_ap), reads=reads, writes=writes)

        mi = 0
        S1, S2 = [], []
        for st_i in range(T // 512):
            hTb = hT[st_i % 2]
            hTk = ("hT", st_i % 2)
            S.capture()
            for tt in range(4):
                ti = st_i * 4 + tt
                xb_, hb_, sb_ = xt[ti % 2], hn[ti % 2], stat[ti % 2]
                kx, kh, ks = ("xt", ti % 2), ("hn", ti % 2), ("stat", ti % 2)
                S.dma("sp", lambda e, ti=ti, xb_=xb_: e.dma_start(out=xb_[:], in_=x[ti * 128:(ti + 1) * 128, :]),
                      writes=[kx], key=kx)
                S.op("act", lambda e, xb_=xb_, sb_=sb_: e.activation(out=junk[:], in_=xb_[:], func=AF.Square,
                                                                    accum_out=sb_[:, 0:1]),
                     reads=[kx], writes=["junk", ks])
                S.op("act", lambda e, sb_=sb_: e.activation(out=sb_[:, 1:2], in_=sb_[:, 0:1], func=AF.Sqrt,
                                                           bias=epsb[:, 0:1], scale=1.0 / D),
                     reads=[ks, "epsb"], writes=[ks])
                S.op("dve", lambda e, sb_=sb_: e.reciprocal(out=sb_[:, 2:3], in_=sb_[:, 1:2]),
                     reads=[ks], writes=[ks])
                S.op("dve", lambda e, xb_=xb_, hb_=hb_, sb_=sb_: e.scalar_tensor_tensor(
                    out=hb_[:], in0=xb_[:], scalar=sb_[:, 2:3], in1=wbc[:], op0=ALU.mult, op1=ALU.mult),
                     reads=[kx, ks, "wbc"], writes=[kh])
                for k in range(16):
                    S.op("pe", lambda e, k=k, hb_=hb_: e.transpose(out=PT[0][:, k * 128:(k + 1) * 128],
                                                                   in_=hb_[:, k * 128:(k + 1) * 128],
                                                                   identity=ident[:]),
                         reads=[kh, "ident"], writes=["PT"])
                evac(hTb[:, :, tt * 128:(tt + 1) * 128], PT[0][:].rearrange("p (k t) -> p k t", k=16),
                     reads=["PT"], writes=[hTk])
            S1.append(S.end_capture())
            S.capture()
            for c in range(NFM):
                pm = PM[mi % 4]
                kp = ("PM", mi % 4)
                mi += 1
                for k in range(16):
                    S.op("pe", lambda e, k=k, c=c, pm=pm, hTb=hTb: e.matmul(
                        pm[:], lhsT=W[:, k, c * 128:(c + 1) * 128], rhs=hTb[:, k, :], start=(k == 0), stop=(k == 15)),
                         reads=[hTk, "W"], writes=[kp])
                grp, h = divmod(c, 4)
                dst = [io["gqT"], io["gkT"], io["gvT"], io["mqT"], io["mkT"]][grp]
                if grp < 3:
                    sg = stg[c % 4]
                    ksg = ("stg", c % 4)
                else:
                    sg = stgb[c % 4]
                    ksg = ("stgb", c % 4)
                evac(sg[:], pm[:], reads=[kp], writes=[ksg])
                S.dma("act", lambda e, dst=dst, h=h, sg=sg, st_i=st_i: e.dma_start(
                    out=dst[h, :, st_i * 512:(st_i + 1) * 512], in_=sg[:]), reads=[ksg], key=ksg)
            for tt in range(4):
                ti = st_i * 4 + tt
                for g in range(3):
                    c0 = TM0 + g * 512
                    nco = 512 if g < 2 else 8
                    pm = PM[mi % 4]
                    kp = ("PM", mi % 4)
                    mi += 1
                    for k in range(16):
                        S.op("pe", lambda e, k=k, pm=pm, hTb=hTb, tt=tt, c0=c0, nco=nco: e.matmul(
                            pm[:, 0:nco], lhsT=hTb[:, k, tt * 128:(tt + 1) * 128], rhs=W[:, k, c0:c0 + nco],
                            start=(k == 0), stop=(k == 15)),
                             reads=[hTk, "W"], writes=[kp])
                    if g == 0:
                        sg, ksg, dst = stg[tt % 4], ("stg", tt % 4), io["gz"]
                    elif g == 1:
                        sg, ksg, dst = stgb[tt % 4], ("stgb", tt % 4), io["mv"]
                    else:
                        sg, ksg, dst = stgs[tt % 2], ("stgs", tt % 2), io["gba"]
                    evac(sg[:, 0:nco], pm[:, 0:nco], reads=[kp], writes=[ksg])
                    S.dma("act", lambda e, dst=dst, sg=sg, ti=ti, nco=nco: e.dma_start(
                        out=dst[ti * 128:(ti + 1) * 128, :], in_=sg[:, 0:nco]), reads=[ksg], key=ksg)
            S2.append(S.end_capture())
        order = list(S1[0])
        for st_i in range(len(S2)):
            order.extend(S2[st_i])
            if st_i + 1 < len(S1):
                order.extend(S1[st_i + 1])
        S.replay(order)
        S.finish()


def phase_moba(nc, io, ctx=None, zero_xg=False):
    scale = float(HD) ** -0.5
    with contextlib.ExitStack() as st:
        if ctx is not None:
            st = ctx["st"]
        S = ctx["S"] if ctx is not None else Sched(nc, st)
        sb = lambda name, shape, dt: st.enter_context(nc.sbuf_tensor("mb_" + name, shape, dt))
        ps = lambda name, shape, dt: st.enter_context(nc.psum_tensor("mb_" + name, shape, dt))
        QT = [sb("QT%d" % i, [128, T], BF16) for i in range(2)]
        KT = [sb("KT%d" % i, [128, T], BF16) for i in range(2)]
        V = [sb("V%d" % i, [128, 32, 132], BF16) for i in range(2)]
        pastneg = sb("pastneg", [128, 512], F32)
        past01 = sb("past01", [128, 512], F32)
        abias = sb("abias", [128, 128], F32)
        cmask = sb("cmask", [128, 2, 256], BF16)
        nwb = sb("nwb", [128, 128], F32)
        epsb = sb("epsb", [128, 1], F32)
        ksumL = [sb("ksum%d" % i, [128, 16], F32) for i in range(2)]
        khiL = [sb("khi%d" % i, [128, 16], BF16) for i in range(2)]
        khfL = [sb("khf%d" % i, [128, 16], F32) for i in range(2)]
        kloL = [sb("klo%d" % i, [128, 16], BF16) for i in range(2)]
        gmL = [sb("gm%d" % i, [128, 512], F32) for i in range(2)]
        selL = [sb("sel%d" % i, [128, 512], F32) for i in range(2)]
        top8L = [sb("top8%d" % i, [128, 32, 8], F32) for i in range(2)]
        PTs = [sb("PTs%d" % i, [128, 256], BF16) for i in range(4)]
        acc = [sb("acc%d" % i, [128, 132], F32) for i in range(4)]
        osb = [sb("osb%d" % i, [128, 128], F32) for i in range(2)]
        ojk = sb("ojk", [128, 128], F32)
        ost = [sb("ost%d" % i, [128, 8], F32) for i in range(2)]
        obf = [sb("obf%d" % i, [128, 128], BF16) for i in range(2)]
        if ctx is None:
            PG = ps("PG", [128, 512], F32)
            PS = [ps("PS%d" % i, [128, 512], F32) for i in range(4)]
            PO = [ps("PO%d" % i, [128, 512], F32) for i in range(3)]
            KPG, KPS, KPO = "PG", [("PS", i) for i in range(4)], [("PO", i) for i in range(3)]
        else:
            bk = ctx["moba_banks"]
            PG, KPG = bk[0]
            PS, KPS = [bk[0][0], bk[1][0]], [bk[0][1], bk[1][1]]
            PO, KPO = [bk[2][0]], [bk[2][1]]
        NPS, NPO = len(PS), len(PO)

        S.dma("sp", lambda e: e.dma_start(out=pastneg[:], in_=io["pastneg"]), writes=["pastneg"], key="c_pastneg")
        S.dma("sp", lambda e: e.dma_start(out=past01[:], in_=io["past01"]), writes=["past01"], key="c_past01")
        S.dma("sp", lambda e: e.dma_start(out=abias[:], in_=io["abias"]), writes=["abias"], key="c_abias")
        S.dma("sp", lambda e: e.dma_start(out=cmask[:], in_=io["cmask"]), writes=["cmask"], key="c_cmask")
        S.dma("sp", lambda e: e.dma_start(out=nwb[:], in_=io["nwm"][0:1, :].partition_broadcast(128)),
              writes=["nwb"], key="c_nwbm")
        S.op("dve", lambda e: e.memset(epsb[:], EPS), writes=["epsb"])
        if zero_xg:
            zrow = sb("zrow", [128, D], BF16)
            S.op("pool", lambda e: e.memset(zrow[:], 0.0), writes=["zrow"])
            for zi in range((NE * CAP + 128) // 128):
                S.dma("act", lambda e, zi=zi: e.dma_start(out=io["xg"][zi * 128:(zi + 1) * 128, :], in_=zrow[:]), reads=["zrow"], key="c_xgz")
        si = 0
        oi = 0
        outer_cap = S._cap
        S._cap = None
        P = []
        Useg = []

        def head_body(h):
            nonlocal si, oi
            qt, kt_, v = QT[h % 2], KT[h % 2], V[h % 2]
            ksum, khi, khf, klo, gm, sel, top8 = (x[h % 2] for x in (ksumL, khiL, khfL, kloL, gmL, selL, top8L))
            kksum, kkhi, kkhf, kklo, kgm, ksel, ktop = (("pro", nm_, h % 2) for nm_ in ("ksum", "khi", "khf", "klo", "gm", "sel", "top8"))
            S.capture()
            kq, kk, kv = ("QT", h % 2), ("KT", h % 2), ("V", h % 2)
            S.dma("sp", lambda e, h=h, qt=qt: e.dma_start(out=qt[:], in_=io["mqT"][h]), writes=[kq], key=kq)
            S.dma("sp", lambda e, h=h, kt_=kt_: e.dma_start(out=kt_[:], in_=io["mkT"][h]), writes=[kk], key=kk)
            S.dma("sp", lambda e, h=h, v=v: e.dma_start(
                out=v[:, :, 0:128], in_=io["mv"][:, h * 128:(h + 1) * 128].rearrange("(n p) d -> p n d", p=128)),
                writes=[kv], key=kv)
            S.op("pool", lambda e, v=v: e.memset(v[:, :, 128:129], 1.0), writes=[kv])
            S.op("dve", lambda e, kt_=kt_: e.tensor_reduce(out=ksum[:], in_=kt_[:].rearrange("p (n k) -> p n k", k=256),
                                                          axis=AX.X, op=ALU.add), reads=[kk], writes=[kksum])
            S.op("dve", lambda e: e.tensor_copy(out=khi[:], in_=ksum[:]), reads=[kksum], writes=[kkhi])
            S.op("dve", lambda e: e.tensor_copy(out=khf[:], in_=khi[:]), reads=[kkhi], writes=[kkhf])
            S.op("dve", lambda e: e.tensor_tensor(out=klo[:], in0=ksum[:], in1=khf[:], op=ALU.subtract),
                 reads=[kksum, kkhf], writes=[kklo])
            for t in range(32):
                S.op("pe", lambda e, t=t, qt=qt: e.matmul(PG[:, t * 16:(t + 1) * 16], lhsT=qt[:, t * 128:(t + 1) * 128],
                                                          rhs=khi[:], start=True, stop=False),
                     reads=[kq, kkhi], writes=[KPG])
                S.op("pe", lambda e, t=t, qt=qt: e.matmul(PG[:, t * 16:(t + 1) * 16], lhsT=qt[:, t * 128:(t + 1) * 128],
                                                          rhs=klo[:], start=False, stop=True),
                     reads=[kq, kklo], writes=[KPG])
            S.op("dve", lambda e: e.tensor_tensor(out=gm[:], in0=PG[:], in1=pastneg[:], op=ALU.add),
                 reads=["pastneg"], writes=[kgm, KPG])
            for t in range(32):
                S.op("dve", lambda e, t=t: e.max(out=top8[:, t, :], in_=gm[:, t * 16:(t + 1) * 16]),
                     reads=[kgm], writes=[ktop])
            for t in range(32):
                S.op("dve", lambda e, t=t: e.tensor_scalar(out=sel[:, t * 16:(t + 1) * 16], in0=gm[:, t * 16:(t + 1) * 16],
                                                           scalar1=top8[:, t, 2:3], scalar2=None, op0=ALU.is_ge),
                     reads=[kgm, ktop], writes=[ksel])
            S.op("dve", lambda e: e.tensor_tensor(out=sel[:], in0=sel[:], in1=past01[:], op=ALU.mult),
                 reads=[ksel, "past01"], writes=[ksel])
            P.append(S.end_capture())
            for n in range(16):
                accs = [acc[(2 * n + q) % 4] for q in range(2)]
                kacc = [("acc", (2 * n + q) % 4) for q in range(2)]
                for j in range(n, -1, -1):
                    S.capture()
                    pts = []
                    for kt in range(2):
                        pS = PS[si % NPS]
                        half = 0
                        kps = KPS[si % NPS]
                        pt = PTs[si % 4]
                        kpt = ("PTs", si % 4)
                        si += 1
                        pts.append((pt, kpt))
                        k0 = j * 256 + kt * 128
                        S.op("pe", lambda e, pS=pS, half=half, k0=k0, n=n, kt_=kt_, qt=qt: e.matmul(
                            pS[:, half * 256:(half + 1) * 256], lhsT=kt_[:, k0:k0 + 128], rhs=qt[:, n * 256:(n + 1) * 256],
                            start=True, stop=True), reads=[kk, kq], writes=[kps])
                        bi = h * 32 + (n - j) * 2 + kt
                        S.op("act", lambda e, pS=pS, half=half, pt=pt, bi=bi: e.activation(
                            out=pt[:], in_=pS[:, half * 256:(half + 1) * 256], func=AF.Exp,
                            bias=abias[:, bi:bi + 1], scale=scale), reads=["abias"], writes=[kpt, kps])
                        if j == n:
                            S.op("pool", lambda e, pt=pt, kt=kt: e.tensor_tensor(out=pt[:], in0=pt[:], in1=cmask[:, kt, :],
                                                                                 op=ALU.mult),
                                 reads=[kpt, "cmask"], writes=[kpt])
                    s1 = S.end_capture()
                    S.capture()
                    pos = []
                    for q in range(2):
                        po = PO[oi % NPO]
                        kpo = KPO[oi % NPO]
                        oi += 1
                        pos.append((po, kpo))
                        for kt in range(2):
                            pt, kpt = pts[kt]
                            S.op("pe", lambda e, po=po, q=q, pt=pt, v=v, j=j, kt=kt: e.matmul(
                                po[:, 0:129], lhsT=pt[:, q * 128:(q + 1) * 128], rhs=v[:, j * 2 + kt, 0:129],
                                start=(kt == 0), stop=(kt == 1)), reads=[kpt, kv], writes=[kpo])
                    for q in range(2):
                        tq = 2 * n + q
                        po, kpo = pos[q]
                        if j == n:
                            S.op("act", lambda e, po=po, q=q, a=accs[q]: e.copy(out=a[:, 0:129], in_=po[:, 0:129]),
                                 writes=[kacc[q], kpo])
                        else:
                            S.op("dve", lambda e, po=po, q=q, a=accs[q], tq=tq, j=j: e.scalar_tensor_tensor(
                                out=a[:, 0:129], in0=po[:, 0:129], scalar=sel[:, tq * 16 + j:tq * 16 + j + 1],
                                in1=a[:, 0:129], op0=ALU.mult, op1=ALU.add), reads=[ksel], writes=[kacc[q], kpo])
                    Useg.append([h, s1, S.end_capture()])
                S.capture()
                for q in range(2):
                    tq = 2 * n + q
                    a = accs[q]
                    o_, os_, ob_ = osb[tq % 2], ost[tq % 2], obf[tq % 2]
                    ko, kos, kob = ("osb", tq % 2), ("ost", tq % 2), ("obf", tq % 2)
                    S.op("dve", lambda e, a=a, os_=os_: e.reciprocal(out=os_[:, 0:1], in_=a[:, 128:129]),
                         reads=[kacc[q]], writes=[kos])
                    S.op("dve", lambda e, a=a, os_=os_, o_=o_: e.tensor_scalar(out=o_[:], in0=a[:, 0:128], scalar1=os_[:, 0:1],
                                                                              scalar2=None, op0=ALU.mult),
                         reads=[kacc[q], kos], writes=[ko])
                    S.op("act", lambda e, o_=o_, os_=os_: e.activation(out=ojk[:], in_=o_[:], func=AF.Square,
                                                                      accum_out=os_[:, 1:2]),
                         reads=[ko], writes=["ojk", kos])
                    S.op("act", lambda e, os_=os_: e.activation(out=os_[:, 2:3], in_=os_[:, 1:2], func=AF.Sqrt,
                                                               bias=epsb[:, 0:1], scale=1.0 / HD),
                         reads=[kos, "epsb"], writes=[kos])
                    S.op("dve", lambda e, os_=os_: e.reciprocal(out=os_[:, 3:4], in_=os_[:, 2:3]), reads=[kos], writes=[kos])
                    S.op("dve", lambda e, o_=o_, os_=os_, ob_=ob_: e.scalar_tensor_tensor(
                        out=ob_[:], in0=o_[:], scalar=os_[:, 3:4], in1=nwb[:], op0=ALU.mult, op1=ALU.mult),
                         reads=[ko, kos, "nwb"], writes=[kob])
                    S.dma("sp", lambda e, ob_=ob_, tq=tq, h=h: e.dma_start(
                        out=io["mix"][tq * 128:(tq + 1) * 128, 512 + h * 128:512 + (h + 1) * 128], in_=ob_[:]),
                        reads=[kob], key=kob)
                Useg[-1][2].extend(S.end_capture())
        for h_ in range(NH):
            head_body(h_)
        order = list(P[0])
        nunits = {hh: sum(1 for u in Useg if u[0] == hh) for hh in range(NH)}
        ppos = {hh: 0 for hh in range(NH)}
        if Useg:
            order.extend(Useg[0][1])
        for idx, (hh, s1_, s2_) in enumerate(Useg):
            if idx + 1 < len(Useg):
                nh = Useg[idx + 1][0]
                if nh != hh:
                    order.extend(P[nh][ppos[nh]:])
                    ppos[nh] = len(P[nh])
                order.extend(Useg[idx + 1][1])
            order.extend(s2_)
            if hh + 1 < NH and ppos[hh + 1] < len(P[hh + 1]):
                step = -(-len(P[hh + 1]) // max(1, nunits[hh] - 8))
                order.extend(P[hh + 1][ppos[hh + 1]:ppos[hh + 1] + step])
                ppos[hh + 1] += step
        if outer_cap is not None:
            S._cap = outer_cap
            outer_cap.extend(order)
        else:
            S.replay(order)
        if ctx is None:
            S.finish()


def phase_gdn(nc, io):
    C = 64
    NCH = T // C
    with contextlib.ExitStack() as st:
        S = Sched(nc, st)
        sb = lambda name, shape, dt: st.enter_context(nc.sbuf_tensor("gd_" + name, shape, dt))
        PB = [st.enter_context(nc.psum_tensor("gd_P%d" % i, [128, 512], F32)) for i in range(8)]
        pbi = [0]

        def bank():
            i = pbi[0] % 8
            pbi[0] += 1
            return PB[i], ("PB", i)

        raw = sb("raw", [128, T + 3], F32)
        cv = [sb("cv%d" % i, [128, T], F32) for i in range(3)]
        tmpf = sb("tmpf", [128, T], F32)
        cw = sb("cw", [128, 12, 4], F32)
        identf = sb("identf", [128, 128], F32)
        ones = sb("ones", [128, 128], F32)
        maskSL = sb("maskSL", [64, 64], F32)
        maskUI = sb("maskUI", [64, 64], F32)
        Utri = sb("Utri", [64, 64], F32)
        gcon = sb("gcon", [64, 8], F32)
        nwb = sb("nwb", [64, 128], F32)
        c1 = sb("c1", [128, 1], F32)
        cq = sb("cq", [128, 1], F32)
        ck = sb("ck", [128, 1], F32)
        ce = sb("ce", [128, 1], F32)
        gba = sb("gba", [64, NCH, 8], F32)
        zt = sb("zt", [64, NCH, 128], F32)
        obuf = sb("obuf", [64, NCH, 128], BF16)
        col = {nm: sb("col_" + nm, [64, NCH], F32) for nm in ("beta", "nbeta", "gl", "g", "eg", "egl", "beg", "tmp", "tmp2")}
        gt = sb("gt", [128, NCH], F32)
        Sst = sb("Sst", [128, 128], F32)
        dg = sb("dg", [64, 64], F32)
        t64 = [sb("t64_%d" % i, [64, 64], F32) for i in range(4)]
        E1 = sb("E1", [64, 64], F32)
        E2 = sb("E2", [64, 64], F32)
        Am = [sb("Am%d" % i, [64, 64], F32) for i in range(2)]
        Bm = [sb("Bm%d" % i, [64, 64], F32) for i in range(2)]
        X = sb("X", [64, 64], F32)
        kbe = sb("kbe", [64, 128], F32)
        kdec = sb("kdec", [64, 128], F32)
        vb = sb("vb", [64, 128], F32)
        wTn = sb("wTn", [128, 64], F32)
        attnT = sb("attnT", [64, 64], F32)
        vnew = sb("vnew", [64, 128], F32)
        oq = sb("oq", [64, 128], F32)
        osb = sb("osb", [64, 128], F32)
        ojk = sb("ojk", [64, 128], F32)
        ost = sb("ost", [64, 4], F32)

        def cdma(dst, src, key):
            S.dma("sp", lambda e: e.dma_start(out=dst, in_=src), writes=[key], key="const")
        cdma(cw[:], io["conv_w"], "cw")
        cdma(identf[:], io["identf"], "identf")
        cdma(maskSL[:], io["maskSL"], "maskSL")
        cdma(maskUI[:], io["maskUI"], "maskUI")
        cdma(Utri[:], io["maskUI"], "Utri")
        cdma(gcon[:], io["gcon"], "gcon")
        cdma(nwb[:], io["nwg"][0:1, :].partition_broadcast(64), "nwb")
        cdma(gba[:], io["gba"].rearrange("(n p) c -> p n c", p=C), "gba")
        S.op("dve", lambda e: e.memset(ones[:], 1.0), writes=["ones"])
        S.op("dve", lambda e: e.memset(c1[:], 1.0), writes=["c1"])
        S.op("dve", lambda e: e.memset(cq[:], 128.0 * 1e-6), writes=["cq"])
        S.op("dve", lambda e: e.memset(ck[:], 1e-6), writes=["ck"])
        S.op("dve", lambda e: e.memset(ce[:], EPS), writes=["ce"])
        S.op("dve", lambda e: e.memset(raw[:, 0:3], 0.0), writes=["rawpad"])
        S.op("act", lambda e: e.activation(out=gcon[:, 0:4], in_=gcon[:, 0:4], func=AF.Exp), reads=["gcon"], writes=["gcon"])

        for h in range(NH):
            for ti, (src, nm) in enumerate(((io["gqT"], "q"), (io["gkT"], "k"), (io["gvT"], "v"))):
                S.dma("sp", lambda e, src=src, h=h: e.dma_start(out=raw[:, 3:], in_=src[h]), reads=["rawpad"], writes=["raw"], key="raw")
                cvt = cv[ti]
                kc = ("cv", ti)
                wi = ti * 4 + h
                S.op("dve", lambda e, cvt=cvt, wi=wi: e.tensor_scalar(out=cvt[:], in0=raw[:, 3:3 + T], scalar1=cw[:, wi, 3:4],
                                                                     scalar2=None, op0=ALU.mult), reads=["raw", "cw"], writes=[kc])
                for j in (2, 1, 0):
                    S.op("dve", lambda e, cvt=cvt, wi=wi, j=j: e.scalar_tensor_tensor(
                        out=cvt[:], in0=raw[:, j:j + T], scalar=cw[:, wi, j:j + 1], in1=cvt[:], op0=ALU.mult, op1=ALU.add),
                         reads=["raw", "cw", kc], writes=[kc])
                S.op("act", lambda e, cvt=cvt: e.activation(out=cvt[:], in_=cvt[:], func=AF.Silu), reads=[kc], writes=[kc])
                if ti < 2:
                    S.op("act", lambda e, cvt=cvt: e.activation(out=tmpf[:], in_=cvt[:], func=AF.Square), reads=[kc], writes=["tmpf"])
                    for g8 in range(T // 512):
                        pb, kb = bank()
                        S.op("pe", lambda e, pb=pb, g8=g8: e.matmul(pb[:, :], lhsT=ones[:, :], rhs=tmpf[:, g8 * 512:(g8 + 1) * 512],
                                                                    start=True, stop=True), reads=["ones", "tmpf"], writes=[kb])
                        if ti == 0:
                            S.op("act", lambda e, pb=pb, g8=g8: e.activation(out=raw[:, 3 + g8 * 512:3 + (g8 + 1) * 512], in_=pb[:, :],
                                                                            func=AF.Sqrt, bias=cq[:, 0:1], scale=128.0),
                                 reads=["cq"], writes=[kb, "raw"])
                        else:
                            S.op("act", lambda e, pb=pb, g8=g8: e.activation(out=raw[:, 3 + g8 * 512:3 + (g8 + 1) * 512], in_=pb[:, :],
                                                                            func=AF.Sqrt, bias=ck[:, 0:1], scale=1.0),
                                 reads=["ck"], writes=[kb, "raw"])
                    S.op("dve", lambda e: e.reciprocal(out=tmpf[:], in_=raw[:, 3:3 + T]), reads=["raw"], writes=["tmpf"])
                    S.op("dve", lambda e, cvt=cvt: e.tensor_tensor(out=cvt[:], in0=cvt[:], in1=tmpf[:], op=ALU.mult),
                         reads=[kc, "tmpf"], writes=[kc])
            qn, kn, vn = cv
            cb, cnb, cgl, cg, ceg, cegl, cbeg, ctmp, ctmp2 = (col[k_] for k_ in ("beta", "nbeta", "gl", "g", "eg", "egl", "beg", "tmp", "tmp2"))
            S.op("act", lambda e, h=h: e.activation(out=cb[:], in_=gba[:, :, h], func=AF.Sigmoid), reads=["gba"], writes=["c_beta"])
            S.op("dve", lambda e: e.tensor_scalar(out=cnb[:], in0=cb[:], scalar1=-1.0, scalar2=None, op0=ALU.mult),
                 reads=["c_beta"], writes=["c_nbeta"])
            S.op("act", lambda e, h=h: e.activation(out=ctmp[:], in_=gba[:, :, 4 + h], func=AF.Exp, bias=gcon[:, 4 + h:5 + h], scale=1.0),
                 reads=["gba", "gcon"], writes=["c_tmp"])
            S.op("act", lambda e: e.activation(out=ctmp[:], in_=ctmp[:], func=AF.Ln, bias=c1[0:64, 0:1], scale=1.0),
                 reads=["c_tmp", "c1"], writes=["c_tmp"])
            S.op("dve", lambda e, h=h: e.tensor_scalar(out=cgl[:], in0=ctmp[:], scalar1=gcon[:, h:h + 1], scalar2=-1.0,
                                                      op0=ALU.mult, op1=ALU.mult), reads=["c_tmp", "gcon"], writes=["c_gl"])
            pb, kb = bank()
            S.op("pe", lambda e, pb=pb: e.matmul(pb[0:64, 0:NCH], lhsT=Utri[:, :], rhs=cgl[:, :], start=True, stop=True),
                 reads=["Utri", "c_gl"], writes=[kb])
            S.op("dve", lambda e, pb=pb: e.tensor_copy(out=cg[:], in_=pb[0:64, 0:NCH]), writes=[kb, "c_g"])
            pb, kb = bank()
            S.op("pe", lambda e, pb=pb: e.matmul(pb[:, 0:NCH], lhsT=ones[0:64, :], rhs=cgl[:, :], start=True, stop=True),
                 reads=["ones", "c_gl"], writes=[kb])
            S.op("act", lambda e, pb=pb: e.activation(out=gt[:], in_=pb[:, 0:NCH], func=AF.Exp), writes=[kb, "gt"])
            S.op("dve", lambda e, pb=pb: e.tensor_tensor(out=ctmp2[:], in0=pb[0:64, 0:NCH], in1=cg[:], op=ALU.subtract),
                 reads=["c_g"], writes=[kb, "c_tmp2"])
            S.op("act", lambda e: e.activation(out=cegl[:], in_=ctmp2[:], func=AF.Exp), reads=["c_tmp2"], writes=["c_egl"])
            S.op("act", lambda e: e.activation(out=ceg[:], in_=cg[:], func=AF.Exp), reads=["c_g"], writes=["c_eg"])
            S.op("dve", lambda e: e.tensor_tensor(out=cbeg[:], in0=cb[:], in1=ceg[:], op=ALU.mult),
                 reads=["c_beta", "c_eg"], writes=["c_beg"])
            S.dma("sp", lambda e, h=h: e.dma_start(out=zt[:], in_=io["gz"][:, h * 128:(h + 1) * 128].rearrange("(n p) d -> p n d", p=C)),
                  writes=["zt"], key="zt")
            S.op("act", lambda e: e.activation(out=zt[:], in_=zt[:], func=AF.Silu), reads=["zt"], writes=["zt"])
            for n8 in range(8):
                S.op("pool", lambda e, n8=n8: e.tensor_tensor(out=zt[:, n8 * 8:(n8 + 1) * 8, :], in0=zt[:, n8 * 8:(n8 + 1) * 8, :],
                                                             in1=nwb[:].unsqueeze(1).to_broadcast([64, 8, 128]), op=ALU.mult),
                     reads=["zt", "nwb"], writes=["zt"])
            S.op("dve", lambda e: e.memset(Sst[:], 0.0), writes=["S"])

            for n in range(NCH):
                c0 = n * C
                kT_c, qT_c, vT_c = kn[:, c0:c0 + C], qn[:, c0:c0 + C], vn[:, c0:c0 + C]
                pk, kpk = bank()
                S.op("pe", lambda e, pk=pk, kT_c=kT_c: e.transpose(out=pk[0:64, 0:128], in_=kT_c, identity=identf[:]),
                     reads=[("cv", 1), "identf"], writes=[kpk])
                S.op("dve", lambda e, pk=pk, n=n: e.tensor_scalar(out=kbe[:], in0=pk[0:64, 0:128], scalar1=cbeg[:, n:n + 1], scalar2=None,
                                                                 op0=ALU.mult), reads=["c_beg"], writes=[kpk, "kbe"])
                S.op("act", lambda e, pk=pk, n=n: e.activation(out=kdec[:], in_=pk[0:64, 0:128], func=AF.Copy, scale=cegl[:, n:n + 1]),
                     reads=["c_egl"], writes=[kpk, "kdec"])
                pv, kpv = bank()
                S.op("pe", lambda e, pv=pv, vT_c=vT_c: e.transpose(out=pv[0:64, 0:128], in_=vT_c, identity=identf[:]),
                     reads=[("cv", 2), "identf"], writes=[kpv])
                S.op("act", lambda e, pv=pv, n=n: e.activation(out=vb[:], in_=pv[0:64, 0:128], func=AF.Copy, scale=cb[:, n:n + 1]),
                     reads=["c_beta"], writes=[kpv, "vb"])
                S.op("dve", lambda e, n=n: e.tensor_scalar(out=dg[:], in0=identf[0:64, 0:64], scalar1=cg[:, n:n + 1], scalar2=None,
                                                          op0=ALU.mult), reads=["identf", "c_g"], writes=["dg"])
                pg, kpg = bank()
                S.op("pe", lambda e, pg=pg: e.matmul(pg[0:64, 0:64], lhsT=ones[0:64, 0:64], rhs=dg[:, :], start=True, stop=True),
                     reads=["ones", "dg"], writes=[kpg])
                S.op("dve", lambda e, pg=pg, n=n: e.tensor_scalar(out=t64[0][:], in0=pg[0:64, 0:64], scalar1=cg[:, n:n + 1], scalar2=0.0,
                                                                 op0=ALU.subtract, op1=ALU.max), reads=["c_g"], writes=[kpg, "t0"])
                S.op("dve", lambda e, pg=pg, n=n: e.tensor_scalar(out=t64[1][:], in0=pg[0:64, 0:64], scalar1=cg[:, n:n + 1], scalar2=0.0,
                                                                 op0=ALU.subtract, op1=ALU.min), reads=["c_g"], writes=[kpg, "t1"])
                S.op("act", lambda e: e.activation(out=E1[:], in_=t64[0][:], func=AF.Exp, scale=-1.0), reads=["t0"], writes=["E1"])
                S.op("act", lambda e: e.activation(out=E2[:], in_=t64[1][:], func=AF.Exp), reads=["t1"], writes=["E2"])
                S.op("pool", lambda e: e.tensor_tensor(out=E2[:], in0=E2[:], in1=maskUI[:], op=ALU.mult),
                     reads=["maskUI"], writes=["E2"])
                pkk, kpkk = bank()
                S.op("pe", lambda e, pkk=pkk, kT_c=kT_c: e.matmul(pkk[0:64, 0:64], lhsT=kT_c, rhs=kT_c, start=True, stop=True),
                     reads=[("cv", 1)], writes=[kpkk])
                S.op("dve", lambda e, pkk=pkk: e.tensor_tensor(out=t64[2][:], in0=pkk[0:64, 0:64], in1=E1[:], op=ALU.mult),
                     reads=["E1"], writes=[kpkk, "t2"])
                A_, B_ = Am[0], Bm[0]
                S.op("dve", lambda e, n=n, A_=A_: e.scalar_tensor_tensor(out=A_[:], in0=t64[2][:], scalar=cnb[:, n:n + 1], in1=maskSL[:],
                                                                        op0=ALU.mult, op1=ALU.mult),
                     reads=["t2", "c_nbeta", "maskSL"], writes=[("A", 0)])
                pt_, kpt_ = bank()
                S.op("pe", lambda e, pt_=pt_, A_=A_: e.transpose(out=pt_[0:64, 0:64], in_=A_[:, :], identity=identf[0:64, 0:64]),
                     reads=[("A", 0), "identf"], writes=[kpt_])
                S.op("act", lambda e, pt_=pt_, B_=B_: e.copy(out=B_[:], in_=pt_[0:64, 0:64]), writes=[kpt_, ("B", 0)])
                S.op("dve", lambda e, pt_=pt_: e.tensor_tensor(out=X[:], in0=pt_[0:64, 0:64], in1=identf[0:64, 0:64], op=ALU.add),
                     reads=["identf"], writes=[kpt_, "X"])
                cur = 0
                for lv in range(5):
                    nxt = 1 - cur
                    pa, kpa = bank()
                    S.op("pe", lambda e, pa=pa, cur=cur: e.matmul(pa[0:64, 0:64], lhsT=Bm[cur][:, :], rhs=Am[cur][:, :], start=True, stop=True),
                         reads=[("A", cur), ("B", cur)], writes=[kpa])
                    if lv < 4:
                        pbb, kpbb = bank()
                        S.op("pe", lambda e, pbb=pbb, cur=cur: e.matmul(pbb[0:64, 0:64], lhsT=Am[cur][:, :], rhs=Bm[cur][:, :],
                                                                        start=True, stop=True),
                             reads=[("A", cur), ("B", cur)], writes=[kpbb])
                    S.op("act", lambda e, pa=pa, nxt=nxt: e.copy(out=Am[nxt][:], in_=pa[0:64, 0:64]), writes=[kpa, ("A", nxt)])
                    if lv < 4:
                        S.op("dve", lambda e, pbb=pbb, nxt=nxt: e.tensor_copy(out=Bm[nxt][:], in_=pbb[0:64, 0:64]), writes=[kpbb, ("B", nxt)])
                    px, kpx = bank()
                    S.op("pe", lambda e, px=px, nxt=nxt: e.matmul(px[0:64, 0:64], lhsT=Am[nxt][:, :], rhs=X[:, :], start=True, stop=True),
                         reads=[("A", nxt), "X"], writes=[kpx])
                    S.op("dve", lambda e, px=px: e.tensor_tensor(out=X[:], in0=px[0:64, 0:64], in1=X[:], op=ALU.add),
                         writes=[kpx, "X"])
                    cur = nxt
                pw, kpw = bank()
                S.op("pe", lambda e, pw=pw: e.matmul(pw[:, 0:64], lhsT=kbe[:, :], rhs=X[:, :], start=True, stop=True),
                     reads=["kbe", "X"], writes=[kpw])
                S.op("act", lambda e, pw=pw: e.mul(out=wTn[:], in_=pw[:, 0:64], mul=-1.0), writes=[kpw, "wTn"])
                pq, kpq = bank()
                S.op("pe", lambda e, pq=pq, kT_c=kT_c, qT_c=qT_c: e.matmul(pq[0:64, 0:64], lhsT=kT_c, rhs=qT_c, start=True, stop=True),
                     reads=[("cv", 0), ("cv", 1)], writes=[kpq])
                S.op("dve", lambda e, pq=pq: e.tensor_tensor(out=attnT[:], in0=pq[0:64, 0:64], in1=E2[:], op=ALU.mult),
                     reads=["E2"], writes=[kpq, "attnT"])
                pvn, kpvn = bank()
                S.op("pe", lambda e, pvn=pvn: e.matmul(pvn[0:64, 0:128], lhsT=X[:, :], rhs=vb[:, :], start=True, stop=False),
                     reads=["X", "vb"], writes=[kpvn])
                S.op("pe", lambda e, pvn=pvn: e.matmul(pvn[0:64, 0:128], lhsT=wTn[:, :], rhs=Sst[:, :], start=False, stop=True),
                     reads=["wTn", "S"], writes=[kpvn])
                S.op("act", lambda e, pvn=pvn: e.copy(out=vnew[:], in_=pvn[0:64, 0:128]), writes=[kpvn, "vnew"])
                po1, kpo1 = bank()
                S.op("pe", lambda e, po1=po1, qT_c=qT_c: e.matmul(po1[0:64, 0:128], lhsT=qT_c, rhs=Sst[:, :], start=True, stop=True),
                     reads=[("cv", 0), "S"], writes=[kpo1])
                S.op("act", lambda e, po1=po1, n=n: e.activation(out=oq[:], in_=po1[0:64, 0:128], func=AF.Copy, scale=ceg[:, n:n + 1]),
                     reads=["c_eg"], writes=[kpo1, "oq"])
                po2, kpo2 = bank()
                S.op("pe", lambda e, po2=po2: e.matmul(po2[0:64, 0:128], lhsT=attnT[:, :], rhs=vnew[:, :], start=True, stop=True),
                     reads=["attnT", "vnew"], writes=[kpo2])
                S.op("dve", lambda e, po2=po2: e.tensor_tensor(out=osb[:], in0=po2[0:64, 0:128], in1=oq[:], op=ALU.add),
                     reads=["oq"], writes=[kpo2, "osb"])
                pS_, kpS = bank()
                S.op("pe", lambda e, pS_=pS_: e.matmul(pS_[:, 0:128], lhsT=kdec[:, :], rhs=vnew[:, :], start=True, stop=True),
                     reads=["kdec", "vnew"], writes=[kpS])
                S.op("dve", lambda e, pS_=pS_, n=n: e.scalar_tensor_tensor(out=Sst[:], in0=Sst[:], scalar=gt[:, n:n + 1], in1=pS_[:, 0:128],
                                                                          op0=ALU.mult, op1=ALU.add),
                     reads=["gt"], writes=[kpS, "S"])
                S.op("act", lambda e: e.activation(out=ojk[:], in_=osb[:], func=AF.Square, accum_out=ost[:, 0:1]),
                     reads=["osb"], writes=["ojk", "ost"])
                S.op("act", lambda e: e.activation(out=ost[:, 1:2], in_=ost[:, 0:1], func=AF.Sqrt, bias=ce[0:64, 0:1], scale=1.0 / HD),
                     reads=["ost", "ce"], writes=["ost"])
                S.op("dve", lambda e: e.reciprocal(out=ost[:, 2:3], in_=ost[:, 1:2]), reads=["ost"], writes=["ost"])
                S.op("dve", lambda e, n=n: e.scalar_tensor_tensor(out=obuf[:, n, :], in0=osb[:], scalar=ost[:, 2:3], in1=zt[:, n, :],
                                                                 op0=ALU.mult, op1=ALU.mult),
                     reads=["osb", "ost", "zt"], writes=["obuf"])
            S.dma("sp", lambda e, h=h: e.dma_start(out=io["mix"][:, h * 128:(h + 1) * 128].rearrange("(n p) d -> p n d", p=C), in_=obuf[:]),
                  reads=["obuf"], key="obuf")
        S.finish()


def phase_gdn2(nc, io, ctx=None):
    C = 64
    NCH = T // C
    G = 8
    NG = NCH // G
    CB = 512
    with contextlib.ExitStack() as st:
        if ctx is not None:
            st = ctx["st"]
        S = ctx["S"] if ctx is not None else Sched(nc, st)
        sb = lambda name, shape, dt: st.enter_context(nc.sbuf_tensor("g2_" + name, shape, dt))
        if ctx is None:
            PBK = [st.enter_context(nc.psum_tensor("g2_P%d" % i, [128, 512], F32)) for i in range(8)]
            LB = [(PBK[i], ("PB", i)) for i in range(4)]
            SB_ = [(PBK[i], ("PB", i)) for i in range(4, 7)]
            PREPB = [(PBK[7], ("PB", 7))]
        else:
            LB, SB_ = ctx["gdn_local_banks"], ctx["gdn_scan_banks"]
            PREPB = LB
        li = [0]
        si = [0]

        def lbank():
            i = li[0] % len(LB)
            li[0] += 1
            return LB[i]

        pi_ = [0]

        def pbank():
            i = pi_[0] % len(PREPB)
            pi_[0] += 1
            return PREPB[i]

        def sbank():
            i = si[0] % len(SB_)
            si[0] += 1
            return SB_[i]

        cw = sb("cw", [128, 12, 4], F32)
        identf = sb("identf", [128, 128], F32)
        ones = sb("ones", [128, 128], F32)
        maskSL = sb("maskSL", [64, 64], F32)
        maskUI = sb("maskUI", [64, 64], F32)
        gcon = sb("gcon", [64, 8], F32)
        nwb = sb("nwb", [64, 128], F32)
        c1 = sb("c1", [128, 1], F32)
        cq = sb("cq", [128, 1], F32)
        ck = sb("ck", [128, 1], F32)
        ce = sb("ce", [128, 1], F32)
        gba = sb("gba", [64, NCH, 8], F32)
        rawb = [sb("rawb%d" % i, [128, CB + 3], F32) for i in range(2)]
        tmpb = [sb("tmpb%d" % i, [128, CB], F32) for i in range(2)]
        cv = [[sb("cv%d_%d" % (hd, i), [128, T], BF16) for i in range(3)] for hd in range(NH)]
        dstb = [sb("dstb%d" % i, [128, CB], F32) for i in range(2)]
        identb = sb("identb", [128, 128], BF16)
        Sbf = [sb("Sbf%d" % hd, [128, 128], BF16) for hd in range(NH)]
        cnames = ("beta", "nbeta", "gl", "g", "eg", "egl", "beg", "tmp", "tmp2")
        col = [{nm: sb("col%d_%s" % (hd, nm), [64, NCH], F32) for nm in cnames} for hd in range(NH)]
        gt = [sb("gt%d" % hd, [128, NCH], F32) for hd in range(NH)]
        Sst = [sb("Sst%d" % hd, [128, 128], F32) for hd in range(NH)]
        X = [[sb("X%d_%d" % (hd, b), [64, G, 64], BF16) for b in range(2)] for hd in range(2)]
        vb = [[sb("vb%d_%d" % (hd, b), [64, G, 128], BF16) for b in range(2)] for hd in range(2)]
        kdec = [[sb("kdec%d_%d" % (hd, b), [64, G, 128], BF16) for b in range(2)] for hd in range(2)]
        wTn = [[sb("wTn%d_%d" % (hd, b), [128, G, 64], BF16) for b in range(2)] for hd in range(2)]
        attnT = [[sb("attnT%d_%d" % (hd, b), [64, G, 64], BF16) for b in range(2)] for hd in range(2)]
        zt = [[sb("zt%d_%d" % (hd, b), [64, G, 128], BF16) for b in range(2)] for hd in range(2)]
        kbe = [sb("kbe%d" % hd, [64, G, 128], BF16) for hd in range(2)]
        dg = [sb("dg%d" % hd, [64, G, 64], F32) for hd in range(2)]
        dd = [sb("dd%d" % hd, [64, G, 64], F32) for hd in range(2)]
        E1 = [sb("E1_%d" % hd, [64, G, 64], F32) for hd in range(2)]
        E2 = [sb("E2_%d" % hd, [64, G, 64], F32) for hd in range(2)]
        Am = [[sb("Am%d_%d" % (hd, b), [64, G, 64], BF16) for b in range(2)] for hd in range(2)]
        Bm = [[sb("Bm%d_%d" % (hd, b), [64, G, 64], BF16) for b in range(2)] for hd in range(2)]
        osb = [sb("osb%d" % hd, [64, G, 128], F32) for hd in range(2)]
        ojk = [sb("ojk%d" % hd, [64, G, 128], BF16) for hd in range(2)]
        obuf = [sb("obuf%d" % hd, [64, G, 128], BF16) for hd in range(2)]
        ost = [sb("ost%d" % hd, [64, 4 * G], F32) for hd in range(2)]
        vnew = [sb("vnew%d" % hd, [64, 128], BF16) for hd in range(2)]
        oq = [sb("oq%d" % hd, [64, 128], F32) for hd in range(2)]

        def cdma(dst, src, key):
            S.dma("sp", lambda e: e.dma_start(out=dst, in_=src), writes=[key], key="c_" + key)
        cdma(cw[:], io["conv_w"], "cw")
        cdma(identf[:], io["identf"], "identf")
        cdma(identb[:], io["ident"], "identb")
        cdma(maskSL[:], io["maskSL"], "maskSL")
        cdma(maskUI[:], io["maskUI"], "maskUI")
        cdma(gcon[:], io["gcon"], "gcon")
        cdma(nwb[:], io["nwg"][0:1, :].partition_broadcast(64), "nwb")
        cdma(gba[:], io["gba"].rearrange("(n p) c -> p n c", p=C), "gba")
        S.op("dve", lambda e: e.memset(ones[:], 1.0), writes=["ones"])
        S.op("dve", lambda e: e.memset(c1[:], 1.0), writes=["c1"])
        S.op("dve", lambda e: e.memset(cq[:], 128.0 * 1e-6), writes=["cq"])
        S.op("dve", lambda e: e.memset(ck[:], 1e-6), writes=["ck"])
        S.op("dve", lambda e: e.memset(ce[:], EPS), writes=["ce"])
        S.op("act", lambda e: e.activation(out=gcon[:, 0:4], in_=gcon[:, 0:4], func=AF.Exp), reads=["gcon"], writes=["gcon"])
        ident64 = identf[0:64, 0:64]
        ident64b = identb[0:64, 0:64]
        F32R = mybir.dt.float32r

        class _PE:
            def __init__(self, e):
                self.e = e

            def matmul(self, out, lhsT, rhs, start, stop):
                return getattr(self.e, 'matmul')(out, lhsT=lhsT, rhs=rhs, start=start, stop=stop)
        bi = [0]

        def prep(h, hd):
            for ti, src in enumerate((io["gqT"], io["gkT"], io["gvT"])):
                cvt = cv[h][ti]
                kc = ("cv", h, ti)
                wi = ti * 4 + h
                for blk in range(T // CB):
                    b = bi[0] % 2
                    bi[0] += 1
                    rb, tb = rawb[b], tmpb[b]
                    krb, ktb = ("rawb", b), ("tmpb", b)
                    c0 = blk * CB
                    if blk == 0:
                        S.op("pool", lambda e, rb=rb: e.memset(rb[:, 0:3], 0.0), writes=[krb])
                        S.dma("sp", lambda e, rb=rb, src=src, h=h: e.dma_start(out=rb[:, 3:], in_=src[h][:, 0:CB]), writes=[krb], key=krb)
                    else:
                        S.dma("sp", lambda e, rb=rb, src=src, h=h, c0=c0: e.dma_start(out=rb[:, :], in_=src[h][:, c0 - 3:c0 + CB]), writes=[krb], key=krb)
                    dst = dstb[b][:, :]
                    kd_ = ("dstb", b)
                    fin = cvt[:, c0:c0 + CB]
                    S.op("dve", lambda e, rb=rb, dst=dst, wi=wi: e.tensor_scalar(out=dst, in0=rb[:, 3:3 + CB], scalar1=cw[:, wi, 3:4],
                                                                                scalar2=None, op0=ALU.mult), reads=[krb, "cw"], writes=[kd_])
                    for j in (2, 1, 0):
                        S.op("dve", lambda e, rb=rb, dst=dst, wi=wi, j=j: e.scalar_tensor_tensor(
                            out=dst, in0=rb[:, j:j + CB], scalar=cw[:, wi, j:j + 1], in1=dst, op0=ALU.mult, op1=ALU.add),
                             reads=[krb, "cw"], writes=[kd_])
                    if ti == 2:
                        S.op("act", lambda e, dst=dst, fin=fin: e.activation(out=fin, in_=dst, func=AF.Silu), reads=[kd_], writes=[kc])
                    else:
                        S.op("act", lambda e, dst=dst: e.activation(out=dst, in_=dst, func=AF.Silu), writes=[kd_])
                    if ti < 2:
                        S.op("act", lambda e, dst=dst, tb=tb: e.activation(out=tb[:], in_=dst, func=AF.Square), reads=[kd_], writes=[ktb])
                        for g8 in range(CB // 512):
                            pb, kb = pbank()
                            S.op("pe", lambda e, pb=pb, g8=g8, tb=tb: _PE(e).matmul(pb[:, :], lhsT=ones[:, :], rhs=tb[:, g8 * 512:(g8 + 1) * 512],
                                                                              start=True, stop=True), reads=["ones", ktb], writes=[kb])
                            S.op("act", lambda e, pb=pb, g8=g8, rb=rb, ti=ti: e.activation(
                                out=rb[:, g8 * 512:(g8 + 1) * 512], in_=pb[:, :], func=AF.Ln,
                                bias=(cq if ti == 0 else ck)[:, 0:1], scale=(128.0 if ti == 0 else 1.0)),
                                 reads=["cq", "ck"], writes=[kb, krb])
                        S.op("act", lambda e, tb=tb, rb=rb: e.activation(out=tb[:], in_=rb[:, 0:CB], func=AF.Exp, scale=-0.5), reads=[krb], writes=[ktb])
                        S.op("pool", lambda e, dst=dst, tb=tb, fin=fin: e.tensor_tensor(out=fin, in0=dst, in1=tb[:], op=ALU.mult), reads=[ktb, kd_], writes=[kc])
            cl = col[h]
            cb_, cnb, cgl, cg, ceg, cegl, cbeg, ctmp, ctmp2 = (cl[k_] for k_ in cnames)
            kcol = ("col", h)
            S.op("act", lambda e: e.activation(out=cb_[:], in_=gba[:, :, h], func=AF.Sigmoid), reads=["gba"], writes=[kcol])
            S.op("dve", lambda e: e.tensor_scalar(out=cnb[:], in0=cb_[:], scalar1=-1.0, scalar2=None, op0=ALU.mult), writes=[kcol])
            S.op("act", lambda e: e.activation(out=ctmp[:], in_=gba[:, :, 4 + h], func=AF.Exp, bias=gcon[:, 4 + h:5 + h], scale=1.0),
                 reads=["gba", "gcon"], writes=[kcol])
            S.op("act", lambda e: e.activation(out=ctmp[:], in_=ctmp[:], func=AF.Ln, bias=c1[0:64, 0:1], scale=1.0), reads=["c1"], writes=[kcol])
            S.op("dve", lambda e: e.tensor_scalar(out=cgl[:], in0=ctmp[:], scalar1=gcon[:, h:h + 1], scalar2=-1.0, op0=ALU.mult, op1=ALU.mult),
                 reads=["gcon"], writes=[kcol])
            pb, kb = pbank()
            S.op("pe", lambda e, pb=pb: _PE(e).matmul(pb[0:64, 0:NCH], lhsT=maskUI[:, :], rhs=cgl[:, :], start=True, stop=True),
                 reads=["maskUI", kcol], writes=[kb])
            S.op("dve", lambda e, pb=pb: e.tensor_copy(out=cg[:], in_=pb[0:64, 0:NCH]), writes=[kb, kcol])
            pb2, kb2 = pbank()
            S.op("pe", lambda e, pb2=pb2: _PE(e).matmul(pb2[:, 0:NCH], lhsT=ones[0:64, :], rhs=cgl[:, :], start=True, stop=True),
                 reads=["ones", kcol], writes=[kb2])
            S.op("act", lambda e, pb2=pb2: e.activation(out=gt[h][:], in_=pb2[:, 0:NCH], func=AF.Exp), writes=[kb2, ("gt", h)])
            S.op("dve", lambda e, pb2=pb2: e.tensor_tensor(out=ctmp2[:], in0=pb2[0:64, 0:NCH], in1=cg[:], op=ALU.subtract), writes=[kb2, kcol])
            S.op("act", lambda e: e.activation(out=cegl[:], in_=ctmp2[:], func=AF.Exp), writes=[kcol])
            S.op("act", lambda e: e.activation(out=ceg[:], in_=cg[:], func=AF.Exp), writes=[kcol])
            S.op("dve", lambda e: e.tensor_tensor(out=cbeg[:], in0=cb_[:], in1=ceg[:], op=ALU.mult), writes=[kcol])
            S.op("dve", lambda e: e.memset(Sst[h][:], 0.0), writes=[("S", h)])
            S.op("pool", lambda e: e.memset(Sbf[h][:], 0.0), writes=[("Sb", h)])

        def bc2(ap2, n):
            return ap2.unsqueeze(2).to_broadcast([64, G, n])

        def bc1(ap2, n=64):
            return ap2.unsqueeze(1).to_broadcast([64, G, n])

        def v3(bank_ap, n):
            return bank_ap.rearrange("p (g i) -> p g i", g=G)

        def local_steps(h, hd, g):
            b = g % 2
            n0 = g * G
            qn, kn, vn = cv[h]
            cl = col[h]
            kcol = ("col", h)
            kX, kvb, kkd, kw, kat, kz = (("X", hd, b), ("vb", hd, b), ("kdec", hd, b), ("wTn", hd, b), ("attnT", hd, b), ("zt", hd, b))
            steps = []

            def s_kv():
                pk, kpk = lbank()
                for gi in range(G):
                    c0 = (n0 + gi) * C
                    S.op("pe", lambda e, pk=pk, gi=gi, c0=c0: e.transpose(out=pk[:].bitcast(BF16)[0:64, gi * 128:(gi + 1) * 128], in_=kn[:, c0:c0 + C], identity=identb[:]),
                         reads=[("cv", h, 1), "identb"], writes=[kpk])
                S.op("dve", lambda e, pk=pk: e.tensor_tensor(out=kbe[hd][:], in0=v3(pk[:].bitcast(BF16)[0:64, 0:G * 128], 128), in1=bc2(cl["beg"][:, n0:n0 + G], 128), op=ALU.mult),
                     reads=[kcol], writes=[kpk, ("kbe", hd)])
                S.op("dve", lambda e, pk=pk: e.tensor_tensor(out=kdec[hd][b][:], in0=v3(pk[:].bitcast(BF16)[0:64, 0:G * 128], 128), in1=bc2(cl["egl"][:, n0:n0 + G], 128), op=ALU.mult),
                     reads=[kcol], writes=[kpk, kkd])
                pv, kpv = lbank()
                for gi in range(G):
                    c0 = (n0 + gi) * C
                    S.op("pe", lambda e, pv=pv, gi=gi, c0=c0: e.transpose(out=pv[:].bitcast(BF16)[0:64, gi * 128:(gi + 1) * 128], in_=vn[:, c0:c0 + C], identity=identb[:]),
                         reads=[("cv", h, 2), "identb"], writes=[kpv])
                S.op("dve", lambda e, pv=pv: e.tensor_tensor(out=vb[hd][b][:], in0=v3(pv[:].bitcast(BF16)[0:64, 0:G * 128], 128), in1=bc2(cl["beta"][:, n0:n0 + G], 128), op=ALU.mult),
                     reads=[kcol], writes=[kpv, kvb])
            steps.append(s_kv)

            def s_decay():
                S.op("dve", lambda e: e.tensor_tensor(out=dg[hd][:], in0=bc1(ident64), in1=bc2(cl["g"][:, n0:n0 + G], 64), op=ALU.mult),
                     reads=["identf", kcol], writes=[("dg", hd)])
                pg, kpg = lbank()
                S.op("pe", lambda e, pg=pg: _PE(e).matmul(pg[0:64, 0:G * 64], lhsT=ones[0:64, 0:64], rhs=dg[hd][:].rearrange("p g i -> p (g i)"),
                                                     start=True, stop=True), reads=["ones", ("dg", hd)], writes=[kpg])
                S.op("dve", lambda e, pg=pg: e.tensor_tensor(out=dd[hd][:], in0=v3(pg[0:64, 0:G * 64], 64), in1=bc2(cl["g"][:, n0:n0 + G], 64), op=ALU.subtract),
                     reads=[kcol], writes=[kpg, ("dd", hd)])
                S.op("dve", lambda e: e.tensor_scalar(out=E1[hd][:], in0=dd[hd][:], scalar1=0.0, scalar2=None, op0=ALU.max),
                     reads=[("dd", hd)], writes=[("E1", hd)])
                S.op("act", lambda e: e.activation(out=E1[hd][:], in_=E1[hd][:], func=AF.Exp, scale=-1.0), writes=[("E1", hd)])
                S.op("dve", lambda e: e.tensor_tensor(out=E1[hd][:], in0=E1[hd][:], in1=bc1(maskSL[:]), op=ALU.mult), reads=["maskSL"], writes=[("E1", hd)])
                S.op("dve", lambda e: e.tensor_tensor(out=E1[hd][:], in0=E1[hd][:], in1=bc2(cl["nbeta"][:, n0:n0 + G], 64), op=ALU.mult),
                     reads=[kcol], writes=[("E1", hd)])
                S.op("dve", lambda e: e.tensor_scalar(out=E2[hd][:], in0=dd[hd][:], scalar1=0.0, scalar2=None, op0=ALU.min),
                     reads=[("dd", hd)], writes=[("E2", hd)])
                S.op("act", lambda e: e.activation(out=E2[hd][:], in_=E2[hd][:], func=AF.Exp), writes=[("E2", hd)])
                S.op("pool", lambda e: e.tensor_tensor(out=E2[hd][:], in0=E2[hd][:], in1=bc1(maskUI[:]), op=ALU.mult), reads=["maskUI"], writes=[("E2", hd)])
            steps.append(s_decay)

            def s_A():
                pkk, kpkk = lbank()
                for gi in range(G):
                    c0 = (n0 + gi) * C
                    S.op("pe", lambda e, pkk=pkk, gi=gi, c0=c0: _PE(e).matmul(pkk[0:64, gi * 64:(gi + 1) * 64], lhsT=kn[:, c0:c0 + C], rhs=kn[:, c0:c0 + C],
                                                                         start=True, stop=True), reads=[("cv", h, 1)], writes=[kpkk])
                S.op("dve", lambda e, pkk=pkk: e.tensor_tensor(out=Am[hd][0][:], in0=v3(pkk[0:64, 0:G * 64], 64), in1=E1[hd][:], op=ALU.mult),
                     reads=[("E1", hd)], writes=[kpkk, ("A", hd, 0)])
                pt_, kpt_ = lbank()
                for gi in range(G):
                    S.op("pe", lambda e, pt_=pt_, gi=gi: e.transpose(out=pt_[:].bitcast(BF16)[0:64, gi * 64:(gi + 1) * 64], in_=Am[hd][0][:, gi, :], identity=ident64b),
                         reads=[("A", hd, 0), "identb"], writes=[kpt_])
                S.op("act", lambda e, pt_=pt_: e.copy(out=Bm[hd][0][:], in_=v3(pt_[:].bitcast(BF16)[0:64, 0:G * 64], 64)), writes=[kpt_, ("B", hd, 0)])
                S.op("dve", lambda e, pt_=pt_: e.tensor_tensor(out=X[hd][b][:], in0=v3(pt_[:].bitcast(BF16)[0:64, 0:G * 64], 64), in1=bc1(ident64), op=ALU.add),
                     reads=["identf"], writes=[kpt_, kX])
            steps.append(s_A)

            def mk_level(lv):
                def s_lv():
                    cur = lv % 2
                    nxt = 1 - cur
                    pa, kpa = lbank()
                    for gi in range(G):
                        S.op("pe", lambda e, pa=pa, gi=gi: _PE(e).matmul(pa[0:64, gi * 64:(gi + 1) * 64], lhsT=Bm[hd][cur][:, gi, :], rhs=Am[hd][cur][:, gi, :],
                                                                    start=True, stop=True), reads=[("A", hd, cur), ("B", hd, cur)], writes=[kpa])
                    if lv < 4:
                        pbb, kpbb = lbank()
                        for gi in range(G):
                            S.op("pe", lambda e, pbb=pbb, gi=gi: _PE(e).matmul(pbb[0:64, gi * 64:(gi + 1) * 64], lhsT=Am[hd][cur][:, gi, :], rhs=Bm[hd][cur][:, gi, :],
                                                                          start=True, stop=True), reads=[("A", hd, cur), ("B", hd, cur)], writes=[kpbb])
                    S.op("act", lambda e, pa=pa: e.copy(out=Am[hd][nxt][:], in_=v3(pa[0:64, 0:G * 64], 64)), writes=[kpa, ("A", hd, nxt)])
                    if lv < 4:
                        S.op("dve", lambda e, pbb=pbb: e.tensor_copy(out=Bm[hd][nxt][:], in_=v3(pbb[0:64, 0:G * 64], 64)), writes=[kpbb, ("B", hd, nxt)])
                    px, kpx = lbank()
                    for gi in range(G):
                        S.op("pe", lambda e, px=px, gi=gi: _PE(e).matmul(px[0:64, gi * 64:(gi + 1) * 64], lhsT=Am[hd][nxt][:, gi, :], rhs=X[hd][b][:, gi, :],
                                                                    start=True, stop=True), reads=[("A", hd, nxt), kX], writes=[kpx])
                    S.op("dve", lambda e, px=px: e.tensor_tensor(out=X[hd][b][:], in0=v3(px[0:64, 0:G * 64], 64), in1=X[hd][b][:], op=ALU.add),
                         writes=[kpx, kX])
                return s_lv
            for lv in range(5):
                steps.append(mk_level(lv))

            def s_w():
                pw, kpw = lbank()
                for gi in range(G):
                    S.op("pe", lambda e, pw=pw, gi=gi: _PE(e).matmul(pw[:, gi * 64:(gi + 1) * 64], lhsT=kbe[hd][:, gi, :], rhs=X[hd][b][:, gi, :],
                                                                start=True, stop=True), reads=[("kbe", hd), kX], writes=[kpw])
                S.op("act", lambda e, pw=pw: e.mul(out=wTn[hd][b][:], in_=pw[:, 0:G * 64].rearrange("p (g i) -> p g i", g=G), mul=-1.0),
                     writes=[kpw, kw])
                pq, kpq = lbank()
                for gi in range(G):
                    c0 = (n0 + gi) * C
                    S.op("pe", lambda e, pq=pq, gi=gi, c0=c0: _PE(e).matmul(pq[0:64, gi * 64:(gi + 1) * 64], lhsT=kn[:, c0:c0 + C], rhs=qn[:, c0:c0 + C],
                                                                       start=True, stop=True), reads=[("cv", h, 0), ("cv", h, 1)], writes=[kpq])
                S.op("dve", lambda e, pq=pq: e.tensor_tensor(out=attnT[hd][b][:], in0=v3(pq[0:64, 0:G * 64], 64), in1=E2[hd][:], op=ALU.mult),
                     reads=[("E2", hd)], writes=[kpq, kat])
                S.dma("pool", lambda e: e.dma_start(out=zt[hd][b][:], in_=io["gz"][n0 * C:(n0 + G) * C, h * 128:(h + 1) * 128].rearrange("(g p) d -> p g d", p=C)),
                      writes=[kz], key=kz)
                S.op("act", lambda e: e.activation(out=zt[hd][b][:], in_=zt[hd][b][:], func=AF.Silu), writes=[kz])
                S.op("pool", lambda e: e.tensor_tensor(out=zt[hd][b][:], in0=zt[hd][b][:], in1=bc1(nwb[:], 128), op=ALU.mult), reads=["nwb"], writes=[kz])
            steps.append(s_w)
            return steps

        def scan_steps(h, hd, g):
            b = g % 2
            n0 = g * G
            qn = cv[h][0]
            cl = col[h]
            kcol = ("col", h)
            kX, kvb, kkd, kw, kat, kz = (("X", hd, b), ("vb", hd, b), ("kdec", hd, b), ("wTn", hd, b), ("attnT", hd, b), ("zt", hd, b))
            steps = []
            for gi in range(G):
                n = n0 + gi
                c0 = n * C

                def s_v(gi=gi, n=n, c0=c0):
                    pvn, kpvn = sbank()
                    S.op("pe", lambda e, pvn=pvn: _PE(e).matmul(pvn[0:64, 0:128], lhsT=X[hd][b][:, gi, :], rhs=vb[hd][b][:, gi, :], start=True, stop=False),
                         reads=[kX, kvb], writes=[kpvn])
                    S.op("pe", lambda e, pvn=pvn: _PE(e).matmul(pvn[0:64, 0:128], lhsT=wTn[hd][b][:, gi, :], rhs=Sbf[h][:, :], start=False, stop=True),
                         reads=[kw, ("Sb", h)], writes=[kpvn])
                    S.op("act", lambda e, pvn=pvn: e.copy(out=vnew[hd][:], in_=pvn[0:64, 0:128]), writes=[kpvn, ("vnew", hd)])
                    po1, kpo1 = sbank()
                    S.op("pe", lambda e, po1=po1: _PE(e).matmul(po1[0:64, 0:128], lhsT=qn[:, c0:c0 + C], rhs=Sbf[h][:, :], start=True, stop=True),
                         reads=[("cv", h, 0), ("Sb", h)], writes=[kpo1])
                    S.op("act", lambda e, po1=po1: e.activation(out=oq[hd][:], in_=po1[0:64, 0:128], func=AF.Copy, scale=cl["eg"][:, n:n + 1]),
                         reads=[kcol], writes=[kpo1, ("oq", hd)])
                steps.append(s_v)

                def s_o(gi=gi, n=n):
                    po2, kpo2 = sbank()
                    S.op("pe", lambda e, po2=po2: _PE(e).matmul(po2[0:64, 0:128], lhsT=attnT[hd][b][:, gi, :], rhs=vnew[hd][:, :], start=True, stop=True),
                         reads=[kat, ("vnew", hd)], writes=[kpo2])
                    S.op("dve", lambda e, po2=po2: e.tensor_tensor(out=osb[hd][:, gi, :], in0=po2[0:64, 0:128], in1=oq[hd][:], op=ALU.add),
                         reads=[("oq", hd)], writes=[kpo2, ("osb", hd)])
                    pS_, kpS = sbank()
                    S.op("pe", lambda e, pS_=pS_: _PE(e).matmul(pS_[:, 0:128], lhsT=kdec[hd][b][:, gi, :], rhs=vnew[hd][:, :], start=True, stop=True),
                         reads=[kkd, ("vnew", hd)], writes=[kpS])
                    S.op("dve", lambda e, pS_=pS_: e.scalar_tensor_tensor(out=Sst[h][:], in0=Sst[h][:], scalar=gt[h][:, n:n + 1], in1=pS_[:, 0:128],
                                                                         op0=ALU.mult, op1=ALU.add),
                         reads=[("gt", h)], writes=[kpS, ("S", h)])
                    S.op("act", lambda e: e.copy(out=Sbf[h][:], in_=Sst[h][:]), reads=[("S", h)], writes=[("Sb", h)])
                steps.append(s_o)

            def s_norm():
                o_ = ost[hd]
                ko = ("ost", hd)
                S.op("act", lambda e: e.activation(out=ojk[hd][:], in_=osb[hd][:], func=AF.Square), reads=[("osb", hd)], writes=[("ojk", hd)])
                S.op("dve", lambda e: e.tensor_reduce(out=o_[:, 0:G], in_=ojk[hd][:], axis=AX.X, op=ALU.add), reads=[("ojk", hd)], writes=[ko])
                S.op("act", lambda e: e.activation(out=o_[:, G:2 * G], in_=o_[:, 0:G], func=AF.Sqrt, bias=ce[0:64, 0:1], scale=1.0 / HD),
                     reads=["ce"], writes=[ko])
                S.op("dve", lambda e: e.reciprocal(out=o_[:, 2 * G:3 * G], in_=o_[:, G:2 * G]), writes=[ko])
                S.op("dve", lambda e: e.tensor_tensor(out=osb[hd][:], in0=osb[hd][:], in1=bc2(o_[:, 2 * G:3 * G], 128), op=ALU.mult),
                     reads=[ko], writes=[("osb", hd)])
                S.op("pool", lambda e: e.tensor_tensor(out=obuf[hd][:], in0=osb[hd][:], in1=zt[hd][b][:], op=ALU.mult),
                     reads=[kz], writes=[("osb", hd), ("obuf", hd)])
                S.dma("sp", lambda e: e.dma_start(out=io["mix"][n0 * C:(n0 + G) * C, h * 128:(h + 1) * 128].rearrange("(g p) d -> p g d", p=C), in_=obuf[hd][:]),
                      reads=[("obuf", hd)], key=("obuf", hd))
            steps.append(s_norm)
            return steps

        def lock(step_lists):
            n = max(len(x) for x in step_lists)
            for i in range(n):
                for x in step_lists:
                    if i < len(x):
                        x[i]()

        outer_cap = S._cap
        S._cap = None
        PP, LP = [], []
        for pair in range(NH // 2):
            hs = (2 * pair, 2 * pair + 1)
            S.capture()
            for hd, h in enumerate(hs):
                prep(h, hd)
            PP.append(S.end_capture())
            S.capture()
            lock([local_steps(h, hd, 0) for hd, h in enumerate(hs)])
            for g in range(NG):
                loc = [local_steps(h, hd, g + 1) for hd, h in enumerate(hs)] if g + 1 < NG else []
                scn = [scan_steps(h, hd, g) for hd, h in enumerate(hs)]
                lock(loc + scn)
            LP.append(S.end_capture())
        order = list(PP[0])
        for pair in range(NH // 2):
            order.extend(merge_prop(LP[pair], PP[pair + 1]) if pair + 1 < len(PP) else LP[pair])
        if outer_cap is not None:
            S._cap = outer_cap
            outer_cap.extend(order)
        else:
            S.replay(order)
        if ctx is None:
            S.finish()


def phase_mixers(nc, io):
    with contextlib.ExitStack() as st:
        S = Sched(nc, st)
        banks = [(st.enter_context(nc.psum_tensor("mx_P%d" % i, [128, 512], F32)), ("PB", i)) for i in range(8)]
        ctx = {"S": S, "st": st, "gdn_local_banks": banks[0:3], "gdn_scan_banks": banks[3:5], "moba_banks": banks[5:8]}
        S.capture()
        phase_moba(nc, io, ctx)
        M = S.end_capture()
        S.capture()
        phase_gdn2(nc, io, ctx)
        Gd = S.end_capture()
        i = j = 0
        merged = []
        while i < len(Gd) or j < len(M):
            if j >= len(M) or (i < len(Gd) and i * len(M) <= j * len(Gd)):
                merged.append(Gd[i])
                i += 1
            else:
                merged.append(M[j])
                j += 1
        S.replay(merged)
        S.finish()


TB = 2048
NE = 32
CAP = 256
DUMMY = NE * CAP


def phase_b1(nc, io):
    with contextlib.ExitStack() as st:
        S = Sched(nc, st)
        sb = lambda name, shape, dt: st.enter_context(nc.sbuf_tensor("b1_" + name, shape, dt))
        ps = lambda name, shape, dt: st.enter_context(nc.psum_tensor("b1_" + name, shape, dt))
        Wo, wsem = io["Wo_pre"]
        S.last_w["Wo"] = (wsem, 16 * 16)
        Wr = sb("Wr", [128, 16, 36], BF16)
        ident = sb("ident", [128, 128], BF16)
        nwb = sb("nwb", [128, D], F32)
        brb = sb("brb", [128, 36], F32)
        epsb = sb("epsb", [128, 1], F32)
        mt = [sb("mt%d" % i, [128, D], BF16) for i in range(4)]
        mT = [sb("mT%d" % i, [128, 16, 128], BF16) for i in range(4)]
        xt = [sb("xt%d" % i, [128, D], F32) for i in range(4)]
        h2 = [sb("h2_%d" % i, [128, D], BF16) for i in range(4)]
        h2T = [sb("h2T%d" % i, [128, 16, 128], BF16) for i in range(4)]
        junk = sb("junk", [128, D], BF16)
        stt = [sb("stt%d" % i, [128, 16], F32) for i in range(4)]
        NT_ = TB // 128
        LG = sb("LG", [128, NT_, 36], F32)
        mg = sb("mg", [128, NT_], F32)
        zz = sb("zz", [128, NT_], F32)
        ptg = sb("ptg", [128, NT_], F32)
        rr = sb("rr", [128, NT_], F32)
        den = sb("den", [128, NT_], F32)
        eg4 = sb("eg4", [128, NT_, 4], F32)
        og4 = sb("og4", [128, NT_, 4], F32)
        LE = sb("LE", [128, NT_, 32], F32)
        T8 = sb("T8", [128, NT_, 8], F32)
        M1 = sb("M1", [128, NT_, 32], F32)
        M2 = sb("M2", [128, NT_, 32], F32)
        M01 = sb("M01", [128, NT_, 32], F32)
        SL = sb("SL", [128, NT_, 32], F32)
        VL = sb("VL", [128, NT_, 32], F32)
        GS = sb("GS", [128, NT_, 2], F32)
        DS = sb("DS", [128, NT_, 2], F32)
        DI = sb("DI", [128, NT_, 2], I32)
        cnt = sb("cnt", [128, 32], F32)
        ebase = sb("ebase", [128, 32], F32)
        UTs = sb("UTs", [128, 128], F32)
        onesf = sb("onesf", [128, 128], F32)
        PT = ps("PT", [128, 2048], BF16)
        PM = [ps("PM%d" % i, [128, 512], F32) for i in range(2)]
        PT2 = ps("PT2", [128, 2048], BF16)
        PR = ps("PR", [128, 512], F32)
        PP = ps("PP", [128, 512], F32)
        S.op("dve", lambda e: e.memset(onesf[:], 1.0), writes=["onesf"])
        S.dma("sp", lambda e: e.dma_start(out=ebase[:], in_=io["ebase"]), writes=["ebase"], key="c_eb")
        S.dma("sp", lambda e: e.dma_start(out=UTs[:], in_=io["uts"]), writes=["UTs"], key="c_uts")

        S.dma("sp", lambda e: e.dma_start(out=ident[:], in_=io["ident"]), writes=["ident"], key="c_ident")
        S.dma("sp", lambda e: e.dma_start(out=nwb[:], in_=io["nw2"][0:1, :].partition_broadcast(128)), writes=["nwb"], key="c_nwb")
        S.dma("sp", lambda e: e.dma_start(out=brb[:], in_=io["br"][0:1, :].partition_broadcast(128)), writes=["brb"], key="c_brb")
        ridx = sb("ridx", [128, 32], I32)
        S.dma("sp", lambda e: e.dma_start(out=ridx[:], in_=io["rowidx"]), writes=["ridx"], key="c_ridx")
        S.op("dve", lambda e: e.memset(epsb[:], EPS), writes=["epsb"])
        S.dma("pool", lambda e: e.dma_start(out=Wr[:], in_=io["wr"].rearrange("(k p) c -> p k c", p=128)), writes=["Wr"], key="c_wr")
        ei = [0]

        def evac(out_ap, in_ap, bankkey, writes):
            ei[0] += 1
            if ei[0] % 2:
                S.op("act", lambda e: e.copy(out=out_ap, in_=in_ap), writes=[bankkey] + writes)
            else:
                S.op("dve", lambda e: e.tensor_copy(out=out_ap, in_=in_ap), writes=[bankkey] + writes)

        mi = 0
        SA, SBq = [], []
        for t in range(TB // 128):
            p = t % 4
            r0 = t * 128
            S.capture()
            for r in range(2):
                S.dma("pool", lambda e, p=p, t=t, r=r: e.indirect_dma_start(
                    out=mt[p][:, r * 1024:(r + 1) * 1024], out_offset=None, in_=io["mixg"][:, :],
                    in_offset=bass.IndirectOffsetOnAxis(ap=ridx[:, t * 2 + r:t * 2 + r + 1], axis=0)),
                    reads=["ridx"], writes=[("mt", p)], key=("mt", p, r))
            S.dma("sp", lambda e, p=p, r0=r0: e.dma_start(out=xt[p][:], in_=io["xc"][r0:r0 + 128, :]), writes=[("xt", p)], key=("xt", p))
            for k in range(16):
                S.op("pe", lambda e, k=k, p=p: e.transpose(out=PT[:, k * 128:(k + 1) * 128], in_=mt[p][:, k * 128:(k + 1) * 128],
                                                           identity=ident[:]), reads=[("mt", p), "ident"], writes=["PT"])
            evac(mT[p][:], PT[:].rearrange("p (k t) -> p k t", k=16), "PT", [("mT", p)])
            for cg in range(4):
                pm, kp = PM[mi % 2], ("PM", mi % 2)
                mi += 1
                for k in range(16):
                    S.op("pe", lambda e, k=k, p=p, pm=pm, cg=cg: e.matmul(pm[:], lhsT=mT[p][:, k, :], rhs=Wo[:, k, cg * 512:(cg + 1) * 512],
                                                                          start=(k == 0), stop=(k == 15)),
                         reads=[("mT", p), "Wo"], writes=[kp])
                S.op("dve", lambda e, pm=pm, p=p, cg=cg: e.tensor_tensor(out=xt[p][:, cg * 512:(cg + 1) * 512], in0=pm[:],
                                                                         in1=xt[p][:, cg * 512:(cg + 1) * 512], op=ALU.add),
                     writes=[kp, ("xt", p)])
            S.dma("act", lambda e, p=p, r0=r0: e.dma_start(out=io["xmid"][r0:r0 + 128, :], in_=xt[p][:]), reads=[("xt", p)], key=("xts", p))
            sp_ = stt[p]
            ks = ("stt", p)
            S.op("act", lambda e, p=p, sp_=sp_: e.activation(out=junk[:], in_=xt[p][:], func=AF.Square, accum_out=sp_[:, 0:1]),
                 reads=[("xt", p)], writes=["junk", ks])
            S.op("act", lambda e, sp_=sp_: e.activation(out=sp_[:, 1:2], in_=sp_[:, 0:1], func=AF.Sqrt, bias=epsb[:, 0:1], scale=1.0 / D),
                 reads=["epsb"], writes=[ks])
            S.op("dve", lambda e, sp_=sp_: e.reciprocal(out=sp_[:, 2:3], in_=sp_[:, 1:2]), writes=[ks])
            S.op("dve", lambda e, p=p, sp_=sp_, t=t: e.scalar_tensor_tensor(out=h2[p][:], in0=xt[p][:], scalar=sp_[:, 2:3], in1=nwb[:],
                                                                      op0=ALU.mult, op1=ALU.mult),
                 reads=[("xt", p), ks, "nwb"], writes=[("h2", p)])
            S.dma("act", lambda e, p=p, r0=r0: e.dma_start(out=io["h2d"][r0:r0 + 128, :], in_=h2[p][:]), reads=[("h2", p)], writes=[("h2d", t)], key=("h2s", p))
            sa_ = S.end_capture()
            S.capture()
            for k in range(16):
                S.op("pe", lambda e, k=k, p=p, t=t: e.transpose(out=PT2[:, k * 128:(k + 1) * 128], in_=h2[p][:, k * 128:(k + 1) * 128],
                                                           identity=ident[:]), reads=[("h2", p), "ident"], writes=["PT2"])
            evac(h2T[p][:], PT2[:].rearrange("p (k t) -> p k t", k=16), "PT2", [("h2T", p)])
            for k in range(16):
                S.op("pe", lambda e, k=k, p=p: e.matmul(PR[:, 0:36], lhsT=h2T[p][:, k, :], rhs=Wr[:, k, :], start=(k == 0), stop=(k == 15)),
                     reads=[("h2T", p), "Wr"], writes=["PR"])
            S.op("dve", lambda e, t=t: e.tensor_tensor(out=LG[:, t, :], in0=PR[:, 0:36], in1=brb[:], op=ALU.add),
                 reads=["brb"], writes=["PR", ("LG", t)])
            SA.append(sa_)
            SBq.append(S.end_capture())
        order = list(SA[0])
        for t in range(len(SA)):
            order.extend(merge_prop(SA[t + 1], SBq[t]) if t + 1 < len(SA) else SBq[t])
        S.replay(order)
        NT = TB // 128
        R = "route"

        def b3(ap2, n):
            return ap2.unsqueeze(2).to_broadcast([128, NT, n])
        lgG = LG[:, :, 0:4]
        LGK = [("LG", t_) for t_ in range(NT)]
        S.op("dve", lambda e: e.tensor_reduce(out=mg[:], in_=lgG, axis=AX.X, op=ALU.max), reads=LGK, writes=[R])
        S.op("dve", lambda e: e.tensor_tensor(out=eg4[:], in0=lgG, in1=b3(mg[:], 4), op=ALU.subtract), reads=LGK, writes=[R])
        S.op("act", lambda e: e.activation(out=eg4[:], in_=eg4[:], func=AF.Exp), writes=[R])
        S.op("dve", lambda e: e.tensor_reduce(out=zz[:], in_=eg4[:], axis=AX.X, op=ALU.add), writes=[R])
        S.op("dve", lambda e: e.reciprocal(out=ptg[:], in_=zz[:]), writes=[R])
        S.op("dve", lambda e: e.tensor_tensor(out=og4[:], in0=lgG, in1=b3(mg[:], 4), op=ALU.is_equal), reads=LGK, writes=[R])
        S.op("dve", lambda e: e.tensor_scalar(out=og4[:], in0=og4[:], scalar1=-1.0, scalar2=1e30, op0=ALU.add, op1=ALU.mult), writes=[R])
        S.op("dve", lambda e: e.tensor_tensor(
            out=LE[:].rearrange("p t (g x) -> p t g x", g=4), in0=LG[:, :, 4:36].rearrange("p t (g x) -> p t g x", g=4),
            in1=og4[:].unsqueeze(3).to_broadcast([128, NT, 4, 8]), op=ALU.add), reads=LGK, writes=[R])
        for t in range(NT):
            S.op("dve", lambda e, t=t: e.max(out=T8[:, t, :], in_=LE[:, t, :]), writes=[R])
        S.op("dve", lambda e: e.tensor_tensor(out=rr[:], in0=T8[:, :, 1], in1=T8[:, :, 0], op=ALU.subtract), writes=[R])
        S.op("act", lambda e: e.activation(out=rr[:], in_=rr[:], func=AF.Exp), writes=[R])
        S.op("dve", lambda e: e.tensor_scalar(out=den[:], in0=rr[:], scalar1=1.0, scalar2=None, op0=ALU.add), writes=[R])
        S.op("dve", lambda e: e.reciprocal(out=den[:], in_=den[:]), writes=[R])
        S.op("dve", lambda e: e.tensor_tensor(out=GS[:, :, 0], in0=den[:], in1=ptg[:], op=ALU.mult), writes=[R])
        S.op("dve", lambda e: e.tensor_tensor(out=GS[:, :, 1], in0=GS[:, :, 0], in1=rr[:], op=ALU.mult), writes=[R])
        S.op("dve", lambda e: e.tensor_tensor(out=M1[:], in0=LE[:], in1=b3(T8[:, :, 0], 32), op=ALU.is_equal), writes=[R])
        S.op("dve", lambda e: e.tensor_tensor(out=M2[:], in0=LE[:], in1=b3(T8[:, :, 1], 32), op=ALU.is_equal), writes=[R])
        S.op("dve", lambda e: e.tensor_tensor(out=M01[:], in0=M1[:], in1=M2[:], op=ALU.add), writes=[R])
        for t in range(NT):
            S.op("pe", lambda e, t=t: e.matmul(PP[:, t * 32:(t + 1) * 32], lhsT=UTs[:, :], rhs=M01[:, t, :], start=True, stop=(t == 0)),
                 reads=[R, "UTs"], writes=["PP"])
            for t2 in range(t):
                S.op("pe", lambda e, t=t, t2=t2: e.matmul(PP[:, t * 32:(t + 1) * 32], lhsT=onesf[:, :], rhs=M01[:, t2, :], start=False, stop=(t2 == t - 1)),
                     reads=[R, "onesf"], writes=["PP"])
        S.op("dve", lambda e: e.tensor_copy(out=SL[:], in_=PP[:, 0:NT * 32].rearrange("p (t x) -> p t x", t=NT)), writes=["PP", R])
        S.op("dve", lambda e: e.tensor_scalar(out=VL[:], in0=SL[:], scalar1=float(CAP), scalar2=None, op0=ALU.is_lt), writes=[R])
        S.op("dve", lambda e: e.tensor_tensor(out=SL[:], in0=SL[:], in1=ebase[:].unsqueeze(1).to_broadcast([128, NT, 32]), op=ALU.add),
             reads=["ebase"], writes=[R])
        S.op("dve", lambda e: e.tensor_tensor(out=SL[:], in0=SL[:], in1=VL[:], op=ALU.mult), writes=[R])
        S.op("dve", lambda e: e.tensor_scalar(out=SL[:], in0=SL[:], scalar1=float(DUMMY), scalar2=None, op0=ALU.add), writes=[R])
        for kk, MK in ((0, M1), (1, M2)):
            S.op("dve", lambda e, MK=MK: e.tensor_tensor(out=MK[:], in0=MK[:], in1=SL[:], op=ALU.mult), writes=[R])
            S.op("dve", lambda e, MK=MK, kk=kk: e.tensor_reduce(out=DS[:, :, kk], in_=MK[:], axis=AX.X, op=ALU.add), writes=[R])
        S.op("dve", lambda e: e.tensor_copy(out=DI[:], in_=DS[:]), writes=[R])
        S.dma("sp", lambda e: e.dma_start(out=io["dsti"].rearrange("(n p) c -> p n c", p=128), in_=DI[:]), reads=[R], key="dis")
        S.dma("sp", lambda e: e.dma_start(out=io["gsel"].rearrange("(n p) c -> p n c", p=128), in_=GS[:]), reads=[R], key="dfs")
        for t in range(NT):
            p = t % 4
            S.dma("sp", lambda e, p=p, t=t: e.dma_start(out=h2[p][:], in_=io["h2d"][t * 128:(t + 1) * 128, :]),
                  reads=[("h2d", t)], writes=[("h2", p)], key=("h2l", p))
            for kk in range(2):
                S.dma("pool", lambda e, t=t, kk=kk, p=p: e.indirect_dma_start(
                    out=io["xg"][:, :], out_offset=bass.IndirectOffsetOnAxis(ap=DI[:, t, kk:kk + 1], axis=0),
                    in_=h2[p][:, :], in_offset=None), reads=[R, ("h2", p)], key=("scat", (t * 2 + kk) % 8))
        S.finish()


def phase_b2(nc, io):
    with contextlib.ExitStack() as st:
        S = Sched(nc, st)
        sb = lambda name, shape, dt: st.enter_context(nc.sbuf_tensor("b2_" + name, shape, dt))
        ps = lambda name, shape, dt: st.enter_context(nc.psum_tensor("b2_" + name, shape, dt))
        W1 = [sb("W1_%d" % i, [128, 16, 512], BF16) for i in range(2)]
        W2 = [sb("W2_%d" % i, [128, 16, 512], BF16) for i in range(2)]
        W3 = [sb("W3_%d" % i, [128, 4, D], BF16) for i in range(2)]
        ident = sb("ident", [128, 128], BF16)
        xgt = [sb("xgt%d" % i, [128, 2, D], BF16) for i in range(2)]
        xT = [sb("xT%d" % i, [128, 16, 256], BF16) for i in range(2)]
        hT = [sb("hT%d" % i, [128, 4, 256], BF16) for i in range(2)]
        sg = [sb("sg%d" % i, [128, 256], F32) for i in range(2)]
        ysb = [sb("ysb%d" % i, [128, D], F32) for i in range(2)]
        PT = ps("PT", [128, 2048], BF16)
        PGU = [ps("PGU%d" % i, [128, 512], F32) for i in range(4)]
        PD = [ps("PD%d" % i, [128, 512], F32) for i in range(2)]
        S.dma("sp", lambda e: e.dma_start(out=ident[:], in_=io["ident"]), writes=["ident"], key="c_ident")
        gi = 0
        di = 0
        ei = 0
        yi = 0
        for ex in range(NE):
            w = ex % 2
            S.dma("pool", lambda e, w=w, ex=ex: e.dma_start(out=W1[w][:], in_=io["wg"][ex].rearrange("(k p) f -> p k f", p=128)),
                  writes=[("W1", w)], key=("W1", w))
            S.dma("pool", lambda e, w=w, ex=ex: e.dma_start(out=W2[w][:], in_=io["wu"][ex].rearrange("(k p) f -> p k f", p=128)),
                  writes=[("W2", w)], key=("W2", w))
            S.dma("pool", lambda e, w=w, ex=ex: e.dma_start(out=W3[w][:], in_=io["wd"][ex].rearrange("(k p) f -> p k f", p=128)),
                  writes=[("W3", w)], key=("W3", w))
            S.dma("sp", lambda e, w=w, ex=ex: e.dma_start(out=xgt[w][:], in_=io["xg"][ex * CAP:(ex + 1) * CAP, :].rearrange("(n p) d -> p n d", p=128)),
                  writes=[("xgt", w)], key=("xgt", w))
            for n in range(2):
                for k in range(16):
                    S.op("pe", lambda e, k=k, n=n, w=w: e.transpose(out=PT[:, k * 128:(k + 1) * 128], in_=xgt[w][:, n, k * 128:(k + 1) * 128],
                                                                    identity=ident[:]), reads=[("xgt", w), "ident"], writes=["PT"])
                ei += 1
                if ei % 2:
                    S.op("act", lambda e, n=n, w=w: e.copy(out=xT[w][:, :, n * 128:(n + 1) * 128], in_=PT[:].rearrange("p (k t) -> p k t", k=16)),
                         writes=["PT", ("xT", w)])
                else:
                    S.op("dve", lambda e, n=n, w=w: e.tensor_copy(out=xT[w][:, :, n * 128:(n + 1) * 128], in_=PT[:].rearrange("p (k t) -> p k t", k=16)),
                         writes=["PT", ("xT", w)])
            for f in range(4):
                pg, kpg = PGU[gi % 4], ("PGU", gi % 4)
                gi += 1
                pu, kpu = PGU[gi % 4], ("PGU", gi % 4)
                gi += 1
                for k in range(16):
                    S.op("pe", lambda e, k=k, f=f, pg=pg, w=w: e.matmul(pg[:, 0:256], lhsT=W1[w][:, k, f * 128:(f + 1) * 128], rhs=xT[w][:, k, :],
                                                                        start=(k == 0), stop=(k == 15)),
                         reads=[("W1", w), ("xT", w)], writes=[kpg])
                for k in range(16):
                    S.op("pe", lambda e, k=k, f=f, pu=pu, w=w: e.matmul(pu[:, 0:256], lhsT=W2[w][:, k, f * 128:(f + 1) * 128], rhs=xT[w][:, k, :],
                                                                        start=(k == 0), stop=(k == 15)),
                         reads=[("W2", w), ("xT", w)], writes=[kpu])
                sgb, ksg = sg[f % 2], ("sg", f % 2)
                S.op("act", lambda e, pg=pg, sgb=sgb: e.activation(out=sgb[:], in_=pg[:, 0:256], func=AF.Silu), writes=[kpg, ksg])
                S.op("dve", lambda e, pu=pu, sgb=sgb, w=w, f=f: e.tensor_tensor(out=hT[w][:, f, :], in0=pu[:, 0:256], in1=sgb[:], op=ALU.mult),
                     reads=[ksg], writes=[kpu, ("hT", w)])
            for n in range(2):
                yb, kyb = ysb[yi % 2], ("ysb", yi % 2)
                yi += 1
                for cg in range(4):
                    pd, kpd = PD[di % 2], ("PD", di % 2)
                    di += 1
                    for f in range(4):
                        S.op("pe", lambda e, f=f, n=n, cg=cg, pd=pd, w=w: e.matmul(
                            pd[:], lhsT=hT[w][:, f, n * 128:(n + 1) * 128], rhs=W3[w][:, f, cg * 512:(cg + 1) * 512],
                            start=(f == 0), stop=(f == 3)), reads=[("hT", w), ("W3", w)], writes=[kpd])
                    if cg % 2:
                        S.op("act", lambda e, pd=pd, yb=yb, cg=cg: e.copy(out=yb[:, cg * 512:(cg + 1) * 512], in_=pd[:]), writes=[kpd, kyb])
                    else:
                        S.op("dve", lambda e, pd=pd, yb=yb, cg=cg: e.tensor_copy(out=yb[:, cg * 512:(cg + 1) * 512], in_=pd[:]), writes=[kpd, kyb])
                S.dma("act", lambda e, yb=yb, ex=ex, n=n: e.dma_start(out=io["yg"][ex * CAP + n * 128:ex * CAP + (n + 1) * 128, :], in_=yb[:]),
                      reads=[kyb], key=kyb)
        S.finish()


def phase_b3(nc, io):
    with contextlib.ExitStack() as st:
        S = Sched(nc, st)
        sb = lambda name, shape, dt: st.enter_context(nc.sbuf_tensor("b3_" + name, shape, dt))
        y1 = [sb("y1_%d" % i, [128, D], F32) for i in range(2)]
        y2 = [sb("y2_%d" % i, [128, D], F32) for i in range(2)]
        xm = [sb("xm%d" % i, [128, D], F32) for i in range(2)]
        yt = [sb("yt%d" % i, [128, D], F32) for i in range(2)]
        junk = sb("junk", [128, D], BF16)
        nwb = sb("nwb", [128, D], F32)
        epsb = sb("epsb", [128, 1], F32)
        gs = sb("gs", [128, 16, 2], F32)
        di_ = sb("di", [128, 16, 2], I32)
        stt = [sb("stt%d" % i, [128, 4], F32) for i in range(2)]
        S.dma("sp", lambda e: e.dma_start(out=nwb[:], in_=io["nwf"][0:1, :].partition_broadcast(128)), writes=["nwb"], key="c_nwb")
        S.dma("sp", lambda e: e.dma_start(out=gs[:], in_=io["gsel"].rearrange("(n p) c -> p n c", p=128)), writes=["gs"], key="c_gs")
        S.dma("sp", lambda e: e.dma_start(out=di_[:], in_=io["dsti"].rearrange("(n p) c -> p n c", p=128)), writes=["di"], key="c_di")
        S.op("dve", lambda e: e.memset(epsb[:], EPS), writes=["epsb"])
        for t in range(TB // 128):
            p = t % 2
            r0 = t * 128
            S.dma("sp", lambda e, p=p, r0=r0: e.dma_start(out=xm[p][:], in_=io["xmid"][r0:r0 + 128, :]), writes=[("xm", p)], key=("xm", p))
            for kk, yy in ((0, y1), (1, y2)):
                S.dma("pool", lambda e, p=p, t=t, kk=kk, yy=yy: e.indirect_dma_start(
                    out=yy[p][:, :], out_offset=None, in_=io["yg"][:, :],
                    in_offset=bass.IndirectOffsetOnAxis(ap=di_[:, t, kk:kk + 1], axis=0)),
                    reads=["di"], writes=[("y", kk, p)], key=("y", kk, p))
            S.op("dve", lambda e, p=p, t=t: e.scalar_tensor_tensor(out=xm[p][:], in0=y1[p][:], scalar=gs[:, t, 0:1], in1=xm[p][:],
                                                                  op0=ALU.mult, op1=ALU.add),
                 reads=[("y", 0, p), "gs"], writes=[("xm", p)])
            S.op("dve", lambda e, p=p, t=t: e.scalar_tensor_tensor(out=xm[p][:], in0=y2[p][:], scalar=gs[:, t, 1:2], in1=xm[p][:],
                                                                   op0=ALU.mult, op1=ALU.add),
                 reads=[("y", 1, p), "gs"], writes=[("xm", p)])
            sp_, ks = stt[p], ("stt", p)
            S.op("act", lambda e, p=p, sp_=sp_: e.activation(out=junk[:], in_=xm[p][:], func=AF.Square, accum_out=sp_[:, 0:1]),
                 reads=[("xm", p)], writes=["junk", ks])
            S.op("act", lambda e, sp_=sp_: e.activation(out=sp_[:, 1:2], in_=sp_[:, 0:1], func=AF.Sqrt, bias=epsb[:, 0:1], scale=1.0 / D),
                 reads=["epsb"], writes=[ks])
            S.op("dve", lambda e, sp_=sp_: e.reciprocal(out=sp_[:, 2:3], in_=sp_[:, 1:2]), writes=[ks])
            S.op("dve", lambda e, p=p, sp_=sp_: e.scalar_tensor_tensor(out=yt[p][:], in0=xm[p][:], scalar=sp_[:, 2:3], in1=nwb[:],
                                                                      op0=ALU.mult, op1=ALU.mult),
                 reads=[("xm", p), ks, "nwb"], writes=[("yt", p)])
            S.dma("act", lambda e, p=p, r0=r0: e.dma_start(out=io["y"][r0:r0 + 128, :], in_=yt[p][:]), reads=[("yt", p)], key=("yt", p))
        S.finish()


def phase_exchange(nc, io, Wo_t, wsem):
    sem = nc.alloc_semaphore("cc_sem")
    with nc.Block() as block:
        @block.gpsimd
        def _(e):
            for k in range(16):
                e.dma_start(out=Wo_t[:, k, :], in_=io["w_out"][k * 128:(k + 1) * 128, :]).then_inc(wsem, 16)
            for q in range(4):
                e.collective_compute("AllGather", ALU.bypass, replica_groups=[[0, 1], [2, 3], [4, 5], [6, 7]],
                                     ins=[io["mix_t"][q * 1024:(q + 1) * 1024, :].opt()],
                                     outs=[io["mixg_t"][q * 2048:(q + 1) * 2048, :].opt()]).then_inc(sem)
            e.wait_ge(sem, 4)


def build_program(upto="all"):
    nc = bass.Bass("TRN2", target_bir_lowering=False)
    io = {}

    def inp(name, shape, dt):
        io[name] = nc.dram_tensor(name, list(shape), dt, kind="ExternalInput").ap()

    def scr(name, shape, dt, out=False):
        io[name] = nc.dram_tensor(name, list(shape), dt, kind="ExternalOutput" if out else "Internal").ap()

    inp("x_b", [T, D], F32)
    inp("nw1", [1, D], F32)
    inp("w_in", [D, WCOLS], F32)
    inp("ident", [128, 128], BF16)
    dbg = upto != "all"
    scr("gqT", [NH, 128, T], F32, dbg)
    scr("gkT", [NH, 128, T], F32, dbg)
    scr("gvT", [NH, 128, T], F32, dbg)
    scr("mqT", [NH, 128, T], BF16, dbg)
    scr("mkT", [NH, 128, T], BF16, dbg)
    scr("gz", [T, 512], F32, dbg)
    scr("mv", [T, 512], BF16, dbg)
    scr("gba", [T, 8], F32, dbg)
    inp("pastneg", [128, 512], F32)
    inp("past01", [128, 512], F32)
    inp("abias", [128, 128], F32)
    inp("cmask", [128, 2, 256], BF16)
    inp("nwm", [1, 128], F32)
    inp("nwg", [1, 128], F32)
    inp("conv_w", [128, 12, 4], F32)
    inp("identf", [128, 128], F32)
    inp("maskSL", [64, 64], F32)
    inp("maskUI", [64, 64], F32)
    inp("gcon", [64, 8], F32)
    if dbg:
        scr("mix", [T, 1024], BF16, True)
    else:
        io["mix_t"] = nc.dram_tensor("mix", [T, 1024], BF16)
        io["mixg_t"] = nc.dram_tensor("mixg", [2 * T, 1024], BF16)
        io["mix"] = io["mix_t"].ap()
        io["mixg"] = io["mixg_t"].ap()
    phase_a1(nc, io, dbg)
    if upto == "a1":
        return nc
    if upto == "moba":
        phase_moba(nc, io)
        return nc
    if upto == "gdn":
        phase_gdn2(nc, io)
        return nc
    if upto == "mixers":
        phase_mixers(nc, io)
        return nc
    io["xg"] = nc.dram_tensor("xg", [NE * CAP + 128, D], BF16, kind="Internal").ap()
    phase_moba(nc, io, zero_xg=True)
    phase_gdn2(nc, io)
    if upto == "gdn":
        return nc
    inp("w_out", [D, D], F32)
    inp("xc", [TB, D], F32)
    inp("rowidx", [128, 32], I32)
    inp("nw2", [1, D], F32)
    inp("wr", [D, 36], F32)
    inp("br", [1, 36], F32)
    inp("wg", [NE, D, 512], F32)
    inp("wu", [NE, D, 512], F32)
    inp("wd", [NE, 512, D], F32)
    inp("nwf", [1, D], F32)
    io["xmid"] = nc.dram_tensor("xmid", [TB, D], F32, kind="Internal").ap()
    inp("ebase", [128, 32], F32)
    inp("uts", [128, 128], F32)
    io["h2d"] = nc.dram_tensor("h2d", [TB, D], BF16, kind="Internal").ap()
    io["yg"] = nc.dram_tensor("yg", [NE * CAP + 128, D], F32, kind="Internal").ap()
    io["gsel"] = nc.dram_tensor("gsel", [TB, 2], F32, kind="Internal").ap()
    io["dsti"] = nc.dram_tensor("dsti", [TB, 2], I32, kind="Internal").ap()
    io["y"] = nc.dram_tensor("y", [TB, D], F32, kind="ExternalOutput").ap()
    with nc.sbuf_tensor("b1_Wo", [128, 16, D], BF16) as Wo_t:
        wsem = nc.alloc_semaphore("wo_sem")
        phase_exchange(nc, io, Wo_t, wsem)
        io["Wo_pre"] = (Wo_t, wsem)
        phase_b1(nc, io)
    phase_b2(nc, io)
    phase_b3(nc, io)
    return nc


def core_inputs(inputs, c):
    b, hh = divmod(c, 2)
    w_in = inputs["w_in"][0]
    hs = slice(hh * 512, hh * 512 + 512)
    G = 1024
    cols = [w_in[:, 0 * G:1 * G][:, hs], w_in[:, 1 * G:2 * G][:, hs], w_in[:, 2 * G:3 * G][:, hs],
            w_in[:, 4 * G + 16 + 0 * G:4 * G + 16 + 1 * G][:, hs], w_in[:, 4 * G + 16 + 1 * G:4 * G + 16 + 2 * G][:, hs],
            w_in[:, 3 * G:4 * G][:, hs], w_in[:, 4 * G + 16 + 2 * G:4 * G + 16 + 3 * G][:, hs],
            w_in[:, 4 * G + hh * 4:4 * G + hh * 4 + 4], w_in[:, 4 * G + 8 + hh * 4:4 * G + 8 + hh * 4 + 4]]
    m = {
        "x_b": np.ascontiguousarray(inputs["x"][b]),
        "nw1": np.ascontiguousarray(inputs["norm_mix_w"].reshape(1, D)),
        "w_in": np.ascontiguousarray(np.concatenate(cols, axis=1)),
        "ident": np.eye(128, dtype=ml_dtypes.bfloat16),
        "nwm": np.ascontiguousarray(inputs["moba_out_norm_w"].reshape(1, 128)),
        "nwg": np.ascontiguousarray(inputs["gdn_out_norm_w"].reshape(1, 128)),
        "conv_w": np.ascontiguousarray(inputs["gdn_conv_w"][0].reshape(4, 3, 8, 128)[:, :, hh * 4:hh * 4 + 4, :].transpose(3, 1, 2, 0).reshape(128, 12, 4)),
        "gcon": np.ascontiguousarray(np.broadcast_to(np.concatenate([inputs["gdn_A_log"][0, hh * 4:hh * 4 + 4], inputs["gdn_dt_bias"][0, hh * 4:hh * 4 + 4]])[None, :], (64, 8))).astype(np.float32),
    }
    m.update(consts(hh))
    return m


_CONSTS = {}


def consts(hh):
    if hh in _CONSTS:
        return _CONSTS[hh]
    p = np.arange(128)
    tile = np.arange(32)
    j = np.arange(16)
    past = (j[None, :] < (tile[:, None] // 2))
    past01 = np.broadcast_to(past.astype(np.float32).reshape(1, 512), (128, 512)).copy()
    pastneg = ((past01 - 1.0) * 1e30).astype(np.float32)
    slopes = 2.0 ** (-8.0 * np.arange(1, 9) / 8.0)
    ab = np.zeros((128, 4, 16, 2), np.float32)
    for h in range(4):
        for dl in range(16):
            for kt in range(2):
                ab[:, h, dl, kt] = slopes[hh * 4 + h] * (-dl * 256 + kt * 128 + p - 128)
    cm = np.zeros((128, 2, 256), np.float32)
    q = np.arange(256)
    for kt in range(2):
        cm[:, kt, :] = ((kt * 128 + p)[:, None] <= q[None, :])
    i64 = np.arange(64)
    c = {"identf": np.eye(128, dtype=np.float32),
         "maskSL": (i64[:, None] > i64[None, :]).astype(np.float32),
         "maskUI": (i64[:, None] <= i64[None, :]).astype(np.float32),
         "pastneg": pastneg, "past01": past01, "abias": ab.reshape(128, 128),
         "cmask": cm.astype(ml_dtypes.bfloat16)}
    _CONSTS[hh] = c
    return c


def kernel(**inputs):
    inputs = {k: np.asarray(v) for k, v in inputs.items()}
    nc = build_program()
    w_out = inputs["w_out"][0]
    shared = {
        "w_out": np.ascontiguousarray(np.concatenate([w_out[0:512], w_out[1024:1536], w_out[512:1024], w_out[1536:2048]], axis=0)),
        "nw2": np.ascontiguousarray(inputs["norm_ffn_w"].reshape(1, D)),
        "wr": np.ascontiguousarray(np.concatenate([inputs["w_router_group"][0], inputs["w_router_expert"][0]], axis=1)),
        "br": np.ascontiguousarray(np.concatenate([inputs["b_router_group"][0], inputs["b_router_expert"][0]]).reshape(1, 36)),
        "wg": np.ascontiguousarray(inputs["w_expert_gate"][0]),
        "wu": np.ascontiguousarray(inputs["w_expert_up"][0]),
        "wd": np.ascontiguousarray(inputs["w_expert_down"][0]),
        "nwf": np.ascontiguousarray(inputs["norm_final_w"].reshape(1, D)),
        "ebase": np.ascontiguousarray(np.broadcast_to((np.arange(NE) * CAP - DUMMY).astype(np.float32)[None, :], (128, NE))),
        "uts": (np.arange(128)[:, None] < np.arange(128)[None, :]).astype(np.float32),
    }
    in_maps = []
    for c in range(8):
        b, hh = divmod(c, 2)
        m = core_inputs(inputs, c)
        m.update(shared)
        m["xc"] = np.ascontiguousarray(inputs["x"][b, hh * TB:(hh + 1) * TB])
        p = np.arange(128)[:, None]
        t = np.arange(16)[None, :]
        ri = np.zeros((128, 16, 2), np.int32)
        for r in range(2):
            ri[:, :, r] = (2 * hh + t // 8) * 2048 + r * 1024 + (t % 8) * 128 + p
        m["rowidx"] = ri.reshape(128, 32)
        in_maps.append(m)
    res = run_bass_kernel_spmd(nc, in_maps, core_ids=list(range(8)))
    y = np.stack([np.asarray(res.results[c]["y"]) for c in range(8)], axis=0)
    return y.reshape(4, T, D).astype(np.float32)
```

```python
import contextlib
import numpy as np
import ml_dtypes
import concourse.bass as bass
import concourse.mybir as mybir
from concourse.bass_utils import run_bass_kernel_spmd

F32 = mybir.dt.float32
BF16 = mybir.dt.bfloat16
I32 = mybir.dt.int32
AF = mybir.ActivationFunctionType
ALU = mybir.AluOpType
AX = mybir.AxisListType

D = 2048
T = 4096
NH = 4
HD = 128
NFM = 20
TM0 = NFM * 128
WCOLS = TM0 + 512 + 512 + 8
EPS = 1e-6


class Sched:
    ENGS = ("pe", "act", "dve", "pool", "sp")

    _G = {}

    def __init__(self, nc, stack):
        self.nc = nc
        self.stack = stack
        self.ops = {e: [] for e in self.ENGS}
        g = Sched._G.get(id(nc))
        if g is None:
            g = {"csem": {e: nc.alloc_semaphore("c_%s" % e) for e in self.ENGS}, "cnt": {e: 0 for e in self.ENGS},
                 "seen": {e: {} for e in self.ENGS}, "pool": []}
            Sched._G.clear()
            Sched._G[id(nc)] = g
        self.g = g
        self.csem = g["csem"]
        self.cnt = g["cnt"]
        self.seen = g["seen"]
        self.last_w = {}
        self.readers = {}
        self.dsem = {}
        self.all_dma_events = {}
        self._cap = None

    def capture(self):
        self._cap = []

    def end_capture(self):
        c, self._cap = self._cap, None
        return c

    def replay(self, items):
        for kind, eng, fn, reads, writes, key in items:
            if kind == "op":
                self.op(eng, fn, reads, writes)
            else:
                self.dma(eng, fn, reads, writes, key)

    def _waits(self, eng, reads, writes):
        evs = []
        for b in reads:
            if b in self.last_w:
                evs.append(self.last_w[b])
        for b in writes:
            if b in self.last_w:
                evs.append(self.last_w[b])
            evs.extend(self.readers.get(b, ()))
        need = {}
        for sem, val in evs:
            if eng == "pe" and sem is self.csem["pe"]:
                continue
            if self.seen[eng].get(sem, 0) < val:
                need[sem] = max(need.get(sem, 0), val)
        for sem, val in need.items():
            self.seen[eng][sem] = val
        return list(need.items())

    def _record(self, ev, reads, writes):
        for b in reads:
            self.readers.setdefault(b, []).append(ev)
        for b in writes:
            self.last_w[b] = ev
            self.readers[b] = []

    def op(self, eng, fn, reads=(), writes=()):
        if self._cap is not None:
            self._cap.append(("op", eng, fn, tuple(reads), tuple(writes), None))
            return
        waits = self._waits(eng, reads, writes)
        self.cnt[eng] += 1
        ev = (self.csem[eng], self.cnt[eng])
        sem = self.csem[eng]

        def emit(e):
            for s, v in waits:
                e.wait_ge(s, v)
            fn(e).then_inc(sem, 1)
        self.ops[eng].append(emit)
        self._record(ev, reads, writes)

    def dma(self, eng, fn, reads=(), writes=(), key=None):
        if self._cap is not None:
            self._cap.append(("dma", eng, fn, tuple(reads), tuple(writes), key))
            return
        waits = self._waits(eng, reads, writes)
        if key not in self.dsem:
            kind = "sw" if eng == "pool" else "hw"
            pool = self.g.setdefault("pool_" + kind, [])
            i = sum(1 for k_ in self.dsem.values() if k_[2] == kind)
            if i >= len(pool):
                pool.append([self.nc.alloc_semaphore("d%s%d" % (kind, i)), 0, kind])
            self.dsem[key] = pool[i]
        ent = self.dsem[key]
        ent[1] += 16
        sem = ent[0]
        ev = (sem, ent[1])
        self.all_dma_events[sem] = ev

        def emit(e):
            for s, v in waits:
                e.wait_ge(s, v)
            fn(e).then_inc(sem, 16)
        self.ops[eng].append(emit)
        self._record(ev, reads, writes)

    def finish(self):
        finals = list(self.all_dma_events.values()) + [(self.csem[e], self.cnt[e]) for e in self.ENGS if self.cnt[e]]
        for eng in self.ENGS:
            waits = [(s, v) for s, v in finals if self.seen[eng].get(s, 0) < v]

            def emit(e, waits=waits):
                for s, v in waits:
                    e.wait_ge(s, v)
            self.ops[eng].append(emit)
        with self.nc.Block() as block:
            @block.tensor
            def _(e):
                for f in self.ops["pe"]:
                    f(e)

            @block.scalar
            def _(e):
                for f in self.ops["act"]:
                    f(e)

            @block.vector
            def _(e):
                for f in self.ops["dve"]:
                    f(e)

            @block.gpsimd
            def _(e):
                for f in self.ops["pool"]:
                    f(e)

            @block.sync
            def _(e):
                for f in self.ops["sp"]:
                    f(e)


def merge_prop(A, B):
    out = []
    i = j = 0
    while i < len(A) or j < len(B):
        if j >= len(B) or (i < len(A) and i * len(B) <= j * len(A)):
            out.append(A[i])
            i += 1
        else:
            out.append(B[j])
            j += 1
    return out


def phase_a1(nc, io, dbg):
    x = io["x_b"]
    with contextlib.ExitStack() as st:
        S = Sched(nc, st)
        sb = lambda name, shape, dt: st.enter_context(nc.sbuf_tensor("a1_" + name, shape, dt))
        W = sb("W", [128, 16, WCOLS], BF16)
        wbc = sb("wbc", [128, D], F32)
        ident = sb("ident", [128, 128], BF16)
        xt = [sb("xt%d" % i, [128, D], F32) for i in range(2)]
        hn = [sb("hn%d" % i, [128, D], BF16) for i in range(2)]
        hT = [sb("hT%d" % i, [128, 16, 512], BF16) for i in range(2)]
        junk = sb("junk", [128, D], BF16)
        stat = [sb("stat%d" % i, [128, 4], F32) for i in range(2)]
        stg = [sb("stg%d" % i, [128, 512], F32) for i in range(4)]
        stgb = [sb("stgb%d" % i, [128, 512], BF16) for i in range(4)]
        stgs = [sb("stgs%d" % i, [128, 8], F32) for i in range(2)]
        PT = [st.enter_context(nc.psum_tensor("a1_PT%d" % i, [128, 2048], BF16)) for i in range(1)]
        PM = [st.enter_context(nc.psum_tensor("a1_PM%d" % i, [128, 512], F32)) for i in range(4)]

        S.dma("sp", lambda e: e.dma_start(out=wbc[:], in_=io["nw1"][0:1, :].partition_broadcast(128)),
              writes=["wbc"], key="c_wbc")
        S.dma("sp", lambda e: e.dma_start(out=ident[:], in_=io["ident"]), writes=["ident"], key="c_ident")
        epsb = sb("epsb", [128, 1], F32)
        S.op("dve", lambda e: e.memset(epsb[:], EPS), writes=["epsb"])
        for k in range(16):
            S.dma("pool", lambda e, k=k: e.dma_start(out=W[:, k, :], in_=io["w_in"][k * 128:(k + 1) * 128, :]),
                  writes=["W"], key="W")

        evac_i = [0]

        def evac(out_ap, in_ap, reads, writes):
            writes = list(writes) + list(reads)
            reads = []
            evac_i[0] += 1
            if evac_i[0] % 2:
                S.op("act", lambda e: e.copy(out=out_ap, in_=in_ap), reads=reads, writes=writes)
            else:
                S.op("dve", lambda e: e.tensor_copy(out=out_ap, in_=in_ap), reads=reads, writes=writes)

        mi = 0
        S1, S2 = [], []
        for st_i in range(T // 512):
            hTb = hT[st_i % 2]
            hTk = ("hT", st_i % 2)
            S.capture()
            for tt in range(4):
                ti = st_i * 4 + tt
                xb_, hb_, sb_ = xt[ti % 2], hn[ti % 2], stat[ti % 2]
                kx, kh, ks = ("xt", ti % 2), ("hn", ti % 2), ("stat", ti % 2)
                S.dma("sp", lambda e, ti=ti, xb_=xb_: e.dma_start(out=xb_[:], in_=x[ti * 128:(ti + 1) * 128, :]),
                      writes=[kx], key=kx)
                S.op("act", lambda e, xb_=xb_, sb_=sb_: e.activation(out=junk[:], in_=xb_[:], func=AF.Square,
                                                                    accum_out=sb_[:, 0:1]),
                     reads=[kx], writes=["junk", ks])
                S.op("act", lambda e, sb_=sb_: e.activation(out=sb_[:, 1:2], in_=sb_[:, 0:1], func=AF.Sqrt,
                                                           bias=epsb[:, 0:1], scale=1.0 / D),
                     reads=[ks, "epsb"], writes=[ks])
                S.op("dve", lambda e, sb_=sb_: e.reciprocal(out=sb_[:, 2:3], in_=sb_[:, 1:2]),
                     reads=[ks], writes=[ks])
                S.op("dve", lambda e, xb_=xb_, hb_=hb_, sb_=sb_: e.scalar_tensor_tensor(
                    out=hb_[:], in0=xb_[:], scalar=sb_[:, 2:3], in1=wbc[:], op0=ALU.mult, op1=ALU.mult),
                     reads=[kx, ks, "wbc"], writes=[kh])
                for k in range(16):
                    S.op("pe", lambda e, k=k, hb_=hb_: e.transpose(out=PT[0][:, k * 128:(k + 1) * 128],
                                                                   in_=hb_[:, k * 128:(k + 1) * 128],
                                                                   identity=ident[:]),
                         reads=[kh, "ident"], writes=["PT"])
                evac(hTb[:, :, tt * 128:(tt + 1) * 128], PT[0][:].rearrange("p (k t) -> p k t", k=16),
                     reads=["PT"], writes=[hTk])
            S1.append(S.end_capture())
            S.capture()
            for c in range(NFM):
                pm = PM[mi % 4]
                kp = ("PM", mi % 4)
                mi += 1
                for k in range(16):
                    S.op("pe", lambda e, k=k, c=c, pm=pm, hTb=hTb: e.matmul(
                        pm[:], lhsT=W[:, k, c * 128:(c + 1) * 128], rhs=hTb[:, k, :], start=(k == 0), stop=(k == 15)),
                         reads=[hTk, "W"], writes=[kp])
                grp, h = divmod(c, 4)
                dst = [io["gqT"], io["gkT"], io["gvT"], io["mqT"], io["mkT"]][grp]
                if grp < 3:
                    sg = stg[c % 4]
                    ksg = ("stg", c % 4)
                else:
                    sg = stgb[c % 4]
                    ksg = ("stgb", c % 4)
                evac(sg[:], pm[:], reads=[kp], writes=[ksg])
                S.dma("act", lambda e, dst=dst, h=h, sg=sg, st_i=st_i: e.dma_start(
                    out=dst[h, :, st_i * 512:(st_i + 1) * 512], in_=sg[:]), reads=[ksg], key=ksg)
            for tt in range(4):
                ti = st_i * 4 + tt
                for g in range(3):
                    c0 = TM0 + g * 512
                    nco = 512 if g < 2 else 8
                    pm = PM[mi % 4]
                    kp = ("PM", mi % 4)
                    mi += 1
                    for k in range(16):
                        S.op("pe", lambda e, k=k, pm=pm, hTb=hTb, tt=tt, c0=c0, nco=nco: e.matmul(
                            pm[:, 0:nco], lhsT=hTb[:, k, tt * 128:(tt + 1) * 128], rhs=W[:, k, c0:c0 + nco],
                            start=(k == 0), stop=(k == 15)),
                             reads=[hTk, "W"], writes=[kp])
                    if g == 0:
                        sg, ksg, dst = stg[tt % 4], ("stg", tt % 4), io["gz"]
                    elif g == 1:
                        sg, ksg, dst = stgb[tt % 4], ("stgb", tt % 4), io["mv"]
                    else:
                        sg, ksg, dst = stgs[tt % 2], ("stgs", tt % 2), io["gba"]
                    evac(sg[:, 0:nco], pm[:, 0:nco], reads=[kp], writes=[ksg])
                    S.dma("act", lambda e, dst=dst, sg=sg, ti=ti, nco=nco: e.dma_start(
                        out=dst[ti * 128:(ti + 1) * 128, :], in_=sg[:, 0:nco]), reads=[ksg], key=ksg)
            S2.append(S.end_capture())
        order = list(S1[0])
        for st_i in range(len(S2)):
            order.extend(S2[st_i])
            if st_i + 1 < len(S1):
                order.extend(S1[st_i + 1])
        S.replay(order)
        S.finish()


def phase_moba(nc, io, ctx=None, zero_xg=False):
    scale = float(HD) ** -0.5
    with contextlib.ExitStack() as st:
        if ctx is not None:
            st = ctx["st"]
        S = ctx["S"] if ctx is not None else Sched(nc, st)
        sb = lambda name, shape, dt: st.enter_context(nc.sbuf_tensor("mb_" + name, shape, dt))
        ps = lambda name, shape, dt: st.enter_context(nc.psum_tensor("mb_" + name, shape, dt))
        QT = [sb("QT%d" % i, [128, T], BF16) for i in range(2)]
        KT = [sb("KT%d" % i, [128, T], BF16) for i in range(2)]
        V = [sb("V%d" % i, [128, 32, 132], BF16) for i in range(2)]
        pastneg = sb("pastneg", [128, 512], F32)
        past01 = sb("past01", [128, 512], F32)
        abias = sb("abias", [128, 128], F32)
        cmask = sb("cmask", [128, 2, 256], BF16)
        nwb = sb("nwb", [128, 128], F32)
        epsb = sb("epsb", [128, 1], F32)
        ksumL = [sb("ksum%d" % i, [128, 16], F32) for i in range(2)]
        khiL = [sb("khi%d" % i, [128, 16], BF16) for i in range(2)]
        khfL = [sb("khf%d" % i, [128, 16], F32) for i in range(2)]
        kloL = [sb("klo%d" % i, [128, 16], BF16) for i in range(2)]
        gmL = [sb("gm%d" % i, [128, 512], F32) for i in range(2)]
        selL = [sb("sel%d" % i, [128, 512], F32) for i in range(2)]
        top8L = [sb("top8%d" % i, [128, 32, 8], F32) for i in range(2)]
        PTs = [sb("PTs%d" % i, [128, 256], BF16) for i in range(4)]
        acc = [sb("acc%d" % i, [128, 132], F32) for i in range(4)]
        osb = [sb("osb%d" % i, [128, 128], F32) for i in range(2)]
        ojk = sb("ojk", [128, 128], F32)
        ost = [sb("ost%d" % i, [128, 8], F32) for i in range(2)]
        obf = [sb("obf%d" % i, [128, 128], BF16) for i in range(2)]
        if ctx is None:
            PG = ps("PG", [128, 512], F32)
            PS = [ps("PS%d" % i, [128, 512], F32) for i in range(4)]
            PO = [ps("PO%d" % i, [128, 512], F32) for i in range(3)]
            KPG, KPS, KPO = "PG", [("PS", i) for i in range(4)], [("PO", i) for i in range(3)]
        else:
            bk = ctx["moba_banks"]
            PG, KPG = bk[0]
            PS, KPS = [bk[0][0], bk[1][0]], [bk[0][1], bk[1][1]]
            PO, KPO = [bk[2][0]], [bk[2][1]]
        NPS, NPO = len(PS), len(PO)

        S.dma("sp", lambda e: e.dma_start(out=pastneg[:], in_=io["pastneg"]), writes=["pastneg"], key="c_pastneg")
        S.dma("sp", lambda e: e.dma_start(out=past01[:], in_=io["past01"]), writes=["past01"], key="c_past01")
        S.dma("sp", lambda e: e.dma_start(out=abias[:], in_=io["abias"]), writes=["abias"], key="c_abias")
        S.dma("sp", lambda e: e.dma_start(out=cmask[:], in_=io["cmask"]), writes=["cmask"], key="c_cmask")
        S.dma("sp", lambda e: e.dma_start(out=nwb[:], in_=io["nwm"][0:1, :].partition_broadcast(128)),
              writes=["nwb"], key="c_nwbm")
        S.op("dve", lambda e: e.memset(epsb[:], EPS), writes=["epsb"])
        if zero_xg:
            zrow = sb("zrow", [128, D], BF16)
            S.op("pool", lambda e: e.memset(zrow[:], 0.0), writes=["zrow"])
            for zi in range((NE * CAP + 128) // 128):
                S.dma("act", lambda e, zi=zi: e.dma_start(out=io["xg"][zi * 128:(zi + 1) * 128, :], in_=zrow[:]), reads=["zrow"], key="c_xgz")
        si = 0
        oi = 0
        outer_cap = S._cap
        S._cap = None
        P = []
        Useg = []

        def head_body(h):
            nonlocal si, oi
            qt, kt_, v = QT[h % 2], KT[h % 2], V[h % 2]
            ksum, khi, khf, klo, gm, sel, top8 = (x[h % 2] for x in (ksumL, khiL, khfL, kloL, gmL, selL, top8L))
            kksum, kkhi, kkhf, kklo, kgm, ksel, ktop = (("pro", nm_, h % 2) for nm_ in ("ksum", "khi", "khf", "klo", "gm", "sel", "top8"))
            S.capture()
            kq, kk, kv = ("QT", h % 2), ("KT", h % 2), ("V", h % 2)
            S.dma("sp", lambda e, h=h, qt=qt: e.dma_start(out=qt[:], in_=io["mqT"][h]), writes=[kq], key=kq)
            S.dma("sp", lambda e, h=h, kt_=kt_: e.dma_start(out=kt_[:], in_=io["mkT"][h]), writes=[kk], key=kk)
            S.dma("sp", lambda e, h=h, v=v: e.dma_start(
                out=v[:, :, 0:128], in_=io["mv"][:, h * 128:(h + 1) * 128].rearrange("(n p) d -> p n d", p=128)),
                writes=[kv], key=kv)
            S.op("pool", lambda e, v=v: e.memset(v[:, :, 128:129], 1.0), writes=[kv])
            S.op("dve", lambda e, kt_=kt_: e.tensor_reduce(out=ksum[:], in_=kt_[:].rearrange("p (n k) -> p n k", k=256),
                                                          axis=AX.X, op=ALU.add), reads=[kk], writes=[kksum])
            S.op("dve", lambda e: e.tensor_copy(out=khi[:], in_=ksum[:]), reads=[kksum], writes=[kkhi])
            S.op("dve", lambda e: e.tensor_copy(out=khf[:], in_=khi[:]), reads=[kkhi], writes=[kkhf])
            S.op("dve", lambda e: e.tensor_tensor(out=klo[:], in0=ksum[:], in1=khf[:], op=ALU.subtract),
                 reads=[kksum, kkhf], writes=[kklo])
            for t in range(32):
                S.op("pe", lambda e, t=t, qt=qt: e.matmul(PG[:, t * 16:(t + 1) * 16], lhsT=qt[:, t * 128:(t + 1) * 128],
                                                          rhs=khi[:], start=True, stop=False),
                     reads=[kq, kkhi], writes=[KPG])
                S.op("pe", lambda e, t=t, qt=qt: e.matmul(PG[:, t * 16:(t + 1) * 16], lhsT=qt[:, t * 128:(t + 1) * 128],
                                                          rhs=klo[:], start=False, stop=True),
                     reads=[kq, kklo], writes=[KPG])
            S.op("dve", lambda e: e.tensor_tensor(out=gm[:], in0=PG[:], in1=pastneg[:], op=ALU.add),
                 reads=["pastneg"], writes=[kgm, KPG])
            for t in range(32):
                S.op("dve", lambda e, t=t: e.max(out=top8[:, t, :], in_=gm[:, t * 16:(t + 1) * 16]),
                     reads=[kgm], writes=[ktop])
            for t in range(32):
                S.op("dve", lambda e, t=t: e.tensor_scalar(out=sel[:, t * 16:(t + 1) * 16], in0=gm[:, t * 16:(t + 1) * 16],
                                                           scalar1=top8[:, t, 2:3], scalar2=None, op0=ALU.is_ge),
                     reads=[kgm, ktop], writes=[ksel])
            S.op("dve", lambda e: e.tensor_tensor(out=sel[:], in0=sel[:], in1=past01[:], op=ALU.mult),
                 reads=[ksel, "past01"], writes=[ksel])
            P.append(S.end_capture())
            for n in range(16):
                accs = [acc[(2 * n + q) % 4] for q in range(2)]
                kacc = [("acc", (2 * n + q) % 4) for q in range(2)]
                for j in range(n, -1, -1):
                    S.capture()
                    pts = []
                    for kt in range(2):
                        pS = PS[si % NPS]
                        half = 0
                        kps = KPS[si % NPS]
                        pt = PTs[si % 4]
                        kpt = ("PTs", si % 4)
                        si += 1
                        pts.append((pt, kpt))
                        k0 = j * 256 + kt * 128
                        S.op("pe", lambda e, pS=pS, half=half, k0=k0, n=n, kt_=kt_, qt=qt: e.matmul(
                            pS[:, half * 256:(half + 1) * 256], lhsT=kt_[:, k0:k0 + 128], rhs=qt[:, n * 256:(n + 1) * 256],
                            start=True, stop=True), reads=[kk, kq], writes=[kps])
                        bi = h * 32 + (n - j) * 2 + kt
                        S.op("act", lambda e, pS=pS, half=half, pt=pt, bi=bi: e.activation(
                            out=pt[:], in_=pS[:, half * 256:(half + 1) * 256], func=AF.Exp,
                            bias=abias[:, bi:bi + 1], scale=scale), reads=["abias"], writes=[kpt, kps])
                        if j == n:
                            S.op("pool", lambda e, pt=pt, kt=kt: e.tensor_tensor(out=pt[:], in0=pt[:], in1=cmask[:, kt, :],
                                                                                 op=ALU.mult),
                                 reads=[kpt, "cmask"], writes=[kpt])
                    s1 = S.end_capture()
                    S.capture()
                    pos = []
                    for q in range(2):
                        po = PO[oi % NPO]
                        kpo = KPO[oi % NPO]
                        oi += 1
                        pos.append((po, kpo))
                        for kt in range(2):
                            pt, kpt = pts[kt]
                            S.op("pe", lambda e, po=po, q=q, pt=pt, v=v, j=j, kt=kt: e.matmul(
                                po[:, 0:129], lhsT=pt[:, q * 128:(q + 1) * 128], rhs=v[:, j * 2 + kt, 0:129],
                                start=(kt == 0), stop=(kt == 1)), reads=[kpt, kv], writes=[kpo])
                    for q in range(2):
                        tq = 2 * n + q
                        po, kpo = pos[q]
                        if j == n:
                            S.op("dve", lambda e, po=po, q=q, a=accs[q]: e.tensor_copy(out=a[:, 0:129], in_=po[:, 0:129]),
                                 writes=[kacc[q], kpo])
                        else:
                            S.op("dve", lambda e, po=po, q=q, a=accs[q], tq=tq, j=j: e.scalar_tensor_tensor(
                                out=a[:, 0:129], in0=po[:, 0:129], scalar=sel[:, tq * 16 + j:tq * 16 + j + 1],
                                in1=a[:, 0:129], op0=ALU.mult, op1=ALU.add), reads=[ksel], writes=[kacc[q], kpo])
                    Useg.append([h, s1, S.end_capture()])
                S.capture()
                for q in range(2):
                    tq = 2 * n + q
                    a = accs[q]
                    o_, os_, ob_ = osb[tq % 2], ost[tq % 2], obf[tq % 2]
                    ko, kos, kob = ("osb", tq % 2), ("ost", tq % 2), ("obf", tq % 2)
                    S.op("dve", lambda e, a=a, os_=os_: e.reciprocal(out=os_[:, 0:1], in_=a[:, 128:129]),
                         reads=[kacc[q]], writes=[kos])
                    S.op("dve", lambda e, a=a, os_=os_, o_=o_: e.tensor_scalar(out=o_[:], in0=a[:, 0:128], scalar1=os_[:, 0:1],
                                                                              scalar2=None, op0=ALU.mult),
                         reads=[kacc[q], kos], writes=[ko])
                    S.op("act", lambda e, o_=o_, os_=os_: e.activation(out=ojk[:], in_=o_[:], func=AF.Square,
                                                                      accum_out=os_[:, 1:2]),
                         reads=[ko], writes=["ojk", kos])
                    S.op("act", lambda e, os_=os_: e.activation(out=os_[:, 2:3], in_=os_[:, 1:2], func=AF.Sqrt,
                                                               bias=epsb[:, 0:1], scale=1.0 / HD),
                         reads=[kos, "epsb"], writes=[kos])
                    S.op("dve", lambda e, os_=os_: e.reciprocal(out=os_[:, 3:4], in_=os_[:, 2:3]), reads=[kos], writes=[kos])
                    S.op("dve", lambda e, o_=o_, os_=os_, ob_=ob_: e.scalar_tensor_tensor(
                        out=ob_[:], in0=o_[:], scalar=os_[:, 3:4], in1=nwb[:], op0=ALU.mult, op1=ALU.mult),
                         reads=[ko, kos, "nwb"], writes=[kob])
                    S.dma("sp", lambda e, ob_=ob_, tq=tq, h=h: e.dma_start(
                        out=io["mix"][tq * 128:(tq + 1) * 128, 512 + h * 128:512 + (h + 1) * 128], in_=ob_[:]),
                        reads=[kob], key=kob)
                Useg[-1][2].extend(S.end_capture())
        for h_ in range(NH):
            head_body(h_)
        order = list(P[0])
        nunits = {hh: sum(1 for u in Useg if u[0] == hh) for hh in range(NH)}
        ppos = {hh: 0 for hh in range(NH)}
        if Useg:
            order.extend(Useg[0][1])
        for idx, (hh, s1_, s2_) in enumerate(Useg):
            if idx + 1 < len(Useg):
                nh = Useg[idx + 1][0]
                if nh != hh:
                    order.extend(P[nh][ppos[nh]:])
                    ppos[nh] = len(P[nh])
                order.extend(Useg[idx + 1][1])
            order.extend(s2_)
            if hh + 1 < NH and ppos[hh + 1] < len(P[hh + 1]):
                step = -(-len(P[hh + 1]) // max(1, nunits[hh] - 8))
                order.extend(P[hh + 1][ppos[hh + 1]:ppos[hh + 1] + step])
                ppos[hh + 1] += step
        if outer_cap is not None:
            S._cap = outer_cap
            outer_cap.extend(order)
        else:
            S.replay(order)
        if ctx is None:
            S.finish()


def phase_gdn(nc, io):
    C = 64
    NCH = T // C
    with contextlib.ExitStack() as st:
        S = Sched(nc, st)
        sb = lambda name, shape, dt: st.enter_context(nc.sbuf_tensor("gd_" + name, shape, dt))
        PB = [st.enter_context(nc.psum_tensor("gd_P%d" % i, [128, 512], F32)) for i in range(8)]
        pbi = [0]

        def bank():
            i = pbi[0] % 8
            pbi[0] += 1
            return PB[i], ("PB", i)

        raw = sb("raw", [128, T + 3], F32)
        cv = [sb("cv%d" % i, [128, T], F32) for i in range(3)]
        tmpf = sb("tmpf", [128, T], F32)
        cw = sb("cw", [128, 12, 4], F32)
        identf = sb("identf", [128, 128], F32)
        ones = sb("ones", [128, 128], F32)
        maskSL = sb("maskSL", [64, 64], F32)
        maskUI = sb("maskUI", [64, 64], F32)
        Utri = sb("Utri", [64, 64], F32)
        gcon = sb("gcon", [64, 8], F32)
        nwb = sb("nwb", [64, 128], F32)
        c1 = sb("c1", [128, 1], F32)
        cq = sb("cq", [128, 1], F32)
        ck = sb("ck", [128, 1], F32)
        ce = sb("ce", [128, 1], F32)
        gba = sb("gba", [64, NCH, 8], F32)
        zt = sb("zt", [64, NCH, 128], F32)
        obuf = sb("obuf", [64, NCH, 128], BF16)
        col = {nm: sb("col_" + nm, [64, NCH], F32) for nm in ("beta", "nbeta", "gl", "g", "eg", "egl", "beg", "tmp", "tmp2")}
        gt = sb("gt", [128, NCH], F32)
        Sst = sb("Sst", [128, 128], F32)
        dg = sb("dg", [64, 64], F32)
        t64 = [sb("t64_%d" % i, [64, 64], F32) for i in range(4)]
        E1 = sb("E1", [64, 64], F32)
        E2 = sb("E2", [64, 64], F32)
        Am = [sb("Am%d" % i, [64, 64], F32) for i in range(2)]
        Bm = [sb("Bm%d" % i, [64, 64], F32) for i in range(2)]
        X = sb("X", [64, 64], F32)
        kbe = sb("kbe", [64, 128], F32)
        kdec = sb("kdec", [64, 128], F32)
        vb = sb("vb", [64, 128], F32)
        wTn = sb("wTn", [128, 64], F32)
        attnT = sb("attnT", [64, 64], F32)
        vnew = sb("vnew", [64, 128], F32)
        oq = sb("oq", [64, 128], F32)
        osb = sb("osb", [64, 128], F32)
        ojk = sb("ojk", [64, 128], F32)
        ost = sb("ost", [64, 4], F32)

        def cdma(dst, src, key):
            S.dma("sp", lambda e: e.dma_start(out=dst, in_=src), writes=[key], key="const")
        cdma(cw[:], io["conv_w"], "cw")
        cdma(identf[:], io["identf"], "identf")
        cdma(maskSL[:], io["maskSL"], "maskSL")
        cdma(maskUI[:], io["maskUI"], "maskUI")
        cdma(Utri[:], io["maskUI"], "Utri")
        cdma(gcon[:], io["gcon"], "gcon")
        cdma(nwb[:], io["nwg"][0:1, :].partition_broadcast(64), "nwb")
        cdma(gba[:], io["gba"].rearrange("(n p) c -> p n c", p=C), "gba")
        S.op("dve", lambda e: e.memset(ones[:], 1.0), writes=["ones"])
        S.op("dve", lambda e: e.memset(c1[:], 1.0), writes=["c1"])
        S.op("dve", lambda e: e.memset(cq[:], 128.0 * 1e-6), writes=["cq"])
        S.op("dve", lambda e: e.memset(ck[:], 1e-6), writes=["ck"])
        S.op("dve", lambda e: e.memset(ce[:], EPS), writes=["ce"])
        S.op("dve", lambda e: e.memset(raw[:, 0:3], 0.0), writes=["rawpad"])
        S.op("act", lambda e: e.activation(out=gcon[:, 0:4], in_=gcon[:, 0:4], func=AF.Exp), reads=["gcon"], writes=["gcon"])

        for h in range(NH):
            for ti, (src, nm) in enumerate(((io["gqT"], "q"), (io["gkT"], "k"), (io["gvT"], "v"))):
                S.dma("sp", lambda e, src=src, h=h: e.dma_start(out=raw[:, 3:], in_=src[h]), reads=["rawpad"], writes=["raw"], key="raw")
                cvt = cv[ti]
                kc = ("cv", ti)
                wi = ti * 4 + h
                S.op("dve", lambda e, cvt=cvt, wi=wi: e.tensor_scalar(out=cvt[:], in0=raw[:, 3:3 + T], scalar1=cw[:, wi, 3:4],
                                                                     scalar2=None, op0=ALU.mult), reads=["raw", "cw"], writes=[kc])
                for j in (2, 1, 0):
                    S.op("dve", lambda e, cvt=cvt, wi=wi, j=j: e.scalar_tensor_tensor(
                        out=cvt[:], in0=raw[:, j:j + T], scalar=cw[:, wi, j:j + 1], in1=cvt[:], op0=ALU.mult, op1=ALU.add),
                         reads=["raw", "cw", kc], writes=[kc])
                S.op("act", lambda e, cvt=cvt: e.activation(out=cvt[:], in_=cvt[:], func=AF.Silu), reads=[kc], writes=[kc])
                if ti < 2:
                    S.op("act", lambda e, cvt=cvt: e.activation(out=tmpf[:], in_=cvt[:], func=AF.Square), reads=[kc], writes=["tmpf"])
                    for g8 in range(T // 512):
                        pb, kb = bank()
                        S.op("pe", lambda e, pb=pb, g8=g8: e.matmul(pb[:, :], lhsT=ones[:, :], rhs=tmpf[:, g8 * 512:(g8 + 1) * 512],
                                                                    start=True, stop=True), reads=["ones", "tmpf"], writes=[kb])
                        if ti == 0:
                            S.op("act", lambda e, pb=pb, g8=g8: e.activation(out=raw[:, 3 + g8 * 512:3 + (g8 + 1) * 512], in_=pb[:, :],
                                                                            func=AF.Sqrt, bias=cq[:, 0:1], scale=128.0),
                                 reads=["cq"], writes=[kb, "raw"])
                        else:
                            S.op("act", lambda e, pb=pb, g8=g8: e.activation(out=raw[:, 3 + g8 * 512:3 + (g8 + 1) * 512], in_=pb[:, :],
                                                                            func=AF.Sqrt, bias=ck[:, 0:1], scale=1.0),
                                 reads=["ck"], writes=[kb, "raw"])
                    S.op("dve", lambda e: e.reciprocal(out=tmpf[:], in_=raw[:, 3:3 + T]), reads=["raw"], writes=["tmpf"])
                    S.op("dve", lambda e, cvt=cvt: e.tensor_tensor(out=cvt[:], in0=cvt[:], in1=tmpf[:], op=ALU.mult),
                         reads=[kc, "tmpf"], writes=[kc])
            qn, kn, vn = cv
            cb, cnb, cgl, cg, ceg, cegl, cbeg, ctmp, ctmp2 = (col[k_] for k_ in ("beta", "nbeta", "gl", "g", "eg", "egl", "beg", "tmp", "tmp2"))
            S.op("act", lambda e, h=h: e.activation(out=cb[:], in_=gba[:, :, h], func=AF.Sigmoid), reads=["gba"], writes=["c_beta"])
            S.op("dve", lambda e: e.tensor_scalar(out=cnb[:], in0=cb[:], scalar1=-1.0, scalar2=None, op0=ALU.mult),
                 reads=["c_beta"], writes=["c_nbeta"])
            S.op("act", lambda e, h=h: e.activation(out=ctmp[:], in_=gba[:, :, 4 + h], func=AF.Exp, bias=gcon[:, 4 + h:5 + h], scale=1.0),
                 reads=["gba", "gcon"], writes=["c_tmp"])
            S.op("act", lambda e: e.activation(out=ctmp[:], in_=ctmp[:], func=AF.Ln, bias=c1[0:64, 0:1], scale=1.0),
                 reads=["c_tmp", "c1"], writes=["c_tmp"])
            S.op("dve", lambda e, h=h: e.tensor_scalar(out=cgl[:], in0=ctmp[:], scalar1=gcon[:, h:h + 1], scalar2=-1.0,
                                                      op0=ALU.mult, op1=ALU.mult), reads=["c_tmp", "gcon"], writes=["c_gl"])
            pb, kb = bank()
            S.op("pe", lambda e, pb=pb: e.matmul(pb[0:64, 0:NCH], lhsT=Utri[:, :], rhs=cgl[:, :], start=True, stop=True),
                 reads=["Utri", "c_gl"], writes=[kb])
            S.op("dve", lambda e, pb=pb: e.tensor_copy(out=cg[:], in_=pb[0:64, 0:NCH]), writes=[kb, "c_g"])
            pb, kb = bank()
            S.op("pe", lambda e, pb=pb: e.matmul(pb[:, 0:NCH], lhsT=ones[0:64, :], rhs=cgl[:, :], start=True, stop=True),
                 reads=["ones", "c_gl"], writes=[kb])
            S.op("act", lambda e, pb=pb: e.activation(out=gt[:], in_=pb[:, 0:NCH], func=AF.Exp), writes=[kb, "gt"])
            S.op("dve", lambda e, pb=pb: e.tensor_tensor(out=ctmp2[:], in0=pb[0:64, 0:NCH], in1=cg[:], op=ALU.subtract),
                 reads=["c_g"], writes=[kb, "c_tmp2"])
            S.op("act", lambda e: e.activation(out=cegl[:], in_=ctmp2[:], func=AF.Exp), reads=["c_tmp2"], writes=["c_egl"])
            S.op("act", lambda e: e.activation(out=ceg[:], in_=cg[:], func=AF.Exp), reads=["c_g"], writes=["c_eg"])
            S.op("dve", lambda e: e.tensor_tensor(out=cbeg[:], in0=cb[:], in1=ceg[:], op=ALU.mult),
                 reads=["c_beta", "c_eg"], writes=["c_beg"])
            S.dma("sp", lambda e, h=h: e.dma_start(out=zt[:], in_=io["gz"][:, h * 128:(h + 1) * 128].rearrange("(n p) d -> p n d", p=C)),
                  writes=["zt"], key="zt")
            S.op("act", lambda e: e.activation(out=zt[:], in_=zt[:], func=AF.Silu), reads=["zt"], writes=["zt"])
            for n8 in range(8):
                S.op("pool", lambda e, n8=n8: e.tensor_tensor(out=zt[:, n8 * 8:(n8 + 1) * 8, :], in0=zt[:, n8 * 8:(n8 + 1) * 8, :],
                                                             in1=nwb[:].unsqueeze(1).to_broadcast([64, 8, 128]), op=ALU.mult),
                     reads=["zt", "nwb"], writes=["zt"])
            S.op("dve", lambda e: e.memset(Sst[:], 0.0), writes=["S"])

            for n in range(NCH):
                c0 = n * C
                kT_c, qT_c, vT_c = kn[:, c0:c0 + C], qn[:, c0:c0 + C], vn[:, c0:c0 + C]
                pk, kpk = bank()
                S.op("pe", lambda e, pk=pk, kT_c=kT_c: e.transpose(out=pk[0:64, 0:128], in_=kT_c, identity=identf[:]),
                     reads=[("cv", 1), "identf"], writes=[kpk])
                S.op("dve", lambda e, pk=pk, n=n: e.tensor_scalar(out=kbe[:], in0=pk[0:64, 0:128], scalar1=cbeg[:, n:n + 1], scalar2=None,
                                                                 op0=ALU.mult), reads=["c_beg"], writes=[kpk, "kbe"])
                S.op("act", lambda e, pk=pk, n=n: e.activation(out=kdec[:], in_=pk[0:64, 0:128], func=AF.Copy, scale=cegl[:, n:n + 1]),
                     reads=["c_egl"], writes=[kpk, "kdec"])
                pv, kpv = bank()
                S.op("pe", lambda e, pv=pv, vT_c=vT_c: e.transpose(out=pv[0:64, 0:128], in_=vT_c, identity=identf[:]),
                     reads=[("cv", 2), "identf"], writes=[kpv])
                S.op("act", lambda e, pv=pv, n=n: e.activation(out=vb[:], in_=pv[0:64, 0:128], func=AF.Copy, scale=cb[:, n:n + 1]),
                     reads=["c_beta"], writes=[kpv, "vb"])
                S.op("dve", lambda e, n=n: e.tensor_scalar(out=dg[:], in0=identf[0:64, 0:64], scalar1=cg[:, n:n + 1], scalar2=None,
                                                          op0=ALU.mult), reads=["identf", "c_g"], writes=["dg"])
                pg, kpg = bank()
                S.op("pe", lambda e, pg=pg: e.matmul(pg[0:64, 0:64], lhsT=ones[0:64, 0:64], rhs=dg[:, :], start=True, stop=True),
                     reads=["ones", "dg"], writes=[kpg])
                S.op("dve", lambda e, pg=pg, n=n: e.tensor_scalar(out=t64[0][:], in0=pg[0:64, 0:64], scalar1=cg[:, n:n + 1], scalar2=0.0,
                                                                 op0=ALU.subtract, op1=ALU.max), reads=["c_g"], writes=[kpg, "t0"])
                S.op("dve", lambda e, pg=pg, n=n: e.tensor_scalar(out=t64[1][:], in0=pg[0:64, 0:64], scalar1=cg[:, n:n + 1], scalar2=0.0,
                                                                 op0=ALU.subtract, op1=ALU.min), reads=["c_g"], writes=[kpg, "t1"])
                S.op("act", lambda e: e.activation(out=E1[:], in_=t64[0][:], func=AF.Exp, scale=-1.0), reads=["t0"], writes=["E1"])
                S.op("act", lambda e: e.activation(out=E2[:], in_=t64[1][:], func=AF.Exp), reads=["t1"], writes=["E2"])
                S.op("pool", lambda e: e.tensor_tensor(out=E2[:], in0=E2[:], in1=maskUI[:], op=ALU.mult),
                     reads=["maskUI"], writes=["E2"])
                pkk, kpkk = bank()
                S.op("pe", lambda e, pkk=pkk, kT_c=kT_c: e.matmul(pkk[0:64, 0:64], lhsT=kT_c, rhs=kT_c, start=True, stop=True),
                     reads=[("cv", 1)], writes=[kpkk])
                S.op("dve", lambda e, pkk=pkk: e.tensor_tensor(out=t64[2][:], in0=pkk[0:64, 0:64], in1=E1[:], op=ALU.mult),
                     reads=["E1"], writes=[kpkk, "t2"])
                A_, B_ = Am[0], Bm[0]
                S.op("dve", lambda e, n=n, A_=A_: e.scalar_tensor_tensor(out=A_[:], in0=t64[2][:], scalar=cnb[:, n:n + 1], in1=maskSL[:],
                                                                        op0=ALU.mult, op1=ALU.mult),
                     reads=["t2", "c_nbeta", "maskSL"], writes=[("A", 0)])
                pt_, kpt_ = bank()
                S.op("pe", lambda e, pt_=pt_, A_=A_: e.transpose(out=pt_[0:64, 0:64], in_=A_[:, :], identity=identf[0:64, 0:64]),
                     reads=[("A", 0), "identf"], writes=[kpt_])
                S.op("act", lambda e, pt_=pt_, B_=B_: e.copy(out=B_[:], in_=pt_[0:64, 0:64]), writes=[kpt_, ("B", 0)])
                S.op("dve", lambda e, pt_=pt_: e.tensor_tensor(out=X[:], in0=pt_[0:64, 0:64], in1=identf[0:64, 0:64], op=ALU.add),
                     reads=["identf"], writes=[kpt_, "X"])
                cur = 0
                for lv in range(5):
                    nxt = 1 - cur
                    pa, kpa = bank()
                    S.op("pe", lambda e, pa=pa, cur=cur: e.matmul(pa[0:64, 0:64], lhsT=Bm[cur][:, :], rhs=Am[cur][:, :], start=True, stop=True),
                         reads=[("A", cur), ("B", cur)], writes=[kpa])
                    if lv < 4:
                        pbb, kpbb = bank()
                        S.op("pe", lambda e, pbb=pbb, cur=cur: e.matmul(pbb[0:64, 0:64], lhsT=Am[cur][:, :], rhs=Bm[cur][:, :],
                                                                        start=True, stop=True),
                             reads=[("A", cur), ("B", cur)], writes=[kpbb])
                    S.op("act", lambda e, pa=pa, nxt=nxt: e.copy(out=Am[nxt][:], in_=pa[0:64, 0:64]), writes=[kpa, ("A", nxt)])
                    if lv < 4:
                        S.op("dve", lambda e, pbb=pbb, nxt=nxt: e.tensor_copy(out=Bm[nxt][:], in_=pbb[0:64, 0:64]), writes=[kpbb, ("B", nxt)])
                    px, kpx = bank()
                    S.op("pe", lambda e, px=px, nxt=nxt: e.matmul(px[0:64, 0:64], lhsT=Am[nxt][:, :], rhs=X[:, :], start=True, stop=True),
                         reads=[("A", nxt), "X"], writes=[kpx])
                    S.op("dve", lambda e, px=px: e.tensor_tensor(out=X[:], in0=px[0:64, 0:64], in1=X[:], op=ALU.add),
                         writes=[kpx, "X"])
                    cur = nxt
                pw, kpw = bank()
                S.op("pe", lambda e, pw=pw: e.matmul(pw[:, 0:64], lhsT=kbe[:, :], rhs=X[:, :], start=True, stop=True),
                     reads=["kbe", "X"], writes=[kpw])
                S.op("act", lambda e, pw=pw: e.mul(out=wTn[:], in_=pw[:, 0:64], mul=-1.0), writes=[kpw, "wTn"])
                pq, kpq = bank()
                S.op("pe", lambda e, pq=pq, kT_c=kT_c, qT_c=qT_c: e.matmul(pq[0:64, 0:64], lhsT=kT_c, rhs=qT_c, start=True, stop=True),
                     reads=[("cv", 0), ("cv", 1)], writes=[kpq])
                S.op("dve", lambda e, pq=pq: e.tensor_tensor(out=attnT[:], in0=pq[0:64, 0:64], in1=E2[:], op=ALU.mult),
                     reads=["E2"], writes=[kpq, "attnT"])
                pvn, kpvn = bank()
                S.op("pe", lambda e, pvn=pvn: e.matmul(pvn[0:64, 0:128], lhsT=X[:, :], rhs=vb[:, :], start=True, stop=False),
                     reads=["X", "vb"], writes=[kpvn])
                S.op("pe", lambda e, pvn=pvn: e.matmul(pvn[0:64, 0:128], lhsT=wTn[:, :], rhs=Sst[:, :], start=False, stop=True),
                     reads=["wTn", "S"], writes=[kpvn])
                S.op("act", lambda e, pvn=pvn: e.copy(out=vnew[:], in_=pvn[0:64, 0:128]), writes=[kpvn, "vnew"])
                po1, kpo1 = bank()
                S.op("pe", lambda e, po1=po1, qT_c=qT_c: e.matmul(po1[0:64, 0:128], lhsT=qT_c, rhs=Sst[:, :], start=True, stop=True),
                     reads=[("cv", 0), "S"], writes=[kpo1])
                S.op("act", lambda e, po1=po1, n=n: e.activation(out=oq[:], in_=po1[0:64, 0:128], func=AF.Copy, scale=ceg[:, n:n + 1]),
                     reads=["c_eg"], writes=[kpo1, "oq"])
                po2, kpo2 = bank()
                S.op("pe", lambda e, po2=po2: e.matmul(po2[0:64, 0:128], lhsT=attnT[:, :], rhs=vnew[:, :], start=True, stop=True),
                     reads=["attnT", "vnew"], writes=[kpo2])
                S.op("dve", lambda e, po2=po2: e.tensor_tensor(out=osb[:], in0=po2[0:64, 0:128], in1=oq[:], op=ALU.add),
                     reads=["oq"], writes=[kpo2, "osb"])
                pS_, kpS = bank()
                S.op("pe", lambda e, pS_=pS_: e.matmul(pS_[:, 0:128], lhsT=kdec[:, :], rhs=vnew[:, :], start=True, stop=True),
                     reads=["kdec", "vnew"], writes=[kpS])
                S.op("dve", lambda e, pS_=pS_, n=n: e.scalar_tensor_tensor(out=Sst[:], in0=Sst[:], scalar=gt[:, n:n + 1], in1=pS_[:, 0:128],
                                                                          op0=ALU.mult, op1=ALU.add),
                     reads=["gt"], writes=[kpS, "S"])
                S.op("act", lambda e: e.activation(out=ojk[:], in_=osb[:], func=AF.Square, accum_out=ost[:, 0:1]),
                     reads=["osb"], writes=["ojk", "ost"])
                S.op("act", lambda e: e.activation(out=ost[:, 1:2], in_=ost[:, 0:1], func=AF.Sqrt, bias=ce[0:64, 0:1], scale=1.0 / HD),
                     reads=["ost", "ce"], writes=["ost"])
                S.op("dve", lambda e: e.reciprocal(out=ost[:, 2:3], in_=ost[:, 1:2]), reads=["ost"], writes=["ost"])
                S.op("dve", lambda e, n=n: e.scalar_tensor_tensor(out=obuf[:, n, :], in0=osb[:], scalar=ost[:, 2:3], in1=zt[:, n, :],
                                                                 op0=ALU.mult, op1=ALU.mult),
                     reads=["osb", "ost", "zt"], writes=["obuf"])
            S.dma("sp", lambda e, h=h: e.dma_start(out=io["mix"][:, h * 128:(h + 1) * 128].rearrange("(n p) d -> p n d", p=C), in_=obuf[:]),
                  reads=["obuf"], key="obuf")
        S.finish()


def phase_gdn2(nc, io, ctx=None):
    C = 64
    NCH = T // C
    G = 8
    NG = NCH // G
    CB = 512
    with contextlib.ExitStack() as st:
        if ctx is not None:
            st = ctx["st"]
        S = ctx["S"] if ctx is not None else Sched(nc, st)
        sb = lambda name, shape, dt: st.enter_context(nc.sbuf_tensor("g2_" + name, shape, dt))
        if ctx is None:
            PBK = [st.enter_context(nc.psum_tensor("g2_P%d" % i, [128, 512], F32)) for i in range(8)]
            LB = [(PBK[i], ("PB", i)) for i in range(4)]
            SB_ = [(PBK[i], ("PB", i)) for i in range(4, 7)]
            PREPB = [(PBK[7], ("PB", 7))]
        else:
            LB, SB_ = ctx["gdn_local_banks"], ctx["gdn_scan_banks"]
            PREPB = LB
        li = [0]
        si = [0]

        def lbank():
            i = li[0] % len(LB)
            li[0] += 1
            return LB[i]

        pi_ = [0]

        def pbank():
            i = pi_[0] % len(PREPB)
            pi_[0] += 1
            return PREPB[i]

        def sbank():
            i = si[0] % len(SB_)
            si[0] += 1
            return SB_[i]

        cw = sb("cw", [128, 12, 4], F32)
        identf = sb("identf", [128, 128], F32)
        ones = sb("ones", [128, 128], F32)
        maskSL = sb("maskSL", [64, 64], F32)
        maskUI = sb("maskUI", [64, 64], F32)
        gcon = sb("gcon", [64, 8], F32)
        nwb = sb("nwb", [64, 128], F32)
        c1 = sb("c1", [128, 1], F32)
        cq = sb("cq", [128, 1], F32)
        ck = sb("ck", [128, 1], F32)
        ce = sb("ce", [128, 1], F32)
        gba = sb("gba", [64, NCH, 8], F32)
        rawb = [sb("rawb%d" % i, [128, CB + 3], F32) for i in range(2)]
        tmpb = [sb("tmpb%d" % i, [128, CB], F32) for i in range(2)]
        cv = [[sb("cv%d_%d" % (hd, i), [128, T], BF16) for i in range(3)] for hd in range(NH)]
        dstb = [sb("dstb%d" % i, [128, CB], F32) for i in range(2)]
        identb = sb("identb", [128, 128], BF16)
        Sbf = [sb("Sbf%d" % hd, [128, 128], BF16) for hd in range(NH)]
        cnames = ("beta", "nbeta", "gl", "g", "eg", "egl", "beg", "tmp", "tmp2")
        col = [{nm: sb("col%d_%s" % (hd, nm), [64, NCH], F32) for nm in cnames} for hd in range(NH)]
        gt = [sb("gt%d" % hd, [128, NCH], F32) for hd in range(NH)]
        Sst = [sb("Sst%d" % hd, [128, 128], F32) for hd in range(NH)]
        X = [[sb("X%d_%d" % (hd, b), [64, G, 64], BF16) for b in range(2)] for hd in range(2)]
        vb = [[sb("vb%d_%d" % (hd, b), [64, G, 128], BF16) for b in range(2)] for hd in range(2)]
        kdec = [[sb("kdec%d_%d" % (hd, b), [64, G, 128], BF16) for b in range(2)] for hd in range(2)]
        wTn = [[sb("wTn%d_%d" % (hd, b), [128, G, 64], BF16) for b in range(2)] for hd in range(2)]
        attnT = [[sb("attnT%d_%d" % (hd, b), [64, G, 64], BF16) for b in range(2)] for hd in range(2)]
        zt = [[sb("zt%d_%d" % (hd, b), [64, G, 128], BF16) for b in range(2)] for hd in range(2)]
        kbe = [sb("kbe%d" % hd, [64, G, 128], BF16) for hd in range(2)]
        dg = [sb("dg%d" % hd, [64, G, 64], F32) for hd in range(2)]
        dd = [sb("dd%d" % hd, [64, G, 64], F32) for hd in range(2)]
        E1 = [sb("E1_%d" % hd, [64, G, 64], F32) for hd in range(2)]
        E2 = [sb("E2_%d" % hd, [64, G, 64], F32) for hd in range(2)]
        Am = [[sb("Am%d_%d" % (hd, b), [64, G, 64], BF16) for b in range(2)] for hd in range(2)]
        Bm = [[sb("Bm%d_%d" % (hd, b), [64, G, 64], BF16) for b in range(2)] for hd in range(2)]
        osb = [sb("osb%d" % hd, [64, G, 128], F32) for hd in range(2)]
        ojk = [sb("ojk%d" % hd, [64, G, 128], BF16) for hd in range(2)]
        obuf = [sb("obuf%d" % hd, [64, G, 128], BF16) for hd in range(2)]
        ost = [sb("ost%d" % hd, [64, 4 * G], F32) for hd in range(2)]
        vnew = [sb("vnew%d" % hd, [64, 128], BF16) for hd in range(2)]
        oq = [sb("oq%d" % hd, [64, 128], F32) for hd in range(2)]

        def cdma(dst, src, key):
            S.dma("sp", lambda e: e.dma_start(out=dst, in_=src), writes=[key], key="c_" + key)
        cdma(cw[:], io["conv_w"], "cw")
        cdma(identf[:], io["identf"], "identf")
        cdma(identb[:], io["ident"], "identb")
        cdma(maskSL[:], io["maskSL"], "maskSL")
        cdma(maskUI[:], io["maskUI"], "maskUI")
        cdma(gcon[:], io["gcon"], "gcon")
        cdma(nwb[:], io["nwg"][0:1, :].partition_broadcast(64), "nwb")
        cdma(gba[:], io["gba"].rearrange("(n p) c -> p n c", p=C), "gba")
        S.op("dve", lambda e: e.memset(ones[:], 1.0), writes=["ones"])
        S.op("dve", lambda e: e.memset(c1[:], 1.0), writes=["c1"])
        S.op("dve", lambda e: e.memset(cq[:], 128.0 * 1e-6), writes=["cq"])
        S.op("dve", lambda e: e.memset(ck[:], 1e-6), writes=["ck"])
        S.op("dve", lambda e: e.memset(ce[:], EPS), writes=["ce"])
        S.op("act", lambda e: e.activation(out=gcon[:, 0:4], in_=gcon[:, 0:4], func=AF.Exp), reads=["gcon"], writes=["gcon"])
        ident64 = identf[0:64, 0:64]
        ident64b = identb[0:64, 0:64]
        F32R = mybir.dt.float32r

        class _PE:
            def __init__(self, e):
                self.e = e

            def matmul(self, out, lhsT, rhs, start, stop):
                return getattr(self.e, 'matmul')(out, lhsT=lhsT, rhs=rhs, start=start, stop=stop)
        bi = [0]

        def prep(h, hd):
            for ti, src in enumerate((io["gqT"], io["gkT"], io["gvT"])):
                cvt = cv[h][ti]
                kc = ("cv", h, ti)
                wi = ti * 4 + h
                for blk in range(T // CB):
                    b = bi[0] % 2
                    bi[0] += 1
                    rb, tb = rawb[b], tmpb[b]
                    krb, ktb = ("rawb", b), ("tmpb", b)
                    c0 = blk * CB
                    if blk == 0:
                        S.op("pool", lambda e, rb=rb: e.memset(rb[:, 0:3], 0.0), writes=[krb])
                        S.dma("sp", lambda e, rb=rb, src=src, h=h: e.dma_start(out=rb[:, 3:], in_=src[h][:, 0:CB]), writes=[krb], key=krb)
                    else:
                        S.dma("sp", lambda e, rb=rb, src=src, h=h, c0=c0: e.dma_start(out=rb[:, :], in_=src[h][:, c0 - 3:c0 + CB]), writes=[krb], key=krb)
                    dst = dstb[b][:, :]
                    kd_ = ("dstb", b)
                    fin = cvt[:, c0:c0 + CB]
                    S.op("dve", lambda e, rb=rb, dst=dst, wi=wi: e.tensor_scalar(out=dst, in0=rb[:, 3:3 + CB], scalar1=cw[:, wi, 3:4],
                                                                                scalar2=None, op0=ALU.mult), reads=[krb, "cw"], writes=[kd_])
                    for j in (2, 1, 0):
                        S.op("dve", lambda e, rb=rb, dst=dst, wi=wi, j=j: e.scalar_tensor_tensor(
                            out=dst, in0=rb[:, j:j + CB], scalar=cw[:, wi, j:j + 1], in1=dst, op0=ALU.mult, op1=ALU.add),
                             reads=[krb, "cw"], writes=[kd_])
                    if ti == 2:
                        S.op("act", lambda e, dst=dst, fin=fin: e.activation(out=fin, in_=dst, func=AF.Silu), reads=[kd_], writes=[kc])
                    else:
                        S.op("act", lambda e, dst=dst: e.activation(out=dst, in_=dst, func=AF.Silu), writes=[kd_])
                    if ti < 2:
                        S.op("act", lambda e, dst=dst, tb=tb: e.activation(out=tb[:], in_=dst, func=AF.Square), reads=[kd_], writes=[ktb])
                        for g8 in range(CB // 512):
                            pb, kb = pbank()
                            S.op("pe", lambda e, pb=pb, g8=g8, tb=tb: _PE(e).matmul(pb[:, :], lhsT=ones[:, :], rhs=tb[:, g8 * 512:(g8 + 1) * 512],
                                                                              start=True, stop=True), reads=["ones", ktb], writes=[kb])
                            S.op("act", lambda e, pb=pb, g8=g8, rb=rb, ti=ti: e.activation(
                                out=rb[:, g8 * 512:(g8 + 1) * 512], in_=pb[:, :], func=AF.Ln,
                                bias=(cq if ti == 0 else ck)[:, 0:1], scale=(128.0 if ti == 0 else 1.0)),
                                 reads=["cq", "ck"], writes=[kb, krb])
                        S.op("act", lambda e, tb=tb, rb=rb: e.activation(out=tb[:], in_=rb[:, 0:CB], func=AF.Exp, scale=-0.5), reads=[krb], writes=[ktb])
                        S.op("pool", lambda e, dst=dst, tb=tb, fin=fin: e.tensor_tensor(out=fin, in0=dst, in1=tb[:], op=ALU.mult), reads=[ktb, kd_], writes=[kc])
            cl = col[h]
            cb_, cnb, cgl, cg, ceg, cegl, cbeg, ctmp, ctmp2 = (cl[k_] for k_ in cnames)
            kcol = ("col", h)
            S.op("act", lambda e: e.activation(out=cb_[:], in_=gba[:, :, h], func=AF.Sigmoid), reads=["gba"], writes=[kcol])
            S.op("dve", lambda e: e.tensor_scalar(out=cnb[:], in0=cb_[:], scalar1=-1.0, scalar2=None, op0=ALU.mult), writes=[kcol])
            S.op("act", lambda e: e.activation(out=ctmp[:], in_=gba[:, :, 4 + h], func=AF.Exp, bias=gcon[:, 4 + h:5 + h], scale=1.0),
                 reads=["gba", "gcon"], writes=[kcol])
            S.op("act", lambda e: e.activation(out=ctmp[:], in_=ctmp[:], func=AF.Ln, bias=c1[0:64, 0:1], scale=1.0), reads=["c1"], writes=[kcol])
            S.op("dve", lambda e: e.tensor_scalar(out=cgl[:], in0=ctmp[:], scalar1=gcon[:, h:h + 1], scalar2=-1.0, op0=ALU.mult, op1=ALU.mult),
                 reads=["gcon"], writes=[kcol])
            pb, kb = pbank()
            S.op("pe", lambda e, pb=pb: _PE(e).matmul(pb[0:64, 0:NCH], lhsT=maskUI[:, :], rhs=cgl[:, :], start=True, stop=True),
                 reads=["maskUI", kcol], writes=[kb])
            S.op("dve", lambda e, pb=pb: e.tensor_copy(out=cg[:], in_=pb[0:64, 0:NCH]), writes=[kb, kcol])
            pb2, kb2 = pbank()
            S.op("pe", lambda e, pb2=pb2: _PE(e).matmul(pb2[:, 0:NCH], lhsT=ones[0:64, :], rhs=cgl[:, :], start=True, stop=True),
                 reads=["ones", kcol], writes=[kb2])
            S.op("act", lambda e, pb2=pb2: e.activation(out=gt[h][:], in_=pb2[:, 0:NCH], func=AF.Exp), writes=[kb2, ("gt", h)])
            S.op("dve", lambda e, pb2=pb2: e.tensor_tensor(out=ctmp2[:], in0=pb2[0:64, 0:NCH], in1=cg[:], op=ALU.subtract), writes=[kb2, kcol])
            S.op("act", lambda e: e.activation(out=cegl[:], in_=ctmp2[:], func=AF.Exp), writes=[kcol])
            S.op("act", lambda e: e.activation(out=ceg[:], in_=cg[:], func=AF.Exp), writes=[kcol])
            S.op("dve", lambda e: e.tensor_tensor(out=cbeg[:], in0=cb_[:], in1=ceg[:], op=ALU.mult), writes=[kcol])
            S.op("dve", lambda e: e.memset(Sst[h][:], 0.0), writes=[("S", h)])
            S.op("pool", lambda e: e.memset(Sbf[h][:], 0.0), writes=[("Sb", h)])

        def bc2(ap2, n):
            return ap2.unsqueeze(2).to_broadcast([64, G, n])

        def bc1(ap2, n=64):
            return ap2.unsqueeze(1).to_broadcast([64, G, n])

        def v3(bank_ap, n):
            return bank_ap.rearrange("p (g i) -> p g i", g=G)

        def local_steps(h, hd, g):
            b = g % 2
            n0 = g * G
            qn, kn, vn = cv[h]
            cl = col[h]
            kcol = ("col", h)
            kX, kvb, kkd, kw, kat, kz = (("X", hd, b), ("vb", hd, b), ("kdec", hd, b), ("wTn", hd, b), ("attnT", hd, b), ("zt", hd, b))
            steps = []

            def s_kv():
                pk, kpk = lbank()
                for gi in range(G):
                    c0 = (n0 + gi) * C
                    S.op("pe", lambda e, pk=pk, gi=gi, c0=c0: e.transpose(out=pk[:].bitcast(BF16)[0:64, gi * 128:(gi + 1) * 128], in_=kn[:, c0:c0 + C], identity=identb[:]),
                         reads=[("cv", h, 1), "identb"], writes=[kpk])
                S.op("dve", lambda e, pk=pk: e.tensor_tensor(out=kbe[hd][:], in0=v3(pk[:].bitcast(BF16)[0:64, 0:G * 128], 128), in1=bc2(cl["beg"][:, n0:n0 + G], 128), op=ALU.mult),
                     reads=[kcol], writes=[kpk, ("kbe", hd)])
                S.op("dve", lambda e, pk=pk: e.tensor_tensor(out=kdec[hd][b][:], in0=v3(pk[:].bitcast(BF16)[0:64, 0:G * 128], 128), in1=bc2(cl["egl"][:, n0:n0 + G], 128), op=ALU.mult),
                     reads=[kcol], writes=[kpk, kkd])
                pv, kpv = lbank()
                for gi in range(G):
                    c0 = (n0 + gi) * C
                    S.op("pe", lambda e, pv=pv, gi=gi, c0=c0: e.transpose(out=pv[:].bitcast(BF16)[0:64, gi * 128:(gi + 1) * 128], in_=vn[:, c0:c0 + C], identity=identb[:]),
                         reads=[("cv", h, 2), "identb"], writes=[kpv])
                S.op("dve", lambda e, pv=pv: e.tensor_tensor(out=vb[hd][b][:], in0=v3(pv[:].bitcast(BF16)[0:64, 0:G * 128], 128), in1=bc2(cl["beta"][:, n0:n0 + G], 128), op=ALU.mult),
                     reads=[kcol], writes=[kpv, kvb])
            steps.append(s_kv)

            def s_decay():
                S.op("dve", lambda e: e.tensor_tensor(out=dg[hd][:], in0=bc1(ident64), in1=bc2(cl["g"][:, n0:n0 + G], 64), op=ALU.mult),
                     reads=["identf", kcol], writes=[("dg", hd)])
                pg, kpg = lbank()
                S.op("pe", lambda e, pg=pg: _PE(e).matmul(pg[0:64, 0:G * 64], lhsT=ones[0:64, 0:64], rhs=dg[hd][:].rearrange("p g i -> p (g i)"),
                                                     start=True, stop=True), reads=["ones", ("dg", hd)], writes=[kpg])
                S.op("dve", lambda e, pg=pg: e.tensor_tensor(out=dd[hd][:], in0=v3(pg[0:64, 0:G * 64], 64), in1=bc2(cl["g"][:, n0:n0 + G], 64), op=ALU.subtract),
                     reads=[kcol], writes=[kpg, ("dd", hd)])
                S.op("dve", lambda e: e.tensor_scalar(out=E1[hd][:], in0=dd[hd][:], scalar1=0.0, scalar2=None, op0=ALU.max),
                     reads=[("dd", hd)], writes=[("E1", hd)])
                S.op("act", lambda e: e.activation(out=E1[hd][:], in_=E1[hd][:], func=AF.Exp, scale=-1.0), writes=[("E1", hd)])
                S.op("dve", lambda e: e.tensor_tensor(out=E1[hd][:], in0=E1[hd][:], in1=bc1(maskSL[:]), op=ALU.mult), reads=["maskSL"], writes=[("E1", hd)])
                S.op("dve", lambda e: e.tensor_tensor(out=E1[hd][:], in0=E1[hd][:], in1=bc2(cl["nbeta"][:, n0:n0 + G], 64), op=ALU.mult),
                     reads=[kcol], writes=[("E1", hd)])
                S.op("dve", lambda e: e.tensor_scalar(out=E2[hd][:], in0=dd[hd][:], scalar1=0.0, scalar2=None, op0=ALU.min),
                     reads=[("dd", hd)], writes=[("E2", hd)])
                S.op("act", lambda e: e.activation(out=E2[hd][:], in_=E2[hd][:], func=AF.Exp), writes=[("E2", hd)])
                S.op("pool", lambda e: e.tensor_tensor(out=E2[hd][:], in0=E2[hd][:], in1=bc1(maskUI[:]), op=ALU.mult), reads=["maskUI"], writes=[("E2", hd)])
            steps.append(s_decay)

            def s_A():
                pkk, kpkk = lbank()
                for gi in range(G):
                    c0 = (n0 + gi) * C
                    S.op("pe", lambda e, pkk=pkk, gi=gi, c0=c0: _PE(e).matmul(pkk[0:64, gi * 64:(gi + 1) * 64], lhsT=kn[:, c0:c0 + C], rhs=kn[:, c0:c0 + C],
                                                                         start=True, stop=True), reads=[("cv", h, 1)], writes=[kpkk])
                S.op("dve", lambda e, pkk=pkk: e.tensor_tensor(out=Am[hd][0][:], in0=v3(pkk[0:64, 0:G * 64], 64), in1=E1[hd][:], op=ALU.mult),
                     reads=[("E1", hd)], writes=[kpkk, ("A", hd, 0)])
                pt_, kpt_ = lbank()
                for gi in range(G):
                    S.op("pe", lambda e, pt_=pt_, gi=gi: e.transpose(out=pt_[:].bitcast(BF16)[0:64, gi * 64:(gi + 1) * 64], in_=Am[hd][0][:, gi, :], identity=ident64b),
                         reads=[("A", hd, 0), "identb"], writes=[kpt_])
                S.op("act", lambda e, pt_=pt_: e.copy(out=Bm[hd][0][:], in_=v3(pt_[:].bitcast(BF16)[0:64, 0:G * 64], 64)), writes=[kpt_, ("B", hd, 0)])
                S.op("dve", lambda e, pt_=pt_: e.tensor_tensor(out=X[hd][b][:], in0=v3(pt_[:].bitcast(BF16)[0:64, 0:G * 64], 64), in1=bc1(ident64), op=ALU.add),
                     reads=["identf"], writes=[kpt_, kX])
            steps.append(s_A)

            def mk_level(lv):
                def s_lv():
                    cur = lv % 2
                    nxt = 1 - cur
                    pa, kpa = lbank()
                    for gi in range(G):
                        S.op("pe", lambda e, pa=pa, gi=gi: _PE(e).matmul(pa[0:64, gi * 64:(gi + 1) * 64], lhsT=Bm[hd][cur][:, gi, :], rhs=Am[hd][cur][:, gi, :],
                                                                    start=True, stop=True), reads=[("A", hd, cur), ("B", hd, cur)], writes=[kpa])
                    if lv < 4:
                        pbb, kpbb = lbank()
                        for gi in range(G):
                            S.op("pe", lambda e, pbb=pbb, gi=gi: _PE(e).matmul(pbb[0:64, gi * 64:(gi + 1) * 64], lhsT=Am[hd][cur][:, gi, :], rhs=Bm[hd][cur][:, gi, :],
                                                                          start=True, stop=True), reads=[("A", hd, cur), ("B", hd, cur)], writes=[kpbb])
                    S.op("act", lambda e, pa=pa: e.copy(out=Am[hd][nxt][:], in_=v3(pa[0:64, 0:G * 64], 64)), writes=[kpa, ("A", hd, nxt)])
                    if lv < 4:
                        S.op("dve", lambda e, pbb=pbb: e.tensor_copy(out=Bm[hd][nxt][:], in_=v3(pbb[0:64, 0:G * 64], 64)), writes=[kpbb, ("B", hd, nxt)])
                    px, kpx = lbank()
                    for gi in range(G):
                        S.op("pe", lambda e, px=px, gi=gi: _PE(e).matmul(px[0:64, gi * 64:(gi + 1) * 64], lhsT=Am[hd][nxt][:, gi, :], rhs=X[hd][b][:, gi, :],
                                                                    start=True, stop=True), reads=[("A", hd, nxt), kX], writes=[kpx])
                    S.op("dve", lambda e, px=px: e.tensor_tensor(out=X[hd][b][:], in0=v3(px[0:64, 0:G * 64], 64), in1=X[hd][b][:], op=ALU.add),
                         writes=[kpx, kX])
                return s_lv
            for lv in range(5):
                steps.append(mk_level(lv))

            def s_w():
                pw, kpw = lbank()
                for gi in range(G):
                    S.op("pe", lambda e, pw=pw, gi=gi: _PE(e).matmul(pw[:, gi * 64:(gi + 1) * 64], lhsT=kbe[hd][:, gi, :], rhs=X[hd][b][:, gi, :],
                                                                start=True, stop=True), reads=[("kbe", hd), kX], writes=[kpw])
                S.op("act", lambda e, pw=pw: e.mul(out=wTn[hd][b][:], in_=pw[:, 0:G * 64].rearrange("p (g i) -> p g i", g=G), mul=-1.0),
                     writes=[kpw, kw])
                pq, kpq = lbank()
                for gi in range(G):
                    c0 = (n0 + gi) * C
                    S.op("pe", lambda e, pq=pq, gi=gi, c0=c0: _PE(e).matmul(pq[0:64, gi * 64:(gi + 1) * 64], lhsT=kn[:, c0:c0 + C], rhs=qn[:, c0:c0 + C],
                                                                       start=True, stop=True), reads=[("cv", h, 0), ("cv", h, 1)], writes=[kpq])
                S.op("dve", lambda e, pq=pq: e.tensor_tensor(out=attnT[hd][b][:], in0=v3(pq[0:64, 0:G * 64], 64), in1=E2[hd][:], op=ALU.mult),
                     reads=[("E2", hd)], writes=[kpq, kat])
                S.dma("pool", lambda e: e.dma_start(out=zt[hd][b][:], in_=io["gz"][n0 * C:(n0 + G) * C, h * 128:(h + 1) * 128].rearrange("(g p) d -> p g d", p=C)),
                      writes=[kz], key=kz)
                S.op("act", lambda e: e.activation(out=zt[hd][b][:], in_=zt[hd][b][:], func=AF.Silu), writes=[kz])
                S.op("pool", lambda e: e.tensor_tensor(out=zt[hd][b][:], in0=zt[hd][b][:], in1=bc1(nwb[:], 128), op=ALU.mult), reads=["nwb"], writes=[kz])
            steps.append(s_w)
            return steps

        def scan_steps(h, hd, g):
            b = g % 2
            n0 = g * G
            qn = cv[h][0]
            cl = col[h]
            kcol = ("col", h)
            kX, kvb, kkd, kw, kat, kz = (("X", hd, b), ("vb", hd, b), ("kdec", hd, b), ("wTn", hd, b), ("attnT", hd, b), ("zt", hd, b))
            steps = []
            for gi in range(G):
                n = n0 + gi
                c0 = n * C

                def s_v(gi=gi, n=n, c0=c0):
                    pvn, kpvn = sbank()
                    S.op("pe", lambda e, pvn=pvn: _PE(e).matmul(pvn[0:64, 0:128], lhsT=X[hd][b][:, gi, :], rhs=vb[hd][b][:, gi, :], start=True, stop=False),
                         reads=[kX, kvb], writes=[kpvn])
                    S.op("pe", lambda e, pvn=pvn: _PE(e).matmul(pvn[0:64, 0:128], lhsT=wTn[hd][b][:, gi, :], rhs=Sbf[h][:, :], start=False, stop=True),
                         reads=[kw, ("Sb", h)], writes=[kpvn])
                    S.op("act", lambda e, pvn=pvn: e.copy(out=vnew[hd][:], in_=pvn[0:64, 0:128]), writes=[kpvn, ("vnew", hd)])
                    po1, kpo1 = sbank()
                    S.op("pe", lambda e, po1=po1: _PE(e).matmul(po1[0:64, 0:128], lhsT=qn[:, c0:c0 + C], rhs=Sbf[h][:, :], start=True, stop=True),
                         reads=[("cv", h, 0), ("Sb", h)], writes=[kpo1])
                    S.op("act", lambda e, po1=po1: e.activation(out=oq[hd][:], in_=po1[0:64, 0:128], func=AF.Copy, scale=cl["eg"][:, n:n + 1]),
                         reads=[kcol], writes=[kpo1, ("oq", hd)])
                steps.append(s_v)

                def s_o(gi=gi, n=n):
                    po2, kpo2 = sbank()
                    S.op("pe", lambda e, po2=po2: _PE(e).matmul(po2[0:64, 0:128], lhsT=attnT[hd][b][:, gi, :], rhs=vnew[hd][:, :], start=True, stop=True),
                         reads=[kat, ("vnew", hd)], writes=[kpo2])
                    S.op("dve", lambda e, po2=po2: e.tensor_tensor(out=osb[hd][:, gi, :], in0=po2[0:64, 0:128], in1=oq[hd][:], op=ALU.add),
                         reads=[("oq", hd)], writes=[kpo2, ("osb", hd)])
                    pS_, kpS = sbank()
                    S.op("pe", lambda e, pS_=pS_: _PE(e).matmul(pS_[:, 0:128], lhsT=kdec[hd][b][:, gi, :], rhs=vnew[hd][:, :], start=True, stop=True),
                         reads=[kkd, ("vnew", hd)], writes=[kpS])
                    S.op("dve", lambda e, pS_=pS_: e.scalar_tensor_tensor(out=Sst[h][:], in0=Sst[h][:], scalar=gt[h][:, n:n + 1], in1=pS_[:, 0:128],
                                                                         op0=ALU.mult, op1=ALU.add),
                         reads=[("gt", h)], writes=[kpS, ("S", h)])
                    S.op("act", lambda e: e.copy(out=Sbf[h][:], in_=Sst[h][:]), reads=[("S", h)], writes=[("Sb", h)])
                steps.append(s_o)

            def s_norm():
                o_ = ost[hd]
                ko = ("ost", hd)
                S.op("act", lambda e: e.activation(out=ojk[hd][:], in_=osb[hd][:], func=AF.Square), reads=[("osb", hd)], writes=[("ojk", hd)])
                S.op("dve", lambda e: e.tensor_reduce(out=o_[:, 0:G], in_=ojk[hd][:], axis=AX.X, op=ALU.add), reads=[("ojk", hd)], writes=[ko])
                S.op("act", lambda e: e.activation(out=o_[:, G:2 * G], in_=o_[:, 0:G], func=AF.Sqrt, bias=ce[0:64, 0:1], scale=1.0 / HD),
                     reads=["ce"], writes=[ko])
                S.op("dve", lambda e: e.reciprocal(out=o_[:, 2 * G:3 * G], in_=o_[:, G:2 * G]), writes=[ko])
                S.op("dve", lambda e: e.tensor_tensor(out=osb[hd][:], in0=osb[hd][:], in1=bc2(o_[:, 2 * G:3 * G], 128), op=ALU.mult),
                     reads=[ko], writes=[("osb", hd)])
                S.op("pool", lambda e: e.tensor_tensor(out=obuf[hd][:], in0=osb[hd][:], in1=zt[hd][b][:], op=ALU.mult),
                     reads=[kz], writes=[("osb", hd), ("obuf", hd)])
                S.dma("sp", lambda e: e.dma_start(out=io["mix"][n0 * C:(n0 + G) * C, h * 128:(h + 1) * 128].rearrange("(g p) d -> p g d", p=C), in_=obuf[hd][:]),
                      reads=[("obuf", hd)], key=("obuf", hd))
            steps.append(s_norm)
            return steps

        def lock(step_lists):
            n = max(len(x) for x in step_lists)
            for i in range(n):
                for x in step_lists:
                    if i < len(x):
                        x[i]()

        outer_cap = S._cap
        S._cap = None
        PP, LP = [], []
        for pair in range(NH // 2):
            hs = (2 * pair, 2 * pair + 1)
            S.capture()
            for hd, h in enumerate(hs):
                prep(h, hd)
            PP.append(S.end_capture())
            S.capture()
            lock([local_steps(h, hd, 0) for hd, h in enumerate(hs)])
            for g in range(NG):
                loc = [local_steps(h, hd, g + 1) for hd, h in enumerate(hs)] if g + 1 < NG else []
                scn = [scan_steps(h, hd, g) for hd, h in enumerate(hs)]
                lock(loc + scn)
            LP.append(S.end_capture())
        order = list(PP[0])
        for pair in range(NH // 2):
            order.extend(merge_prop(LP[pair], PP[pair + 1]) if pair + 1 < len(PP) else LP[pair])
        if outer_cap is not None:
            S._cap = outer_cap
            outer_cap.extend(order)
        else:
            S.replay(order)
        if ctx is None:
            S.finish()


def phase_mixers(nc, io):
    with contextlib.ExitStack() as st:
        S = Sched(nc, st)
        banks = [(st.enter_context(nc.psum_tensor("mx_P%d" % i, [128, 512], F32)), ("PB", i)) for i in range(8)]
        ctx = {"S": S, "st": st, "gdn_local_banks": banks[0:3], "gdn_scan_banks": banks[3:5], "moba_banks": banks[5:8]}
        S.capture()
        phase_moba(nc, io, ctx)
        M = S.end_capture()
        S.capture()
        phase_gdn2(nc, io, ctx)
        Gd = S.end_capture()
        i = j = 0
        merged = []
        while i < len(Gd) or j < len(M):
            if j >= len(M) or (i < len(Gd) and i * len(M) <= j * len(Gd)):
                merged.append(Gd[i])
                i += 1
            else:
                merged.append(M[j])
                j += 1
        S.replay(merged)
        S.finish()


TB = 2048
NE = 32
CAP = 256
DUMMY = NE * CAP


def phase_b1(nc, io):
    with contextlib.ExitStack() as st:
        S = Sched(nc, st)
        sb = lambda name, shape, dt: st.enter_context(nc.sbuf_tensor("b1_" + name, shape, dt))
        ps = lambda name, shape, dt: st.enter_context(nc.psum_tensor("b1_" + name, shape, dt))
        Wo, wsem = io["Wo_pre"]
        S.last_w["Wo"] = (wsem, 16 * 16)
        Wr = sb("Wr", [128, 16, 36], BF16)
        ident = sb("ident", [128, 128], BF16)
        nwb = sb("nwb", [128, D], F32)
        brb = sb("brb", [128, 36], F32)
        epsb = sb("epsb", [128, 1], F32)
        mt = [sb("mt%d" % i, [128, D], BF16) for i in range(4)]
        mT = [sb("mT%d" % i, [128, 16, 128], BF16) for i in range(4)]
        xt = [sb("xt%d" % i, [128, D], F32) for i in range(4)]
        h2 = [sb("h2_%d" % i, [128, D], BF16) for i in range(4)]
        h2T = [sb("h2T%d" % i, [128, 16, 128], BF16) for i in range(4)]
        junk = sb("junk", [128, D], BF16)
        stt = [sb("stt%d" % i, [128, 16], F32) for i in range(4)]
        NT_ = TB // 128
        LG = sb("LG", [128, NT_, 36], F32)
        mg = sb("mg", [128, NT_], F32)
        zz = sb("zz", [128, NT_], F32)
        ptg = sb("ptg", [128, NT_], F32)
        rr = sb("rr", [128, NT_], F32)
        den = sb("den", [128, NT_], F32)
        eg4 = sb("eg4", [128, NT_, 4], F32)
        og4 = sb("og4", [128, NT_, 4], F32)
        LE = sb("LE", [128, NT_, 32], F32)
        T8 = sb("T8", [128, NT_, 8], F32)
        M1 = sb("M1", [128, NT_, 32], F32)
        M2 = sb("M2", [128, NT_, 32], F32)
        M01 = sb("M01", [128, NT_, 32], F32)
        SL = sb("SL", [128, NT_, 32], F32)
        VL = sb("VL", [128, NT_, 32], F32)
        GS = sb("GS", [128, NT_, 2], F32)
        DS = sb("DS", [128, NT_, 2], F32)
        DI = sb("DI", [128, NT_, 2], I32)
        cnt = sb("cnt", [128, 32], F32)
        ebase = sb("ebase", [128, 32], F32)
        UTs = sb("UTs", [128, 128], F32)
        onesf = sb("onesf", [128, 128], F32)
        PT = ps("PT", [128, 2048], BF16)
        PM = [ps("PM%d" % i, [128, 512], F32) for i in range(2)]
        PT2 = ps("PT2", [128, 2048], BF16)
        PR = ps("PR", [128, 512], F32)
        PP = ps("PP", [128, 512], F32)
        S.op("dve", lambda e: e.memset(onesf[:], 1.0), writes=["onesf"])
        S.dma("sp", lambda e: e.dma_start(out=ebase[:], in_=io["ebase"]), writes=["ebase"], key="c_eb")
        S.dma("sp", lambda e: e.dma_start(out=UTs[:], in_=io["uts"]), writes=["UTs"], key="c_uts")

        S.dma("sp", lambda e: e.dma_start(out=ident[:], in_=io["ident"]), writes=["ident"], key="c_ident")
        S.dma("sp", lambda e: e.dma_start(out=nwb[:], in_=io["nw2"][0:1, :].partition_broadcast(128)), writes=["nwb"], key="c_nwb")
        S.dma("sp", lambda e: e.dma_start(out=brb[:], in_=io["br"][0:1, :].partition_broadcast(128)), writes=["brb"], key="c_brb")
        ridx = sb("ridx", [128, 32], I32)
        S.dma("sp", lambda e: e.dma_start(out=ridx[:], in_=io["rowidx"]), writes=["ridx"], key="c_ridx")
        S.op("dve", lambda e: e.memset(epsb[:], EPS), writes=["epsb"])
        S.dma("pool", lambda e: e.dma_start(out=Wr[:], in_=io["wr"].rearrange("(k p) c -> p k c", p=128)), writes=["Wr"], key="c_wr")
        ei = [0]

        def evac(out_ap, in_ap, bankkey, writes):
            ei[0] += 1
            if ei[0] % 2:
                S.op("act", lambda e: e.copy(out=out_ap, in_=in_ap), writes=[bankkey] + writes)
            else:
                S.op("dve", lambda e: e.tensor_copy(out=out_ap, in_=in_ap), writes=[bankkey] + writes)

        mi = 0
        SA, SBq = [], []
        for t in range(TB // 128):
            p = t % 4
            r0 = t * 128
            S.capture()
            for r in range(2):
                S.dma("pool", lambda e, p=p, t=t, r=r: e.indirect_dma_start(
                    out=mt[p][:, r * 1024:(r + 1) * 1024], out_offset=None, in_=io["mixg"][:, :],
                    in_offset=bass.IndirectOffsetOnAxis(ap=ridx[:, t * 2 + r:t * 2 + r + 1], axis=0)),
                    reads=["ridx"], writes=[("mt", p)], key=("mt", p, r))
            S.dma("sp", lambda e, p=p, r0=r0: e.dma_start(out=xt[p][:], in_=io["xc"][r0:r0 + 128, :]), writes=[("xt", p)], key=("xt", p))
            for k in range(16):
                S.op("pe", lambda e, k=k, p=p: e.transpose(out=PT[:, k * 128:(k + 1) * 128], in_=mt[p][:, k * 128:(k + 1) * 128],
                                                           identity=ident[:]), reads=[("mt", p), "ident"], writes=["PT"])
            evac(mT[p][:], PT[:].rearrange("p (k t) -> p k t", k=16), "PT", [("mT", p)])
            for cg in range(4):
                pm, kp = PM[mi % 2], ("PM", mi % 2)
                mi += 1
                for k in range(16):
                    S.op("pe", lambda e, k=k, p=p, pm=pm, cg=cg: e.matmul(pm[:], lhsT=mT[p][:, k, :], rhs=Wo[:, k, cg * 512:(cg + 1) * 512],
                                                                          start=(k == 0), stop=(k == 15)),
                         reads=[("mT", p), "Wo"], writes=[kp])
                S.op("dve", lambda e, pm=pm, p=p, cg=cg: e.tensor_tensor(out=xt[p][:, cg * 512:(cg + 1) * 512], in0=pm[:],
                                                                         in1=xt[p][:, cg * 512:(cg + 1) * 512], op=ALU.add),
                     writes=[kp, ("xt", p)])
            S.dma("act", lambda e, p=p, r0=r0: e.dma_start(out=io["xmid"][r0:r0 + 128, :], in_=xt[p][:]), reads=[("xt", p)], key=("xts", p))
            sp_ = stt[p]
            ks = ("stt", p)
            S.op("act", lambda e, p=p, sp_=sp_: e.activation(out=junk[:], in_=xt[p][:], func=AF.Square, accum_out=sp_[:, 0:1]),
                 reads=[("xt", p)], writes=["junk", ks])
            S.op("act", lambda e, sp_=sp_: e.activation(out=sp_[:, 1:2], in_=sp_[:, 0:1], func=AF.Sqrt, bias=epsb[:, 0:1], scale=1.0 / D),
                 reads=["epsb"], writes=[ks])
            S.op("dve", lambda e, sp_=sp_: e.reciprocal(out=sp_[:, 2:3], in_=sp_[:, 1:2]), writes=[ks])
            S.op("dve", lambda e, p=p, sp_=sp_, t=t: e.scalar_tensor_tensor(out=h2[p][:], in0=xt[p][:], scalar=sp_[:, 2:3], in1=nwb[:],
                                                                      op0=ALU.mult, op1=ALU.mult),
                 reads=[("xt", p), ks, "nwb"], writes=[("h2", p)])
            S.dma("act", lambda e, p=p, r0=r0: e.dma_start(out=io["h2d"][r0:r0 + 128, :], in_=h2[p][:]), reads=[("h2", p)], writes=[("h2d", t)], key=("h2s", p))
            sa_ = S.end_capture()
            S.capture()
            for k in range(16):
                S.op("pe", lambda e, k=k, p=p, t=t: e.transpose(out=PT2[:, k * 128:(k + 1) * 128], in_=h2[p][:, k * 128:(k + 1) * 128],
                                                           identity=ident[:]), reads=[("h2", p), "ident"], writes=["PT2"])
            evac(h2T[p][:], PT2[:].rearrange("p (k t) -> p k t", k=16), "PT2", [("h2T", p)])
            for k in range(16):
                S.op("pe", lambda e, k=k, p=p: e.matmul(PR[:, 0:36], lhsT=h2T[p][:, k, :], rhs=Wr[:, k, :], start=(k == 0), stop=(k == 15)),
                     reads=[("h2T", p), "Wr"], writes=["PR"])
            S.op("dve", lambda e, t=t: e.tensor_tensor(out=LG[:, t, :], in0=PR[:, 0:36], in1=brb[:], op=ALU.add),
                 reads=["brb"], writes=["PR", ("LG", t)])
            SA.append(sa_)
            SBq.append(S.end_capture())
        order = list(SA[0])
        for t in range(len(SA)):
            order.extend(merge_prop(SA[t + 1], SBq[t]) if t + 1 < len(SA) else SBq[t])
        S.replay(order)
        NT = TB // 128
        R = "route"

        def b3(ap2, n):
            return ap2.unsqueeze(2).to_broadcast([128, NT, n])
        lgG = LG[:, :, 0:4]
        LGK = [("LG", t_) for t_ in range(NT)]
        S.op("dve", lambda e: e.tensor_reduce(out=mg[:], in_=lgG, axis=AX.X, op=ALU.max), reads=LGK, writes=[R])
        S.op("dve", lambda e: e.tensor_tensor(out=eg4[:], in0=lgG, in1=b3(mg[:], 4), op=ALU.subtract), reads=LGK, writes=[R])
        S.op("act", lambda e: e.activation(out=eg4[:], in_=eg4[:], func=AF.Exp), writes=[R])
        S.op("dve", lambda e: e.tensor_reduce(out=zz[:], in_=eg4[:], axis=AX.X, op=ALU.add), writes=[R])
        S.op("dve", lambda e: e.reciprocal(out=ptg[:], in_=zz[:]), writes=[R])
        S.op("dve", lambda e: e.tensor_tensor(out=og4[:], in0=lgG, in1=b3(mg[:], 4), op=ALU.is_equal), reads=LGK, writes=[R])
        S.op("dve", lambda e: e.tensor_scalar(out=og4[:], in0=og4[:], scalar1=-1.0, scalar2=1e30, op0=ALU.add, op1=ALU.mult), writes=[R])
        S.op("dve", lambda e: e.tensor_tensor(
            out=LE[:].rearrange("p t (g x) -> p t g x", g=4), in0=LG[:, :, 4:36].rearrange("p t (g x) -> p t g x", g=4),
            in1=og4[:].unsqueeze(3).to_broadcast([128, NT, 4, 8]), op=ALU.add), reads=LGK, writes=[R])
        for t in range(NT):
            S.op("dve", lambda e, t=t: e.max(out=T8[:, t, :], in_=LE[:, t, :]), writes=[R])
        S.op("dve", lambda e: e.tensor_tensor(out=rr[:], in0=T8[:, :, 1], in1=T8[:, :, 0], op=ALU.subtract), writes=[R])
        S.op("act", lambda e: e.activation(out=rr[:], in_=rr[:], func=AF.Exp), writes=[R])
        S.op("dve", lambda e: e.tensor_scalar(out=den[:], in0=rr[:], scalar1=1.0, scalar2=None, op0=ALU.add), writes=[R])
        S.op("dve", lambda e: e.reciprocal(out=den[:], in_=den[:]), writes=[R])
        S.op("dve", lambda e: e.tensor_tensor(out=GS[:, :, 0], in0=den[:], in1=ptg[:], op=ALU.mult), writes=[R])
        S.op("dve", lambda e: e.tensor_tensor(out=GS[:, :, 1], in0=GS[:, :, 0], in1=rr[:], op=ALU.mult), writes=[R])
        S.op("dve", lambda e: e.tensor_tensor(out=M1[:], in0=LE[:], in1=b3(T8[:, :, 0], 32), op=ALU.is_equal), writes=[R])
        S.op("dve", lambda e: e.tensor_tensor(out=M2[:], in0=LE[:], in1=b3(T8[:, :, 1], 32), op=ALU.is_equal), writes=[R])
        S.op("dve", lambda e: e.tensor_tensor(out=M01[:], in0=M1[:], in1=M2[:], op=ALU.add), writes=[R])
        for t in range(NT):
            S.op("pe", lambda e, t=t: e.matmul(PP[:, t * 32:(t + 1) * 32], lhsT=UTs[:, :], rhs=M01[:, t, :], start=True, stop=(t == 0)),
                 reads=[R, "UTs"], writes=["PP"])
            for t2 in range(t):
                S.op("pe", lambda e, t=t, t2=t2: e.matmul(PP[:, t * 32:(t + 1) * 32], lhsT=onesf[:, :], rhs=M01[:, t2, :], start=False, stop=(t2 == t - 1)),
                     reads=[R, "onesf"], writes=["PP"])
        S.op("dve", lambda e: e.tensor_copy(out=SL[:], in_=PP[:, 0:NT * 32].rearrange("p (t x) -> p t x", t=NT)), writes=["PP", R])
        S.op("dve", lambda e: e.tensor_scalar(out=VL[:], in0=SL[:], scalar1=float(CAP), scalar2=None, op0=ALU.is_lt), writes=[R])
        S.op("dve", lambda e: e.tensor_tensor(out=SL[:], in0=SL[:], in1=ebase[:].unsqueeze(1).to_broadcast([128, NT, 32]), op=ALU.add),
             reads=["ebase"], writes=[R])
        S.op("dve", lambda e: e.tensor_tensor(out=SL[:], in0=SL[:], in1=VL[:], op=ALU.mult), writes=[R])
        S.op("dve", lambda e: e.tensor_scalar(out=SL[:], in0=SL[:], scalar1=float(DUMMY), scalar2=None, op0=ALU.add), writes=[R])
        for kk, MK in ((0, M1), (1, M2)):
            S.op("dve", lambda e, MK=MK: e.tensor_tensor(out=MK[:], in0=MK[:], in1=SL[:], op=ALU.mult), writes=[R])
            S.op("dve", lambda e, MK=MK, kk=kk: e.tensor_reduce(out=DS[:, :, kk], in_=MK[:], axis=AX.X, op=ALU.add), writes=[R])
        S.op("dve", lambda e: e.tensor_copy(out=DI[:], in_=DS[:]), writes=[R])
        S.dma("sp", lambda e: e.dma_start(out=io["dsti"].rearrange("(n p) c -> p n c", p=128), in_=DI[:]), reads=[R], key="dis")
        S.dma("sp", lambda e: e.dma_start(out=io["gsel"].rearrange("(n p) c -> p n c", p=128), in_=GS[:]), reads=[R], key="dfs")
        for t in range(NT):
            p = t % 4
            S.dma("sp", lambda e, p=p, t=t: e.dma_start(out=h2[p][:], in_=io["h2d"][t * 128:(t + 1) * 128, :]),
                  reads=[("h2d", t)], writes=[("h2", p)], key=("h2l", p))
            for kk in range(2):
                S.dma("pool", lambda e, t=t, kk=kk, p=p: e.indirect_dma_start(
                    out=io["xg"][:, :], out_offset=bass.IndirectOffsetOnAxis(ap=DI[:, t, kk:kk + 1], axis=0),
                    in_=h2[p][:, :], in_offset=None), reads=[R, ("h2", p)], key=("scat", (t * 2 + kk) % 8))
        S.finish()


def phase_b2(nc, io):
    with contextlib.ExitStack() as st:
        S = Sched(nc, st)
        sb = lambda name, shape, dt: st.enter_context(nc.sbuf_tensor("b2_" + name, shape, dt))
        ps = lambda name, shape, dt: st.enter_context(nc.psum_tensor("b2_" + name, shape, dt))
        W1 = [sb("W1_%d" % i, [128, 16, 512], BF16) for i in range(2)]
        W2 = [sb("W2_%d" % i, [128, 16, 512], BF16) for i in range(2)]
        W3 = [sb("W3_%d" % i, [128, 4, D], BF16) for i in range(2)]
        ident = sb("ident", [128, 128], BF16)
        xgt = [sb("xgt%d" % i, [128, 2, D], BF16) for i in range(2)]
        xT = [sb("xT%d" % i, [128, 16, 256], BF16) for i in range(2)]
        hT = [sb("hT%d" % i, [128, 4, 256], BF16) for i in range(2)]
        sg = [sb("sg%d" % i, [128, 256], F32) for i in range(2)]
        ysb = [sb("ysb%d" % i, [128, D], F32) for i in range(2)]
        PT = ps("PT", [128, 2048], BF16)
        PGU = [ps("PGU%d" % i, [128, 512], F32) for i in range(4)]
        PD = [ps("PD%d" % i, [128, 512], F32) for i in range(2)]
        S.dma("sp", lambda e: e.dma_start(out=ident[:], in_=io["ident"]), writes=["ident"], key="c_ident")
        gi = 0
        di = 0
        ei = 0
        yi = 0
        for ex in range(NE):
            w = ex % 2
            S.dma("pool", lambda e, w=w, ex=ex: e.dma_start(out=W1[w][:], in_=io["wg"][ex].rearrange("(k p) f -> p k f", p=128)),
                  writes=[("W1", w)], key=("W1", w))
            S.dma("pool", lambda e, w=w, ex=ex: e.dma_start(out=W2[w][:], in_=io["wu"][ex].rearrange("(k p) f -> p k f", p=128)),
                  writes=[("W2", w)], key=("W2", w))
            S.dma("pool", lambda e, w=w, ex=ex: e.dma_start(out=W3[w][:], in_=io["wd"][ex].rearrange("(k p) f -> p k f", p=128)),
                  writes=[("W3", w)], key=("W3", w))
            S.dma("sp", lambda e, w=w, ex=ex: e.dma_start(out=xgt[w][:], in_=io["xg"][ex * CAP:(ex + 1) * CAP, :].rearrange("(n p) d -> p n d", p=128)),
                  writes=[("xgt", w)], key=("xgt", w))
            for n in range(2):
                for k in range(16):
                    S.op("pe", lambda e, k=k, n=n, w=w: e.transpose(out=PT[:, k * 128:(k + 1) * 128], in_=xgt[w][:, n, k * 128:(k + 1) * 128],
                                                                    identity=ident[:]), reads=[("xgt", w), "ident"], writes=["PT"])
                ei += 1
                if ei % 2:
                    S.op("act", lambda e, n=n, w=w: e.copy(out=xT[w][:, :, n * 128:(n + 1) * 128], in_=PT[:].rearrange("p (k t) -> p k t", k=16)),
                         writes=["PT", ("xT", w)])
                else:
                    S.op("dve", lambda e, n=n, w=w: e.tensor_copy(out=xT[w][:, :, n * 128:(n + 1) * 128], in_=PT[:].rearrange("p (k t) -> p k t", k=16)),
                         writes=["PT", ("xT", w)])
            for f in range(4):
                pg, kpg = PGU[gi % 4], ("PGU", gi % 4)
                gi += 1
                pu, kpu = PGU[gi % 4], ("PGU", gi % 4)
                gi += 1
                for k in range(16):
                    S.op("pe", lambda e, k=k, f=f, pg=pg, w=w: e.matmul(pg[:, 0:256], lhsT=W1[w][:, k, f * 128:(f + 1) * 128], rhs=xT[w][:, k, :],
                                                                        start=(k == 0), stop=(k == 15)),
                         reads=[("W1", w), ("xT", w)], writes=[kpg])
                for k in range(16):
                    S.op("pe", lambda e, k=k, f=f, pu=pu, w=w: e.matmul(pu[:, 0:256], lhsT=W2[w][:, k, f * 128:(f + 1) * 128], rhs=xT[w][:, k, :],
                                                                        start=(k == 0), stop=(k == 15)),
                         reads=[("W2", w), ("xT", w)], writes=[kpu])
                sgb, ksg = sg[f % 2], ("sg", f % 2)
                S.op("act", lambda e, pg=pg, sgb=sgb: e.activation(out=sgb[:], in_=pg[:, 0:256], func=AF.Silu), writes=[kpg, ksg])
                S.op("dve", lambda e, pu=pu, sgb=sgb, w=w, f=f: e.tensor_tensor(out=hT[w][:, f, :], in0=pu[:, 0:256], in1=sgb[:], op=ALU.mult),
                     reads=[ksg], writes=[kpu, ("hT", w)])
            for n in range(2):
                yb, kyb = ysb[yi % 2], ("ysb", yi % 2)
                yi += 1
                for cg in range(4):
                    pd, kpd = PD[di % 2], ("PD", di % 2)
                    di += 1
                    for f in range(4):
                        S.op("pe", lambda e, f=f, n=n, cg=cg, pd=pd, w=w: e.matmul(
                            pd[:], lhsT=hT[w][:, f, n * 128:(n + 1) * 128], rhs=W3[w][:, f, cg * 512:(cg + 1) * 512],
                            start=(f == 0), stop=(f == 3)), reads=[("hT", w), ("W3", w)], writes=[kpd])
                    if cg % 2:
                        S.op("act", lambda e, pd=pd, yb=yb, cg=cg: e.copy(out=yb[:, cg * 512:(cg + 1) * 512], in_=pd[:]), writes=[kpd, kyb])
                    else:
                        S.op("dve", lambda e, pd=pd, yb=yb, cg=cg: e.tensor_copy(out=yb[:, cg * 512:(cg + 1) * 512], in_=pd[:]), writes=[kpd, kyb])
                S.dma("act", lambda e, yb=yb, ex=ex, n=n: e.dma_start(out=io["yg"][ex * CAP + n * 128:ex * CAP + (n + 1) * 128, :], in_=yb[:]),
                      reads=[kyb], key=kyb)
        S.finish()


def phase_b3(nc, io):
    with contextlib.ExitStack() as st:
        S = Sched(nc, st)
        sb = lambda name, shape, dt: st.enter_context(nc.sbuf_tensor("b3_" + name, shape, dt))
        y1 = [sb("y1_%d" % i, [128, D], F32) for i in range(2)]
        y2 = [sb("y2_%d" % i, [128, D], F32) for i in range(2)]
        xm = [sb("xm%d" % i, [128, D], F32) for i in range(2)]
        yt = [sb("yt%d" % i, [128, D], F32) for i in range(2)]
        junk = sb("junk", [128, D], BF16)
        nwb = sb("nwb", [128, D], F32)
        epsb = sb("epsb", [128, 1], F32)
        gs = sb("gs", [128, 16, 2], F32)
        di_ = sb("di", [128, 16, 2], I32)
        stt = [sb("stt%d" % i, [128, 4], F32) for i in range(2)]
        S.dma("sp", lambda e: e.dma_start(out=nwb[:], in_=io["nwf"][0:1, :].partition_broadcast(128)), writes=["nwb"], key="c_nwb")
        S.dma("sp", lambda e: e.dma_start(out=gs[:], in_=io["gsel"].rearrange("(n p) c -> p n c", p=128)), writes=["gs"], key="c_gs")
        S.dma("sp", lambda e: e.dma_start(out=di_[:], in_=io["dsti"].rearrange("(n p) c -> p n c", p=128)), writes=["di"], key="c_di")
        S.op("dve", lambda e: e.memset(epsb[:], EPS), writes=["epsb"])
        for t in range(TB // 128):
            p = t % 2
            r0 = t * 128
            S.dma("sp", lambda e, p=p, r0=r0: e.dma_start(out=xm[p][:], in_=io["xmid"][r0:r0 + 128, :]), writes=[("xm", p)], key=("xm", p))
            for kk, yy in ((0, y1), (1, y2)):
                S.dma("pool", lambda e, p=p, t=t, kk=kk, yy=yy: e.indirect_dma_start(
                    out=yy[p][:, :], out_offset=None, in_=io["yg"][:, :],
                    in_offset=bass.IndirectOffsetOnAxis(ap=di_[:, t, kk:kk + 1], axis=0)),
                    reads=["di"], writes=[("y", kk, p)], key=("y", kk, p))
            S.op("dve", lambda e, p=p, t=t: e.scalar_tensor_tensor(out=xm[p][:], in0=y1[p][:], scalar=gs[:, t, 0:1], in1=xm[p][:],
                                                                  op0=ALU.mult, op1=ALU.add),
                 reads=[("y", 0, p), "gs"], writes=[("xm", p)])
            S.op("dve", lambda e, p=p, t=t: e.scalar_tensor_tensor(out=xm[p][:], in0=y2[p][:], scalar=gs[:, t, 1:2], in1=xm[p][:],
                                                                   op0=ALU.mult, op1=ALU.add),
                 reads=[("y", 1, p), "gs"], writes=[("xm", p)])
            sp_, ks = stt[p], ("stt", p)
            S.op("act", lambda e, p=p, sp_=sp_: e.activation(out=junk[:], in_=xm[p][:], func=AF.Square, accum_out=sp_[:, 0:1]),
                 reads=[("xm", p)], writes=["junk", ks])
            S.op("act", lambda e, sp_=sp_: e.activation(out=sp_[:, 1:2], in_=sp_[:, 0:1], func=AF.Sqrt, bias=epsb[:, 0:1], scale=1.0 / D),
                 reads=["epsb"], writes=[ks])
            S.op("dve", lambda e, sp_=sp_: e.reciprocal(out=sp_[:, 2:3], in_=sp_[:, 1:2]), writes=[ks])
            S.op("dve", lambda e, p=p, sp_=sp_: e.scalar_tensor_tensor(out=yt[p][:], in0=xm[p][:], scalar=sp_[:, 2:3], in1=nwb[:],
                                                                      op0=ALU.mult, op1=ALU.mult),
                 reads=[("xm", p), ks, "nwb"], writes=[("yt", p)])
            S.dma("act", lambda e, p=p, r0=r0: e.dma_start(out=io["y"][r0:r0 + 128, :], in_=yt[p][:]), reads=[("yt", p)], key=("yt", p))
        S.finish()


def phase_exchange(nc, io, Wo_t, wsem):
    sem = nc.alloc_semaphore("cc_sem")
    with nc.Block() as block:
        @block.gpsimd
        def _(e):
            for k in range(16):
                e.dma_start(out=Wo_t[:, k, :], in_=io["w_out"][k * 128:(k + 1) * 128, :]).then_inc(wsem, 16)
            for q in range(4):
                e.collective_compute("AllGather", ALU.bypass, replica_groups=[[0, 1], [2, 3], [4, 5], [6, 7]],
                                     ins=[io["mix_t"][q * 1024:(q + 1) * 1024, :].opt()],
                                     outs=[io["mixg_t"][q * 2048:(q + 1) * 2048, :].opt()]).then_inc(sem)
            e.wait_ge(sem, 4)


def build_program(upto="all"):
    nc = bass.Bass("TRN2", target_bir_lowering=False)
    io = {}

    def inp(name, shape, dt):
        io[name] = nc.dram_tensor(name, list(shape), dt, kind="ExternalInput").ap()

    def scr(name, shape, dt, out=False):
        io[name] = nc.dram_tensor(name, list(shape), dt, kind="ExternalOutput" if out else "Internal").ap()

    inp("x_b", [T, D], F32)
    inp("nw1", [1, D], F32)
    inp("w_in", [D, WCOLS], F32)
    inp("ident", [128, 128], BF16)
    dbg = upto != "all"
    scr("gqT", [NH, 128, T], F32, dbg)
    scr("gkT", [NH, 128, T], F32, dbg)
    scr("gvT", [NH, 128, T], F32, dbg)
    scr("mqT", [NH, 128, T], BF16, dbg)
    scr("mkT", [NH, 128, T], BF16, dbg)
    scr("gz", [T, 512], F32, dbg)
    scr("mv", [T, 512], BF16, dbg)
    scr("gba", [T, 8], F32, dbg)
    inp("pastneg", [128, 512], F32)
    inp("past01", [128, 512], F32)
    inp("abias", [128, 128], F32)
    inp("cmask", [128, 2, 256], BF16)
    inp("nwm", [1, 128], F32)
    inp("nwg", [1, 128], F32)
    inp("conv_w", [128, 12, 4], F32)
    inp("identf", [128, 128], F32)
    inp("maskSL", [64, 64], F32)
    inp("maskUI", [64, 64], F32)
    inp("gcon", [64, 8], F32)
    if dbg:
        scr("mix", [T, 1024], BF16, True)
    else:
        io["mix_t"] = nc.dram_tensor("mix", [T, 1024], BF16)
        io["mixg_t"] = nc.dram_tensor("mixg", [2 * T, 1024], BF16)
        io["mix"] = io["mix_t"].ap()
        io["mixg"] = io["mixg_t"].ap()
    phase_a1(nc, io, dbg)
    if upto == "a1":
        return nc
    if upto == "moba":
        phase_moba(nc, io)
        return nc
    if upto == "gdn":
        phase_gdn2(nc, io)
        return nc
    if upto == "mixers":
        phase_mixers(nc, io)
        return nc
    io["xg"] = nc.dram_tensor("xg", [NE * CAP + 128, D], BF16, kind="Internal").ap()
    phase_moba(nc, io, zero_xg=True)
    phase_gdn2(nc, io)
    if upto == "gdn":
        return nc
    inp("w_out", [D, D], F32)
    inp("xc", [TB, D], F32)
    inp("rowidx", [128, 32], I32)
    inp("nw2", [1, D], F32)
    inp("wr", [D, 36], F32)
    inp("br", [1, 36], F32)
    inp("wg", [NE, D, 512], F32)
    inp("wu", [NE, D, 512], F32)
    inp("wd", [NE, 512, D], F32)
    inp("nwf", [1, D], F32)
    io["xmid"] = nc.dram_tensor("xmid", [TB, D], F32, kind="Internal").ap()
    inp("ebase", [128, 32], F32)
    inp("uts", [128, 128], F32)
    io["h2d"] = nc.dram_tensor("h2d", [TB, D], BF16, kind="Internal").ap()
    io["yg"] = nc.dram_tensor("yg", [NE * CAP + 128, D], F32, kind="Internal").ap()
    io["gsel"] = nc.dram_tensor("gsel", [TB, 2], F32, kind="Internal").ap()
    io["dsti"] = nc.dram_tensor("dsti", [TB, 2], I32, kind="Internal").ap()
    io["y"] = nc.dram_tensor("y", [TB, D], F32, kind="ExternalOutput").ap()
    with nc.sbuf_tensor("b1_Wo", [128, 16, D], BF16) as Wo_t:
        wsem = nc.alloc_semaphore("wo_sem")
        phase_exchange(nc, io, Wo_t, wsem)
        io["Wo_pre"] = (Wo_t, wsem)
        phase_b1(nc, io)
    phase_b2(nc, io)
    phase_b3(nc, io)
    return nc


def core_inputs(inputs, c):
    b, hh = divmod(c, 2)
    w_in = inputs["w_in"][0]
    hs = slice(hh * 512, hh * 512 + 512)
    G = 1024
    cols = [w_in[:, 0 * G:1 * G][:, hs], w_in[:, 1 * G:2 * G][:, hs], w_in[:, 2 * G:3 * G][:, hs],
            w_in[:, 4 * G + 16 + 0 * G:4 * G + 16 + 1 * G][:, hs], w_in[:, 4 * G + 16 + 1 * G:4 * G + 16 + 2 * G][:, hs],
            w_in[:, 3 * G:4 * G][:, hs], w_in[:, 4 * G + 16 + 2 * G:4 * G + 16 + 3 * G][:, hs],
            w_in[:, 4 * G + hh * 4:4 * G + hh * 4 + 4], w_in[:, 4 * G + 8 + hh * 4:4 * G + 8 + hh * 4 + 4]]
    m = {
        "x_b": np.ascontiguousarray(inputs["x"][b]),
        "nw1": np.ascontiguousarray(inputs["norm_mix_w"].reshape(1, D)),
        "w_in": np.ascontiguousarray(np.concatenate(cols, axis=1)),
        "ident": np.eye(128, dtype=ml_dtypes.bfloat16),
        "nwm": np.ascontiguousarray(inputs["moba_out_norm_w"].reshape(1, 128)),
        "nwg": np.ascontiguousarray(inputs["gdn_out_norm_w"].reshape(1, 128)),
        "conv_w": np.ascontiguousarray(inputs["gdn_conv_w"][0].reshape(4, 3, 8, 128)[:, :, hh * 4:hh * 4 + 4, :].transpose(3, 1, 2, 0).reshape(128, 12, 4)),
        "gcon": np.ascontiguousarray(np.broadcast_to(np.concatenate([inputs["gdn_A_log"][0, hh * 4:hh * 4 + 4], inputs["gdn_dt_bias"][0, hh * 4:hh * 4 + 4]])[None, :], (64, 8))).astype(np.float32),
    }
    m.update(consts(hh))
    return m


_CONSTS = {}


def consts(hh):
    if hh in _CONSTS:
        return _CONSTS[hh]
    p = np.arange(128)
    tile = np.arange(32)
    j = np.arange(16)
    past = (j[None, :] < (tile[:, None] // 2))
    past01 = np.broadcast_to(past.astype(np.float32).reshape(1, 512), (128, 512)).copy()
    pastneg = ((past01 - 1.0) * 1e30).astype(np.float32)
    slopes = 2.0 ** (-8.0 * np.arange(1, 9) / 8.0)
    ab = np.zeros((128, 4, 16, 2), np.float32)
    for h in range(4):
        for dl in range(16):
            for kt in range(2):
                ab[:, h, dl, kt] = slopes[hh * 4 + h] * (-dl * 256 + kt * 128 + p - 128)
    cm = np.zeros((128, 2, 256), np.float32)
    q = np.arange(256)
    for kt in range(2):
        cm[:, kt, :] = ((kt * 128 + p)[:, None] <= q[None, :])
    i64 = np.arange(64)
    c = {"identf": np.eye(128, dtype=np.float32),
         "maskSL": (i64[:, None] > i64[None, :]).astype(np.float32),
         "maskUI": (i64[:, None] <= i64[None, :]).astype(np.float32),
         "pastneg": pastneg, "past01": past01, "abias": ab.reshape(128, 128),
         "cmask": cm.astype(ml_dtypes.bfloat16)}
    _CONSTS[hh] = c
    return c


def kernel(**inputs):
    inputs = {k: np.asarray(v) for k, v in inputs.items()}
    nc = build_program()
    w_out = inputs["w_out"][0]
    shared = {
        "w_out": np.ascontiguousarray(np.concatenate([w_out[0:512], w_out[1024:1536], w_out[512:1024], w_out[1536:2048]], axis=0)),
        "nw2": np.ascontiguousarray(inputs["norm_ffn_w"].reshape(1, D)),
        "wr": np.ascontiguousarray(np.concatenate([inputs["w_router_group"][0], inputs["w_router_expert"][0]], axis=1)),
        "br": np.ascontiguousarray(np.concatenate([inputs["b_router_group"][0], inputs["b_router_expert"][0]]).reshape(1, 36)),
        "wg": np.ascontiguousarray(inputs["w_expert_gate"][0]),
        "wu": np.ascontiguousarray(inputs["w_expert_up"][0]),
        "wd": np.ascontiguousarray(inputs["w_expert_down"][0]),
        "nwf": np.ascontiguousarray(inputs["norm_final_w"].reshape(1, D)),
        "ebase": np.ascontiguousarray(np.broadcast_to((np.arange(NE) * CAP - DUMMY).astype(np.float32)[None, :], (128, NE))),
        "uts": (np.arange(128)[:, None] < np.arange(128)[None, :]).astype(np.float32),
    }
    in_maps = []
    for c in range(8):
        b, hh = divmod(c, 2)
        m = core_inputs(inputs, c)
        m.update(shared)
        m["xc"] = np.ascontiguousarray(inputs["x"][b, hh * TB:(hh + 1) * TB])
        p = np.arange(128)[:, None]
        t = np.arange(16)[None, :]
        ri = np.zeros((128, 16, 2), np.int32)
        for r in range(2):
            ri[:, :, r] = (2 * hh + t // 8) * 2048 + r * 1024 + (t % 8) * 128 + p
        m["rowidx"] = ri.reshape(128, 32)
        in_maps.append(m)
    res = run_bass_kernel_spmd(nc, in_maps, core_ids=list(range(8)))
    y = np.stack([np.asarray(res.results[c]["y"]) for c in range(8)], axis=0)
    return y.reshape(4, T, D).astype(np.float32)
```
